# Optimizing a Trainium2 kernel written in Bass

```python
import math
import jax, jax.numpy as jnp
from jax import lax
import numpy as np

D_MODEL = 1024
BATCH = 8
SEQ = 4096
DEPTH = 1

GRID_W = 64
N_HEADS = 8
N_KV_HEADS = 2
HEAD_DIM = 64
ROPE_THETA = 10000.0
Q_BLOCK = 128
SSD_D_INNER = 1024
SSD_HEAD_DIM = 64
SSD_N_HEADS = SSD_D_INNER // SSD_HEAD_DIM
SSD_GROUPS = 2
SSD_HEADS_PER_GROUP = SSD_N_HEADS // SSD_GROUPS
SSD_D_STATE = 128
SSD_CONV_W = 5
SSD_CHUNK = 128
PEER_HEADS = 8
PEER_N_KEYS = 128
PEER_N_EXPERTS = PEER_N_KEYS * PEER_N_KEYS
PEER_D_QUERY = 256
PEER_D_HALF = PEER_D_QUERY // 2
PEER_TOPK = 16
PEER_TOKEN_BLOCK = 128
N_BRANCH = 2
EPS = 1e-6

ATT_Q_W = N_HEADS * HEAD_DIM
ATT_KV_W = N_KV_HEADS * HEAD_DIM
SSD_XBC_W = SSD_D_INNER + 2 * SSD_GROUPS * SSD_D_STATE
GATE_W = N_BRANCH * D_MODEL
IN_W = ATT_Q_W + 2 * ATT_KV_W + SSD_D_INNER + SSD_XBC_W + 2 * SSD_N_HEADS + GATE_W

kernel_name = 'hybrid_gqa_ssd_peer_encoder_block'


def rmsnorm(x, w):
    xf = x.astype(jnp.float32)
    y = xf * lax.rsqrt(jnp.mean(xf * xf, axis=-1, keepdims=True) + EPS)
    return (y * w.astype(jnp.float32)).astype(x.dtype)


def modulate(h, shift, scale):
    return h * (1.0 + scale[:, None, :]) + shift[:, None, :]


def _rotate(x, ang):
    d2 = x.shape[-1] // 2
    x1, x2 = x[..., :d2], x[..., d2:]
    cos, sin = jnp.cos(ang), jnp.sin(ang)
    return jnp.concatenate([x1 * cos - x2 * sin, x1 * sin + x2 * cos], axis=-1)


def axial_rope(x, row, col):
    half = HEAD_DIM // 2
    inv = ROPE_THETA ** (-jnp.arange(0, half, 2, dtype=jnp.float32) / half)
    xf = x.astype(jnp.float32)
    ang_r = row.astype(jnp.float32)[:, None] * inv
    ang_c = col.astype(jnp.float32)[:, None] * inv
    out = jnp.concatenate([_rotate(xf[..., :half], ang_r), _rotate(xf[..., half:], ang_c)], axis=-1)
    return out.astype(x.dtype)


def block_attention(q, k, v):
    b, H, S, hd = q.shape
    G = H // N_KV_HEADS
    nb = S // Q_BLOCK
    qb = q.reshape(b, N_KV_HEADS, G, nb, Q_BLOCK, hd).transpose(3, 0, 1, 2, 4, 5)
    scale = hd ** -0.5

    def one(qi):
        s = jnp.einsum('bkgqd,bksd->bkgqs', qi, k).astype(jnp.float32) * scale
        p = jax.nn.softmax(s, axis=-1).astype(v.dtype)
        return jnp.einsum('bkgqs,bksd->bkgqd', p, v)

    o = lax.map(one, qb)
    return o.transpose(1, 0, 4, 2, 3, 5).reshape(b, S, H * hd)


def depthwise_conv(u, w, bias):
    C = u.shape[-1]
    pad = (SSD_CONV_W - 1) // 2
    y = lax.conv_general_dilated(u, w[:, None, :].astype(u.dtype), (1,), [(pad, pad)],
                                 dimension_numbers=('NWC', 'WIO', 'NWC'), feature_group_count=C)
    return y + bias


def segsum(x):
    T = x.shape[-1]
    xe = jnp.broadcast_to(x[..., None], x.shape + (T,))
    strict = jnp.tril(jnp.ones((T, T), dtype=bool), -1)
    xe = jnp.where(strict, xe, 0.0)
    cs = jnp.cumsum(xe, axis=-2)
    return jnp.where(jnp.tril(jnp.ones((T, T), dtype=bool), 0), cs, -jnp.inf)


def ssd_causal(xh, dt, A, Bm, Cm):
    b, S, g, r, p = xh.shape
    n = Bm.shape[-1]
    nc, l = S // SSD_CHUNK, SSD_CHUNK
    X = (xh * dt[..., None]).reshape(b, nc, l, g, r, p)
    Adt = (dt * A).reshape(b, nc, l, g, r).transpose(0, 3, 4, 1, 2)
    Bc = Bm.reshape(b, nc, l, g, n)
    Cc = Cm.reshape(b, nc, l, g, n)
    A_cs = jnp.cumsum(Adt, axis=-1)
    Lmat = jnp.exp(segsum(Adt))
    cb = jnp.einsum('bclgn,bcsgn->bgcls', Cc, Bc)
    y_diag = jnp.einsum('bgrcls,bcsgrp->bclgrp', cb[:, :, None] * Lmat, X)
    decay_states = jnp.exp(A_cs[..., -1:] - A_cs)
    states = jnp.einsum('bclgn,bgrcl,bclgrp->bcgrpn', Bc, decay_states, X)
    states = jnp.concatenate([jnp.zeros_like(states[:, :1]), states], axis=1)
    last = jnp.pad(A_cs[..., -1], ((0, 0), (0, 0), (0, 0), (1, 0)))
    decay_chunk = jnp.exp(segsum(last))
    new_states = jnp.einsum('bgrzc,bcgrpn->bzgrpn', decay_chunk, states)
    states = new_states[:, :-1]
    y_off = jnp.einsum('bclgn,bcgrpn,bgrcl->bclgrp', Cc, states, jnp.exp(A_cs))
    return (y_diag + y_off).reshape(b, S, g, r, p)


def ssd_branch(z, xbc, dtf, dtb, conv_w, conv_b, dt_bias_f, dt_bias_b, a_log_f, a_log_b, d_skip, ssd_norm_w):
    b, S, _ = z.shape
    G, R, P, N = SSD_GROUPS, SSD_HEADS_PER_GROUP, SSD_HEAD_DIM, SSD_D_STATE
    xbc = jax.nn.silu(depthwise_conv(xbc, conv_w, conv_b))
    xs, Bm, Cm = jnp.split(xbc, [SSD_D_INNER, SSD_D_INNER + G * N], axis=-1)
    xh = xs.astype(jnp.float32).reshape(b, S, G, R, P)
    Bm = Bm.astype(jnp.float32).reshape(b, S, G, N)
    Cm = Cm.astype(jnp.float32).reshape(b, S, G, N)

    def disc(raw, bias, a_log):
        dt = jax.nn.softplus(raw.astype(jnp.float32) + bias.astype(jnp.float32)).reshape(b, S, G, R)
        A = -jnp.exp(a_log.astype(jnp.float32)).reshape(G, R)
        return dt, A

    dt_f, A_f = disc(dtf, dt_bias_f, a_log_f)
    dt_b, A_b = disc(dtb, dt_bias_b, a_log_b)
    flip = lambda t: jnp.flip(t, axis=1)
    y_f = ssd_causal(xh, dt_f, A_f, Bm, Cm)
    y_b = flip(ssd_causal(flip(xh), flip(dt_b), A_b, flip(Bm), flip(Cm)))
    y = y_f + y_b + xh * d_skip.astype(jnp.float32).reshape(G, R)[:, :, None]
    y = y.reshape(b, S, SSD_D_INNER).astype(z.dtype)
    return rmsnorm(y * jax.nn.silu(z), ssd_norm_w)


def token_mixer(h, row, col, w_in, q_gain, k_gain, conv_w, conv_b, dt_bias_f, dt_bias_b,
                a_log_f, a_log_b, d_skip, ssd_norm_w, w_attn_up, w_ssd_up, w_out):
    b, S, _ = h.shape
    proj = h @ w_in
    o1 = ATT_Q_W
    o2 = o1 + ATT_KV_W
    o3 = o2 + ATT_KV_W
    o4 = o3 + SSD_D_INNER
    o5 = o4 + SSD_XBC_W
    o6 = o5 + SSD_N_HEADS
    o7 = o6 + SSD_N_HEADS
    q, k, v, z, xbc, dtf, dtb, gl = jnp.split(proj, [o1, o2, o3, o4, o5, o6, o7], axis=-1)
    q = q.reshape(b, S, N_HEADS, HEAD_DIM).transpose(0, 2, 1, 3)
    k = k.reshape(b, S, N_KV_HEADS, HEAD_DIM).transpose(0, 2, 1, 3)
    v = v.reshape(b, S, N_KV_HEADS, HEAD_DIM).transpose(0, 2, 1, 3)
    q = axial_rope(rmsnorm(q, q_gain), row, col)
    k = axial_rope(rmsnorm(k, k_gain), row, col)
    att = block_attention(q, k, v)
    ssd = ssd_branch(z, xbc, dtf, dtb, conv_w, conv_b, dt_bias_f, dt_bias_b,
                     a_log_f, a_log_b, d_skip, ssd_norm_w)
    gates = jax.nn.sigmoid(gl.astype(jnp.float32)).astype(h.dtype).reshape(b, S, N_BRANCH, D_MODEL)
    merged = gates[:, :, 0] * (att @ w_attn_up) + gates[:, :, 1] * (ssd @ w_ssd_up)
    return merged @ w_out


def peer(h, w_query, keys1, keys2, u_tab, v_tab):
    b, S, D = h.shape
    xb = h.reshape(-1, PEER_TOKEN_BLOCK, D)

    def one(xt):
        T = xt.shape[0]
        q = (xt @ w_query).reshape(T, PEER_HEADS, 2, PEER_D_HALF)
        s1 = jnp.einsum('thd,hkd->thk', q[:, :, 0], keys1).astype(jnp.float32)
        s2 = jnp.einsum('thd,hkd->thk', q[:, :, 1], keys2).astype(jnp.float32)
        v1, i1 = lax.top_k(s1, PEER_TOPK)
        v2, i2 = lax.top_k(s2, PEER_TOPK)
        cand = (v1[..., :, None] + v2[..., None, :]).reshape(T, PEER_HEADS, PEER_TOPK * PEER_TOPK)
        cidx = (i1[..., :, None] * PEER_N_KEYS + i2[..., None, :]).reshape(T, PEER_HEADS, PEER_TOPK * PEER_TOPK)
        sc, pos = lax.top_k(cand, PEER_TOPK)
        idx = jnp.take_along_axis(cidx, pos, axis=-1)
        g = jax.nn.softmax(sc, axis=-1)
        u = u_tab[idx]
        a = jax.nn.gelu(jnp.einsum('thkd,td->thk', u, xt).astype(jnp.float32))
        vv = v_tab[idx]
        return jnp.einsum('thk,thkd->td', (g * a).astype(vv.dtype), vv)

    return lax.map(one, xb).reshape(b, S, D)


def setup_inputs(seed: int = 0) -> dict:
    key = jax.random.key(seed)
    ks = jax.random.split(key, 32)
    f = jnp.float32
    L, D = DEPTH, D_MODEL

    def nrm(k, shape, scale):
        return jax.random.normal(k, shape, f) * scale

    dt_f0 = jnp.exp(jax.random.uniform(ks[10], (L, SSD_N_HEADS), f, math.log(1e-3), math.log(1e-1)))
    dt_b0 = jnp.exp(jax.random.uniform(ks[11], (L, SSD_N_HEADS), f, math.log(1e-3), math.log(1e-1)))
    return {
        'x': nrm(ks[0], (BATCH, SEQ, D), 1.0),
        'c': nrm(ks[1], (BATCH, D), 1.0),
        'ada_w': nrm(ks[2], (L, D, 6 * D), D ** -0.5),
        'ada_b': nrm(ks[3], (L, 6 * D), 0.02),
        'norm1_w': 1.0 + nrm(ks[4], (L, D), 0.02),
        'w_in': nrm(ks[5], (L, D, IN_W), D ** -0.5),
        'q_gain': 1.0 + nrm(ks[6], (L, HEAD_DIM), 0.02),
        'k_gain': 1.0 + nrm(ks[7], (L, HEAD_DIM), 0.02),
        'conv_w': nrm(ks[8], (L, SSD_CONV_W, SSD_XBC_W), SSD_CONV_W ** -0.5),
        'conv_b': nrm(ks[9], (L, SSD_XBC_W), 0.02),
        'dt_bias_f': dt_f0 + jnp.log(-jnp.expm1(-dt_f0)),
        'dt_bias_b': dt_b0 + jnp.log(-jnp.expm1(-dt_b0)),
        'a_log_f': jnp.log(jax.random.uniform(ks[12], (L, SSD_N_HEADS), f, 1.0, 16.0)),
        'a_log_b': jnp.log(jax.random.uniform(ks[13], (L, SSD_N_HEADS), f, 1.0, 16.0)),
        'd_skip': 1.0 + nrm(ks[14], (L, SSD_N_HEADS), 0.02),
        'ssd_norm_w': 1.0 + nrm(ks[15], (L, SSD_D_INNER), 0.02),
        'w_attn_up': nrm(ks[16], (L, ATT_Q_W, D), ATT_Q_W ** -0.5),
        'w_ssd_up': nrm(ks[17], (L, SSD_D_INNER, D), SSD_D_INNER ** -0.5),
        'w_out': nrm(ks[18], (L, D, D), D ** -0.5),
        'norm2_w': 1.0 + nrm(ks[19], (L, D), 0.02),
        'peer_w_query': nrm(ks[20], (L, D, PEER_HEADS * PEER_D_QUERY), D ** -0.5),
        'peer_keys1': nrm(ks[21], (L, PEER_HEADS, PEER_N_KEYS, PEER_D_HALF), PEER_D_HALF ** -0.5),
        'peer_keys2': nrm(ks[22], (L, PEER_HEADS, PEER_N_KEYS, PEER_D_HALF), PEER_D_HALF ** -0.5),
        'peer_u': nrm(ks[23], (L, PEER_N_EXPERTS, D), D ** -0.5),
        'peer_v': nrm(ks[24], (L, PEER_N_EXPERTS, D), PEER_HEADS ** -0.5),
        'final_norm_w': 1.0 + nrm(ks[25], (D,), 0.02),
    }


def reference(x, c, ada_w, ada_b, norm1_w, w_in, q_gain, k_gain, conv_w, conv_b,
              dt_bias_f, dt_bias_b, a_log_f, a_log_b, d_skip, ssd_norm_w,
              w_attn_up, w_ssd_up, w_out, norm2_w, peer_w_query, peer_keys1,
              peer_keys2, peer_u, peer_v, final_norm_w):
    S = x.shape[1]
    rows = S // GRID_W
    row = jnp.repeat(jnp.arange(rows, dtype=jnp.int32), GRID_W)
    col = jnp.tile(jnp.arange(GRID_W, dtype=jnp.int32), rows)
    c_act = jax.nn.silu(c)
    for l in range(DEPTH):
        mod = c_act @ ada_w[l] + ada_b[l]
        sh1, sc1, g1, sh2, sc2, g2 = jnp.split(mod, 6, axis=-1)
        h = modulate(rmsnorm(x, norm1_w[l]), sh1, sc1)
        mix = token_mixer(h, row, col, w_in[l], q_gain[l], k_gain[l], conv_w[l], conv_b[l],
                          dt_bias_f[l], dt_bias_b[l], a_log_f[l], a_log_b[l], d_skip[l],
                          ssd_norm_w[l], w_attn_up[l], w_ssd_up[l], w_out[l])
        x = x + g1[:, None, :] * mix
        h = modulate(rmsnorm(x, norm2_w[l]), sh2, sc2)
        x = x + g2[:, None, :] * peer(h, peer_w_query[l], peer_keys1[l], peer_keys2[l], peer_u[l], peer_v[l])
    return rmsnorm(x, final_norm_w)
```

```python
import contextlib
import numpy as np
import concourse.bass as bass
import concourse.mybir as mybir
from concourse.bass_utils import run_bass_kernel_spmd

F32 = mybir.dt.float32
BF16 = mybir.dt.bfloat16
U32 = mybir.dt.uint32
I32 = mybir.dt.int32
AF = mybir.ActivationFunctionType
ALU = mybir.AluOpType
AX = mybir.AxisListType

S = 4096
D = 1024
NT = S // 128
EPS = 1e-6
IN_W = 5408
STRICT = True


class Sem:
    def __init__(self, h):
        self.h = h
        self.cnt = 0


class Eng:
    def __init__(self, name, h, sem):
        self.name = name
        self.h = h
        self.sem = sem
        self.seen = {}


class Buf:
    __slots__ = ("w", "rs")

    def __init__(self):
        self.w = None
        self.rs = []


class Ctx:
    def __init__(self, nc, es):
        self.nc = nc
        self.es = es
        self.engs = {}
        for name in ["tensor", "vector", "scalar", "gpsimd", "sync"]:
            sem = Sem(es.enter_context(nc.semaphore("e_" + name)))
            self.engs[name] = Eng(name, getattr(nc, name), sem)
        self.bufs = {}
        self.dsems = {}
        self.ninst = 0

    def buf(self, key):
        b = self.bufs.get(key)
        if b is None:
            b = Buf()
            self.bufs[key] = b
        return b

    def dsem(self, key):
        s = self.dsems.get(key)
        if s is None:
            s = Sem(self.es.enter_context(self.nc.semaphore("d%d" % len(self.dsems))))
            self.dsems[key] = s
        return s

    def _waits(self, E, reads, writes, skip_self=False):
        need = {}

        def add(st):
            sem, val = st
            if need.get(sem, 0) < val:
                need[sem] = val

        for k in reads:
            b = self.bufs.get(k)
            if b is not None and b.w is not None:
                add(b.w)
        for k in writes:
            b = self.buf(k)
            if b.w is not None:
                add(b.w)
            for r in b.rs:
                add(r)
        for sem, val in need.items():
            if sem is E.sem and (skip_self or not STRICT):
                continue
            if E.seen.get(sem, 0) >= val:
                continue
            E.h.wait_ge(sem.h, val)
            E.seen[sem] = val
            self.ninst += 1

    def _stamp(self, stamp, reads, writes):
        for k in reads:
            self.buf(k).rs.append(stamp)
        for k in writes:
            b = self.buf(k)
            b.w = stamp
            b.rs = []

    def op(self, eng, fn, reads=(), writes=()):
        E = self.engs[eng]
        self._waits(E, reads, writes, skip_self=(eng == "tensor"))
        inst = fn(E.h)
        E.sem.cnt += 1
        inst.then_inc(E.sem.h, 1)
        self.ninst += 1
        self._stamp((E.sem, E.sem.cnt), reads, writes)
        return inst

    def dma(self, queue, semkey, out, in_, reads=(), writes=(), **kw):
        E = self.engs[queue]
        sem = self.dsem(semkey)
        self._waits(E, reads, writes)
        if E.seen.get(sem, 0) < sem.cnt:
            E.h.wait_ge(sem.h, sem.cnt)
            E.seen[sem] = sem.cnt
        inst = E.h.dma_start(out=out, in_=in_, **kw)
        sem.cnt += 16
        inst.then_inc(sem.h, 16)
        self.ninst += 1
        self._stamp((sem, sem.cnt), reads, writes)
        return inst

    def barrier(self):
        sems = [e.sem for e in self.engs.values()] + list(self.dsems.values())
        for E in self.engs.values():
            for s in sems:
                if s is E.sem and not STRICT:
                    continue
                if s.cnt > 0 and E.seen.get(s, 0) < s.cnt:
                    E.h.wait_ge(s.h, s.cnt)
                    E.seen[s] = s.cnt
        self.bufs = {k: v for k, v in self.bufs.items() if isinstance(k, tuple) and k[0] in KEEP}


KEEP = ("u_bf", "v_bf", "x1_d", "ssdtm_d")


def rope_tables():
    half = 32
    inv = (10000.0 ** (-np.arange(0, half, 2, dtype=np.float32) / half)).astype(np.float32)
    t = np.arange(S)
    row = (t // 64).astype(np.float32)
    col = (t % 64).astype(np.float32)
    ar = row[:, None] * inv
    ac = col[:, None] * inv
    C = np.concatenate([np.cos(ar), np.cos(ar), np.cos(ac), np.cos(ac)], axis=1).astype(np.float32)
    Sg = np.concatenate([-np.sin(ar), np.sin(ar), -np.sin(ac), np.sin(ac)], axis=1).astype(np.float32)
    return C, Sg


def build(debug=None):
    debug = debug or ()
    nc = bass.Bass("TRN2", target_bir_lowering=False)
    es = contextlib.ExitStack()

    def din(name, shape, dt=F32):
        return nc.dram_tensor(name, list(shape), dt, kind="ExternalInput").ap()

    def dscratch(name, shape, dt):
        kind = "ExternalOutput" if name in debug else "Internal"
        return nc.dram_tensor(name, list(shape), dt, kind=kind).ap()

    x = din("x", [S, D])
    c_col = din("c_col", [128, 8])
    ada_w = din("ada_w", [D, 6 * D])
    ada_bT = din("ada_bT", [128, 48])
    n1T = din("n1T", [128, 8])
    n2T = din("n2T", [128, 8])
    w_in = din("w_in", [D, IN_W])
    ident_in = din("ident", [128, 128])
    ropeC_in = din("ropeC", [S, 64])
    ropeS_in = din("ropeS", [S, 64])
    qkgain_in = din("qkgain", [1, 640])
    convwT_in = din("convwT", [128, 12 * 5])
    convbT_in = din("convbT", [128, 12])
    trif_in = din("trif", [128, 128])
    trib_in = din("trib", [128, 128])
    maskf_in = din("maskf", [128, 128])
    maskb_in = din("maskb", [128, 128])
    ssdp_in = din("ssdp", [1, 80])
    ssdnw_in = din("ssdnw", [1, 1024])
    wq_in = din("wq", [D, 2048])
    keysT_in = din("keysT", [128, 2048])
    iota128_in = din("iota128", [128, 128])
    iota16_in = din("iota16", [128, 16])
    fnw_in = din("fnw", [1, D])
    U_in = din("U_h", [128 * 128, D])
    V_in = din("V_h", [128 * 128, D])
    h2T_d = dscratch("h2T_d", [D, S], BF16)
    u_bf = dscratch("u_bf", [64 * 128, 2 * D], BF16)
    v_bf = dscratch("v_bf", [64 * 128, 2 * D], BF16)
    wau_in = din("wau", [512, D])
    wsu_in = din("wsu", [D, D])
    wout_in = din("wout", [D, D])
    x1_d = dscratch("x1_d", [S, D], F32)
    attT_d = dscratch("attT_d", [512, S], BF16)
    xstm_d = dscratch("xstm_d", [S, 1024], BF16)
    yb_d = dscratch("yb_d", [S, 1024], F32)
    ssdtm_d = dscratch("ssdtm_d", [S, 1024], BF16)
    zs_d = dscratch("zs_d", [S, 1024], BF16)
    xbcT_d = dscratch("xbcT_d", [1536, S], BF16)
    gatesT_d = dscratch("gatesT_d", [2048, S], BF16)
    out = nc.dram_tensor("out", [S, D], F32, kind="ExternalOutput").ap()
    dbg_out = {}
    for name, shape, dt in [("d_modT", [128, 48], F32), ("d_hT", [128, 8 * S], BF16),
                            ("d_QT", [128, 4 * S], BF16), ("d_KT", [128, S], BF16), ("d_V", [128, NT * 130], BF16),
                            ("d_dtraw", [128, NT * 32], F32),
                            ("d_attm", [128, NT * 512], BF16),
                            ("d_ssdT", [128, 8 * S], BF16), ("d_h2T", [128, 8 * S], BF16), ("d_i1T", [128, S], BF16),
                            ("d_i2T", [128, S], BF16), ("d_gT", [128, S], BF16)]:
        if name in debug:
            dbg_out[name] = nc.dram_tensor(name, shape, dt, kind="ExternalOutput").ap()

    with es:
        cx = Ctx(nc, es)

        def sb(name, shape, dt):
            return es.enter_context(nc.sbuf_tensor("s_" + name, list(shape), dt))

        ident = sb("ident", [128, 128], F32)
        identb = sb("identb", [128, 128], BF16)
        ones_f = sb("ones_f", [128, 128], F32)
        cact = sb("cact", [128, 8], F32)
        modT = sb("modT", [128, 48], F32)
        abT = sb("abT", [128, 48], F32)
        n1 = sb("n1", [128, 8], F32)
        n2 = sb("n2", [128, 8], F32)
        gam1 = sb("gam1", [128, 8], F32)
        gam2 = sb("gam2", [128, 8], F32)
        hT = sb("hT", [128, 8, S], BF16)
        dtraw = sb("dtraw", [128, NT, 32], F32)
        mid = contextlib.ExitStack()

        def sbm(name, shape, dt):
            return mid.enter_context(nc.sbuf_tensor("m_" + name, list(shape), dt))
        QT = sbm("QT", [128, 4, S], BF16)
        KT = sbm("KT", [128, S], BF16)
        Vsb = sbm("Vsb", [128, NT, 2, 65], BF16)
        ropeC = sbm("ropeC", [128, NT, 64], F32)
        ropeS = sbm("ropeS", [128, NT, 64], F32)
        qkgain = sbm("qkgain", [128, 640], F32)
        convwT = sbm("convwT", [128, 12, 5], F32)
        convbT = sbm("convbT", [128, 12], F32)

        cx.dma("sync", "c0", ident[:], ident_in[:, :], writes=["ident"])
        cx.dma("sync", "c1", cact[:], c_col[:, :], writes=["cact"])
        cx.dma("sync", "c2", abT[:], ada_bT[:, :], writes=["abT"])
        cx.dma("sync", "c3", n1[:], n1T[:, :], writes=["n1"])
        cx.dma("sync", "c4", n2[:], n2T[:, :], writes=["n2"])
        cx.dma("sync", "c5", ropeC[:], ropeC_in.rearrange("(i p) j -> p i j", p=128), writes=["ropeC"])
        cx.dma("sync", "c6", ropeS[:], ropeS_in.rearrange("(i p) j -> p i j", p=128), writes=["ropeS"])
        cx.dma("sync", "c7", qkgain[:], qkgain_in.partition_broadcast(128), writes=["qkgain"])
        cx.dma("sync", "c8", convwT[:].rearrange("p a b -> p (a b)"), convwT_in[:, :], writes=["convwT"])
        cx.dma("sync", "c9", convbT[:], convbT_in[:, :], writes=["convbT"])
        cx.op("vector", lambda e: e.tensor_scalar(out=qkgain[:, 0:512], in0=qkgain[:, 0:512], scalar1=0.125, scalar2=None,
                                                  op0=ALU.mult), reads=["qkgain"], writes=["qkgain"])
        cx.op("vector", lambda e: e.memset(Vsb[:].rearrange("p a b c -> p (a b c)"), 1.0), writes=["Vsb"])
        cx.op("vector", lambda e: e.tensor_copy(out=identb[:], in_=ident[:]), reads=["ident"], writes=["identb"])
        cx.op("vector", lambda e: e.memset(ones_f[:], 1.0), writes=["ones_f"])
        cx.op("scalar", lambda e: e.activation(out=cact[:], in_=cact[:], func=AF.Silu), reads=["cact"], writes=["cact"])

        with contextlib.ExitStack() as st:
            aw = [st.enter_context(nc.sbuf_tensor("aw%d" % i, [128, 8, 512], F32)) for i in range(2)]
            psm = st.enter_context(nc.psum_tensor("psm", [128, 48], F32))
            awv = ada_w.rearrange("(kc p) n -> p kc n", p=128)
            for blk in range(12):
                t = aw[blk % 2]
                key = "aw%d" % (blk % 2)
                cx.dma("sync", key, t[:], awv[:, :, blk * 512:(blk + 1) * 512], writes=[key])
                for jj in range(4):
                    j = blk * 4 + jj
                    for kc in range(8):
                        cx.op("tensor", lambda e, t=t, jj=jj, kc=kc, j=j: e.matmul(
                            psm[:, j:j + 1], lhsT=t[:, kc, jj * 128:(jj + 1) * 128], rhs=cact[:, kc:kc + 1],
                            start=(kc == 0), stop=(kc == 7)), reads=[key, "cact"], writes=["psm"])
            cx.op("vector", lambda e: e.tensor_tensor(out=modT[:], in0=psm[:], in1=abT[:], op=ALU.add),
                  reads=["psm", "abT"], writes=["modT"])
            cx.op("vector", lambda e: e.scalar_tensor_tensor(out=gam1[:], in0=modT[:, 8:16], scalar=1.0, in1=n1[:],
                                                             op0=ALU.add, op1=ALU.mult),
                  reads=["modT", "n1"], writes=["gam1"])
            cx.op("vector", lambda e: e.scalar_tensor_tensor(out=gam2[:], in0=modT[:, 32:40], scalar=1.0, in1=n2[:],
                                                             op0=ALU.add, op1=ALU.mult),
                  reads=["modT", "n2"], writes=["gam2"])
            if "d_modT" in debug:
                cx.dma("sync", "dbg", dbg_out["d_modT"][:, :], modT[:], reads=["modT"])
            s0_scope = st.pop_all()

        def norm_mod_transpose(src, gam, shT, st, pfx, rkey=None, gkeys=()):
            xt = [st.enter_context(nc.sbuf_tensor(pfx + "xt%d" % i, [128, D], F32)) for i in range(2)]
            xn = [st.enter_context(nc.sbuf_tensor(pfx + "xn%d" % i, [128, D], BF16)) for i in range(2)]
            junk = st.enter_context(nc.sbuf_tensor(pfx + "junk", [128, D], BF16))
            ssq = st.enter_context(nc.sbuf_tensor(pfx + "ssq", [128, NT], F32))
            rstd = st.enter_context(nc.sbuf_tensor(pfx + "rstd", [128, NT], F32))
            pst = [st.enter_context(nc.psum_tensor(pfx + "pst%d" % i, [128, 8, 128], BF16)) for i in range(2)]
            for i in range(NT):
                p = i % 2
                kx, kn, kp = pfx + "xt%d" % p, pfx + "xn%d" % p, pfx + "pst%d" % p
                cx.dma("sync", kx, xt[p][:], src[i * 128:(i + 1) * 128, :], reads=([(rkey, i)] if rkey else []), writes=[kx])
                cx.op("scalar", lambda e, p=p, i=i: e.activation(out=junk[:], in_=xt[p][:], func=AF.Square,
                                                                 accum_out=ssq[:, i:i + 1]),
                      reads=[kx], writes=[pfx + "junk", (pfx + "ssq", i)])
                cx.op("vector", lambda e, i=i: e.tensor_scalar(out=rstd[:, i:i + 1], in0=ssq[:, i:i + 1],
                                                               scalar1=1.0 / D, scalar2=EPS, op0=ALU.mult, op1=ALU.add),
                      reads=[(pfx + "ssq", i)], writes=[(pfx + "rstd", i)])
                cx.op("scalar", lambda e, i=i: e.activation(out=rstd[:, i:i + 1], in_=rstd[:, i:i + 1], func=AF.Sqrt),
                      reads=[(pfx + "rstd", i)], writes=[(pfx + "rstd", i)])
                cx.op("vector", lambda e, i=i: e.reciprocal(out=rstd[:, i:i + 1], in_=rstd[:, i:i + 1]),
                      reads=[(pfx + "rstd", i)], writes=[(pfx + "rstd", i)])
                cx.op("vector", lambda e, p=p, i=i: e.tensor_scalar(out=xn[p][:], in0=xt[p][:], scalar1=rstd[:, i:i + 1],
                                                                    scalar2=None, op0=ALU.mult),
                      reads=[kx, (pfx + "rstd", i)], writes=[kn])
                for c in range(8):
                    cx.op("tensor", lambda e, p=p, c=c: e.transpose(out=pst[p][:, c, :], in_=xn[p][:, c * 128:(c + 1) * 128],
                                                                    identity=identb[:]),
                          reads=[kn, "identb"], writes=[kp])
                for c in range(8):
                    cx.op("scalar", lambda e, p=p, c=c, i=i: e.activation(
                        out=hT[:, c, i * 128:(i + 1) * 128], in_=pst[p][:, c, :], func=AF.Identity,
                        scale=gam[:, c:c + 1], bias=shT[:, c:c + 1]),
                        reads=[kp] + list(gkeys), writes=[("hT", i)])

        with contextlib.ExitStack() as st:
            norm_mod_transpose(x, gam1, modT[:, 0:8], st, "a", gkeys=["gam1", "modT"])
            if "d_hT" in debug:
                cx.dma("sync", "dbg", dbg_out["d_hT"][:, :], hT[:].rearrange("p c s -> p (c s)"),
                       reads=[("hT", i) for i in range(NT)])
            cx.barrier()
        s0_scope.close()


        wv = w_in.rearrange("(kc p) n -> p kc n", p=128)
        with contextlib.ExitStack() as st:
            wA = st.enter_context(nc.sbuf_tensor("wA", [128, 8, 800], BF16))
            wZ = st.enter_context(nc.sbuf_tensor("wZ", [128, 8, 1024], BF16))
            psA = [st.enter_context(nc.psum_tensor("psA%d" % i, [128, 1024], F32)) for i in range(2)]
            psZ = st.enter_context(nc.psum_tensor("psZ", [128, 1024], F32))
            pT = st.enter_context(nc.psum_tensor("pT", [128, 5, 128], BF16))
            qkv = [st.enter_context(nc.sbuf_tensor("qkv%d" % i, [128, 800], F32)) for i in range(2)]
            sq = st.enter_context(nc.sbuf_tensor("sq", [128, 640], F32))
            qn = st.enter_context(nc.sbuf_tensor("qn", [128, 640], F32))
            qa = st.enter_context(nc.sbuf_tensor("qa", [128, 640], F32))
            qb = st.enter_context(nc.sbuf_tensor("qb", [128, 640], F32))
            qr = st.enter_context(nc.sbuf_tensor("qr", [128, 640], BF16))
            ss = st.enter_context(nc.sbuf_tensor("ss10", [128, 10], F32))
            zst = [st.enter_context(nc.sbuf_tensor("zst%d" % i, [128, 1024], BF16)) for i in range(2)]
            cx.dma("gpsimd", "wA", wA[:], wv[:, :, 0:800], writes=["wA"])
            cx.dma("gpsimd", "wZ", wZ[:], wv[:, :, 800:1824], writes=["wZ"])
            for i in range(NT):
                p = i % 2
                tsl = slice(i * 128, (i + 1) * 128)
                kA, kq = "psA%d" % p, "qkv%d" % p
                for (c0, c1) in [(0, 512), (512, 800)]:
                    for kc in range(8):
                        cx.op("tensor", lambda e, p=p, c0=c0, c1=c1, kc=kc: e.matmul(
                            psA[p][:, c0:c1], lhsT=hT[:, kc, tsl], rhs=wA[:, kc, c0:c1], start=(kc == 0), stop=(kc == 7)),
                            reads=[("hT", i), "wA"], writes=[kA])
                for (c0, c1) in [(0, 512), (512, 1024)]:
                    for kc in range(8):
                        cx.op("tensor", lambda e, c0=c0, c1=c1, kc=kc: e.matmul(
                            psZ[:, c0:c1], lhsT=hT[:, kc, tsl], rhs=wZ[:, kc, c0:c1], start=(kc == 0), stop=(kc == 7)),
                            reads=[("hT", i), "wZ"], writes=["psZ"])
                cx.op("scalar", lambda e, p=p: e.activation(out=qkv[p][:], in_=psA[p][:, 0:800], func=AF.Copy),
                      reads=[kA], writes=[kq])
                kz = "zst%d" % p
                cx.op("scalar", lambda e, p=p: e.activation(out=zst[p][:], in_=psZ[:], func=AF.Silu),
                      reads=["psZ"], writes=[kz])
                cx.dma("sync", kz, zs_d[tsl, :], zst[p][:], reads=[kz], writes=[("zs_d", i)])
                Q = qkv[p]
                cx.op("vector", lambda e, Q=Q: e.tensor_tensor(out=sq[:], in0=Q[:, 0:640], in1=Q[:, 0:640], op=ALU.mult),
                      reads=[kq], writes=["sq"])
                cx.op("vector", lambda e: e.tensor_reduce(out=ss[:], in_=sq[:].rearrange("p (h d) -> p h d", d=64),
                                                          axis=AX.X, op=ALU.add), reads=["sq"], writes=["ss10"])
                cx.op("vector", lambda e: e.tensor_scalar(out=ss[:], in0=ss[:], scalar1=1.0 / 64, scalar2=EPS,
                                                          op0=ALU.mult, op1=ALU.add), reads=["ss10"], writes=["ss10"])
                cx.op("scalar", lambda e: e.activation(out=ss[:], in_=ss[:], func=AF.Sqrt), reads=["ss10"], writes=["ss10"])
                cx.op("vector", lambda e: e.reciprocal(out=ss[:], in_=ss[:]), reads=["ss10"], writes=["ss10"])
                cx.op("vector", lambda e, Q=Q: e.tensor_tensor(
                    out=qn[:].rearrange("p (h d) -> p h d", d=64), in0=Q[:, 0:640].rearrange("p (h d) -> p h d", d=64),
                    in1=ss[:].unsqueeze(2).to_broadcast([128, 10, 64]), op=ALU.mult), reads=[kq, "ss10"], writes=["qn"])
                cx.op("vector", lambda e: e.tensor_tensor(out=qn[:], in0=qn[:], in1=qkgain[:], op=ALU.mult),
                      reads=["qn", "qkgain"], writes=["qn"])
                cx.op("vector", lambda e, i=i: e.tensor_tensor(
                    out=qa[:].rearrange("p (h d) -> p h d", d=64), in0=qn[:].rearrange("p (h d) -> p h d", d=64),
                    in1=ropeC[:, i, :].unsqueeze(1).to_broadcast([128, 10, 64]), op=ALU.mult),
                    reads=["qn", "ropeC"], writes=["qa"])
                for blk in range(2):
                    for hh in range(2):
                        o0 = blk * 32 + hh * 16
                        i0 = blk * 32 + (1 - hh) * 16
                        cx.op("vector", lambda e, i=i, o0=o0, i0=i0: e.tensor_tensor(
                            out=qb[:].rearrange("p (h d) -> p h d", d=64)[:, :, o0:o0 + 16],
                            in0=qn[:].rearrange("p (h d) -> p h d", d=64)[:, :, i0:i0 + 16],
                            in1=ropeS[:, i, o0:o0 + 16].unsqueeze(1).to_broadcast([128, 10, 16]), op=ALU.mult),
                            reads=["qn", "ropeS"], writes=["qb"])
                cx.op("vector", lambda e: e.tensor_tensor(out=qr[:], in0=qa[:], in1=qb[:], op=ALU.add),
                      reads=["qa", "qb"], writes=["qr"])
                for j in range(5):
                    cx.op("tensor", lambda e, j=j: e.transpose(out=pT[:, j, :], in_=qr[:, j * 128:(j + 1) * 128],
                                                               identity=identb[:]), reads=["qr", "identb"], writes=["pT"])
                cx.op("scalar", lambda e: e.activation(out=QT[:, :, tsl], in_=pT[:, 0:4, :], func=AF.Copy),
                      reads=["pT"], writes=[("QT", i)])
                cx.op("scalar", lambda e: e.activation(out=KT[:, tsl], in_=pT[:, 4, :], func=AF.Copy),
                      reads=["pT"], writes=[("KT", i)])
                cx.op("vector", lambda e, Q=Q, i=i: e.tensor_copy(
                    out=Vsb[:, i, :, 0:64], in_=Q[:, 640:768].rearrange("p (h d) -> p h d", d=64)),
                    reads=[kq, "Vsb"], writes=[("Vsb", i)])
                cx.op("vector", lambda e, Q=Q, i=i: e.tensor_copy(out=dtraw[:, i, :], in_=Q[:, 768:800]),
                      reads=[kq], writes=[("dtraw", i)])
            cx.barrier()
        with contextlib.ExitStack() as st:
            wF = [st.enter_context(nc.sbuf_tensor("wF%d" % i, [128, 8, 512], BF16)) for i in range(2)]
            psF = [st.enter_context(nc.psum_tensor("psF%d" % i, [128, 512], F32)) for i in range(4)]
            stage = st.enter_context(nc.sbuf_tensor("stage", [128, S + 4], F32))
            acc = st.enter_context(nc.sbuf_tensor("acc", [128, S], F32))
            ob = [st.enter_context(nc.sbuf_tensor("ob%d" % i, [128, S], BF16)) for i in range(2)]
            cx.op("vector", lambda e: e.memset(stage[:], 0.0), writes=["stage"])
            n = 0
            for j in range(28):
                isx = j < 12
                jj = j if isx else j - 12
                bb = j // 4
                W = wF[bb % 2]
                kW = "wF%d" % (bb % 2)
                if j % 4 == 0:
                    cx.dma("gpsimd", kW, W[:], wv[:, :, 1824 + bb * 512:1824 + (bb + 1) * 512], writes=[kW])
                jw = j % 4
                o = ob[j % 2]
                ko = "ob%d" % (j % 2)
                for g in range(8):
                    b = n % 4
                    n += 1
                    gs = slice(g * 512, (g + 1) * 512)
                    for kc in range(8):
                        cx.op("tensor", lambda e, b=b, kc=kc, W=W, jw=jw, gs=gs: e.matmul(
                            psF[b][:], lhsT=W[:, kc, jw * 128:(jw + 1) * 128], rhs=hT[:, kc, gs],
                            start=(kc == 0), stop=(kc == 7)),
                            reads=["hTall", kW], writes=["psF%d" % b])
                    if isx:
                        cx.op("scalar", lambda e, b=b, g=g: e.activation(out=stage[:, 2 + g * 512:2 + (g + 1) * 512],
                                                                         in_=psF[b][:], func=AF.Copy),
                              reads=["psF%d" % b, "acc0"], writes=[("stage", g)])
                    else:
                        cx.op("scalar", lambda e, b=b, gs=gs, o=o: e.activation(out=o[:, gs], in_=psF[b][:], func=AF.Sigmoid),
                              reads=["psF%d" % b], writes=[ko])
                if isx:
                    srd = [("stage", g) for g in range(8)]
                    cx.op("vector", lambda e, jj=jj: e.tensor_scalar(
                        out=acc[:], in0=stage[:, 0:S], scalar1=convwT[:, jj, 0:1], scalar2=convbT[:, jj:jj + 1],
                        op0=ALU.mult, op1=ALU.add), reads=srd + ["convwT", "convbT"], writes=["acc"])
                    for k in range(1, 5):
                        cx.op("vector", lambda e, jj=jj, k=k: e.scalar_tensor_tensor(
                            out=acc[:], in0=stage[:, k:k + S], scalar=convwT[:, jj, k:k + 1], in1=acc[:],
                            op0=ALU.mult, op1=ALU.add), reads=srd + ["acc"], writes=["acc"] + (["acc0"] if k == 4 else []))
                    cx.op("scalar", lambda e, o=o: e.activation(out=o[:], in_=acc[:], func=AF.Silu),
                          reads=["acc"], writes=[ko])
                    cx.dma("sync", ko, xbcT_d[jj * 128:(jj + 1) * 128, :], o[:], reads=[ko], writes=[("xbcT_d", jj)])
                else:
                    cx.dma("sync", ko, gatesT_d[jj * 128:(jj + 1) * 128, :], o[:], reads=[ko], writes=[("gatesT_d", jj)])
            for nm, t in [("d_QT", QT), ("d_KT", KT), ("d_V", Vsb), ("d_dtraw", dtraw)]:
                if nm in debug:
                    flat = t[:] if len(t.shape) == 2 else (
                        t[:].rearrange("p a b -> p (a b)") if len(t.shape) == 3 else t[:].rearrange("p a b c -> p (a b c)"))
                    cx.dma("sync", "dbg", dbg_out[nm][:, :], flat)
            cx.barrier()


        with contextlib.ExitStack() as st:
            psS = [st.enter_context(nc.psum_tensor("psS%d" % i, [128, 512], F32)) for i in range(4)]
            psO = [st.enter_context(nc.psum_tensor("psO%d" % i, [128, 512], F32)) for i in range(2)]
            pTr = st.enter_context(nc.psum_tensor("pTr", [128, 4, 65], F32))
            PT = [st.enter_context(nc.sbuf_tensor("PT%d" % i, [128, 512], BF16)) for i in range(4)]
            oT = [st.enter_context(nc.sbuf_tensor("oT%d" % i, [128, 512], F32)) for i in range(2)]
            attm = st.enter_context(nc.sbuf_tensor("attm", [128, NT, 512], BF16))
            rden = st.enter_context(nc.sbuf_tensor("rden", [128, 4], F32))
            pTa = st.enter_context(nc.psum_tensor("pTa", [128, 4, 128], BF16))
            attT = hT
            cu = [st.enter_context(nc.sbuf_tensor("cu%d" % i, [128, 2, D], BF16)) for i in range(2)]
            cv = [st.enter_context(nc.sbuf_tensor("cv%d" % i, [128, 2, D], BF16)) for i in range(2)]
            for jj in range(64):
                p = jj % 2
                rs = slice(jj * 256, (jj + 1) * 256)
                cx.dma("gpsimd", "cu%d" % p, cu[p][:], U_in[rs, :].rearrange("(a p) n -> p a n", p=128), writes=["cu%d" % p])
                cx.dma("sync", "cuo%d" % p, u_bf[jj * 128:(jj + 1) * 128, :], cu[p][:].rearrange("p a n -> p (a n)"), reads=["cu%d" % p],
                       writes=[("u_bf", 2 * jj), ("u_bf", 2 * jj + 1)])
                cx.dma("gpsimd", "cv%d" % p, cv[p][:], V_in[rs, :].rearrange("(a p) n -> p a n", p=128), writes=["cv%d" % p])
                cx.dma("sync", "cvo%d" % p, v_bf[jj * 128:(jj + 1) * 128, :], cv[p][:].rearrange("p a n -> p (a n)"), reads=["cv%d" % p],
                       writes=[("v_bf", 2 * jj), ("v_bf", 2 * jj + 1)])
            steps = [(j, qg, kt) for j in range(4) for qg in range(8) for kt in range(NT)]

            def emit_qk(n):
                j, qg, kt = steps[n]
                for grp in range(2):
                    b = 2 * (n % 2) + grp
                    ps_ = slice(grp * 64, (grp + 1) * 64)
                    cx.op("tensor", lambda e, b=b, ps_=ps_: e.matmul(
                        psS[b][:], lhsT=KT[ps_, kt * 128:(kt + 1) * 128], rhs=QT[ps_, j, qg * 512:(qg + 1) * 512],
                        start=True, stop=True), reads=["QT", "KT"], writes=["psS%d" % b])

            emit_qk(0)
            for n, (j, qg, kt) in enumerate(steps):
                if n + 1 < len(steps):
                    emit_qk(n + 1)
                for grp in range(2):
                    b = 2 * (n % 2) + grp
                    cx.op("scalar", lambda e, b=b: e.activation(out=PT[b][:], in_=psS[b][:], func=AF.Exp),
                          reads=["psS%d" % b], writes=["PT%d" % b])
                for grp in range(2):
                    b = 2 * (n % 2) + grp
                    cx.op("tensor", lambda e, b=b, kt=kt, grp=grp: e.matmul(
                        psO[grp][0:65, :], lhsT=Vsb[:, kt, grp, :], rhs=PT[b][:],
                        start=(kt == 0), stop=(kt == NT - 1)), reads=["PT%d" % b, "Vsb"], writes=["psO%d" % grp])
                if kt == NT - 1:
                    for grp in range(2):
                        pos = 2 * j + grp
                        cx.op("scalar", lambda e, grp=grp: e.activation(out=oT[grp][0:65, :], in_=psO[grp][0:65, :], func=AF.Copy),
                              reads=["psO%d" % grp], writes=["oT%d" % grp])
                        for sub in range(4):
                            cx.op("tensor", lambda e, grp=grp, sub=sub: e.transpose(
                                out=pTr[:, sub, :], in_=oT[grp][0:65, sub * 128:(sub + 1) * 128], identity=ident[0:65, 0:65]),
                                reads=["oT%d" % grp, "ident"], writes=["pTr"])
                        cx.op("vector", lambda e: e.reciprocal(out=rden[:].unsqueeze(2), in_=pTr[:, :, 64:65]),
                              reads=["pTr"], writes=["rden"])
                        cx.op("vector", lambda e, qg=qg, pos=pos: e.tensor_tensor(
                            out=attm[:, qg * 4:(qg + 1) * 4, pos * 64:(pos + 1) * 64], in0=pTr[:, :, 0:64],
                            in1=rden[:].unsqueeze(2).to_broadcast([128, 4, 64]), op=ALU.mult),
                            reads=["pTr", "rden"], writes=[("attm", qg * 4 + k) for k in range(4)])
            if "d_attm" in debug:
                cx.dma("sync", "dbg", dbg_out["d_attm"][:, :], attm[:].rearrange("p a b -> p (a b)"),
                       reads=[("attm", ti) for ti in range(NT)])
            for i in range(NT):
                for c in range(4):
                    cx.op("tensor", lambda e, i=i, c=c: e.transpose(out=pTa[:, c, :], in_=attm[:, i, c * 128:(c + 1) * 128],
                                                                    identity=identb[:]), reads=[("attm", i)], writes=["pTa"])
                cx.op("scalar", lambda e, i=i: e.activation(out=attT[:, 0:4, i * 128:(i + 1) * 128], in_=pTa[:], func=AF.Copy),
                      reads=["pTa"], writes=[("attT", i)])
            for c in range(4):
                cx.dma("sync", "attTd", attT_d[c * 128:(c + 1) * 128, :], attT[:, c, :],
                       reads=[("attT", i) for i in range(NT)], writes=["attT_d"])
            cx.barrier()
        mid.close()


        ssdT = hT
        with contextlib.ExitStack() as st:
            def T(name, shape, dt):
                return st.enter_context(nc.sbuf_tensor("y_" + name, list(shape), dt))
            trif = T("trif", [128, 128], F32); trib = T("trib", [128, 128], F32)
            ssdp = T("ssdp", [128, 80], F32)
            ssdnw = T("ssdnw", [128, 1024], F32)
            dtv = T("dtv", [128, NT, 32], F32)
            Adt = T("Adt", [128, NT, 32], F32)
            BT = T("BT", [128, 2, S], BF16)
            CT = T("CT", [128, 2, S], BF16)
            Btm = T("Btm", [128, NT, 2, 128], BF16)
            cx.dma("sync", "k0", trif[:], trif_in[:, :], writes=["trif"])
            cx.dma("sync", "k1", trib[:], trib_in[:, :], writes=["trib"])
            cx.dma("sync", "k4", ssdp[:], ssdp_in.partition_broadcast(128), writes=["ssdp"])
            cx.dma("sync", "k5", ssdnw[:], ssdnw_in.partition_broadcast(128), writes=["ssdnw"])
            for g in range(2):
                cx.dma("sync", "k6", BT[:, g, :], xbcT_d[1024 + g * 128:1024 + (g + 1) * 128, :], writes=["BT"])
                cx.dma("sync", "k7", CT[:, g, :], xbcT_d[1280 + g * 128:1280 + (g + 1) * 128, :], writes=["CT"])
            cx.op("vector", lambda e: e.tensor_tensor(out=dtv[:], in0=dtraw[:], in1=ssdp[:, 0:32].unsqueeze(1).to_broadcast([128, NT, 32]),
                                                      op=ALU.add), reads=["ssdp"], writes=["dtv"])
            cx.op("scalar", lambda e: e.activation(out=dtv[:], in_=dtv[:], func=AF.Exp), reads=["dtv"], writes=["dtv"])
            cx.op("scalar", lambda e: e.activation(out=dtv[:], in_=dtv[:], func=AF.Ln, bias=1.0), reads=["dtv"], writes=["dtv"])
            cx.op("scalar", lambda e: e.activation(out=ssdp[:, 32:64], in_=ssdp[:, 32:64], func=AF.Exp), reads=["ssdp"], writes=["ssdp"])
            cx.op("vector", lambda e: e.scalar_tensor_tensor(out=Adt[:], in0=dtv[:], scalar=-1.0,
                                                             in1=ssdp[:, 32:64].unsqueeze(1).to_broadcast([128, NT, 32]),
                                                             op0=ALU.mult, op1=ALU.mult), reads=["dtv", "ssdp"], writes=["Adt"])
            with contextlib.ExitStack() as st2:
                pT4 = [st2.enter_context(nc.psum_tensor("pT4%d" % i, [128, 4, 128], BF16)) for i in range(2)]
                xch = [st2.enter_context(nc.sbuf_tensor("y_xch%d" % i, [128, S], BF16)) for i in range(2)]
                xst = [st2.enter_context(nc.sbuf_tensor("y_xst%d" % i, [128, 1024], BF16)) for i in range(2)]
                n = 0
                for g in range(2):
                    for i0 in range(0, NT, 4):
                        b = n % 2; n += 1
                        for k in range(4):
                            cx.op("tensor", lambda e, b=b, k=k, g=g, i0=i0: e.transpose(
                                out=pT4[b][:, k, :], in_=BT[:, g, (i0 + k) * 128:(i0 + k + 1) * 128], identity=identb[:]),
                                reads=["BT"], writes=["pT4%d" % b])
                        cx.op("scalar", lambda e, b=b, g=g, i0=i0: e.activation(out=Btm[:, i0:i0 + 4, g, :], in_=pT4[b][:], func=AF.Copy),
                              reads=["pT4%d" % b], writes=["Btm"])
                for i in range(NT):
                    cx.buf(("xstm", i))
                for c in range(8):
                    X = xch[c % 2]; kx = "xch%d" % (c % 2)
                    cx.dma("sync", kx, X[:], xbcT_d[c * 128:(c + 1) * 128, :], writes=[kx])
                    for i0 in range(0, NT, 4):
                        b = n % 2; n += 1
                        for k in range(4):
                            cx.op("tensor", lambda e, b=b, k=k, X=X, i0=i0: e.transpose(
                                out=pT4[b][:, k, :], in_=X[:, (i0 + k) * 128:(i0 + k + 1) * 128], identity=identb[:]),
                                reads=[kx], writes=["pT4%d" % b])
                        q = (i0 // 4) % 2
                        cx.op("scalar", lambda e, b=b, q=q: e.activation(out=xst[q][:, 0:512].rearrange("p (a b) -> p a b", a=4),
                                                                         in_=pT4[b][:], func=AF.Copy),
                              reads=["pT4%d" % b], writes=["xst%d" % q])
                        cx.dma("sync", "xst%d" % q,
                               xstm_d[i0 * 128:(i0 + 4) * 128, c * 128:(c + 1) * 128].rearrange("(a p) n -> p a n", p=128),
                               xst[q][:, 0:512].rearrange("p (a b) -> p a b", a=4),
                               reads=["xst%d" % q], writes=[("xstm", i0 + k) for k in range(4)])
                cx.barrier()

            psCBA = st.enter_context(nc.psum_tensor("psCBA", [128, 3, 128], F32))
            psBC = [st.enter_context(nc.psum_tensor("psBC%d" % g, [128, 8, 128], F32)) for g in range(2)]
            psY = [st.enter_context(nc.psum_tensor("psY%d" % g, [128, 512], F32)) for g in range(2)]
            psYS = st.enter_context(nc.psum_tensor("psYS", [128, 512], F32))
            psAcs = psCBA[:, 2, 0:16]
            Acs = T("Acs", [128, 16], F32)
            expA = T("expA", [128, 16], F32)
            rhs2 = [T("rhs2%d" % g, [128, 8, 128], F32) for g in range(2)]
            d1 = [T("d1%d" % g, [128, 8, 128], F32) for g in range(2)]
            LT = [T("LT%d" % g, [128, 8, 128], F32) for g in range(2)]
            Mb = [T("Mb%d" % g, [128, 8, 128], BF16) for g in range(2)]
            cbm = [T("cbm%d" % g, [128, 128], F32) for g in range(2)]
            Xb = T("Xb", [128, 1024], BF16)
            Xd = [T("Xd%d" % g, [128, 512], BF16) for g in range(2)]
            dec = [T("dec%d" % g, [128, 8], F32) for g in range(2)]
            cd = [T("cd%d" % g, [128, 8], F32) for g in range(2)]
            tmpy = [T("tmpy%d" % g, [128, 512], F32) for g in range(2)]
            ydir = [T("ydir%d" % i, [128, 1024], F32) for i in range(2)]
            state = [T("state%d" % g, [128, 512], F32) for g in range(2)]
            stbf = [T("stbf%d" % g, [128, 512], BF16) for g in range(2)]
            xs_t = [T("xs_t%d" % i, [128, 1024], BF16) for i in range(2)]
            zs_t = [T("zs_t%d" % i, [128, 1024], BF16) for i in range(2)]
            yb_t = [T("yb_t%d" % i, [128, 1024], F32) for i in range(2)]
            gg = T("gg", [128, 1024], F32)
            junk2 = T("junk2", [128, 1024], BF16)
            ssq = T("ssq1", [128, 1], F32)
            ssdtm = [T("ssdtm0", [128, 1024], BF16)] * 2

            def ssd_pass(fwd):
                o = 0 if fwd else 16
                tri = trif if fwd else trib
                ktri = "trif" if fwd else "trib"
                lend = 127 if fwd else 0
                order = list(range(NT)) if fwd else list(range(NT - 1, -1, -1))
                for idx, i in enumerate(order):
                    p = idx % 2
                    tsl = slice(i * 128, (i + 1) * 128)
                    kxs = "xs_t%d" % p
                    cx.dma("sync", kxs, xs_t[p][:], xstm_d[tsl, :], reads=[("xstm", i)], writes=[kxs])
                    if fwd:
                        cx.dma("sync", "zs_t%d" % p, zs_t[p][:], zs_d[tsl, :], writes=["zs_t%d" % p])
                        cx.dma("sync", "yb_t%d" % p, yb_t[p][:], yb_d[tsl, :], reads=[("yb_d", i)], writes=["yb_t%d" % p])
                    cx.op("tensor", lambda e, i=i: e.matmul(psAcs, lhsT=tri[:], rhs=Adt[:, i, o:o + 16], start=True, stop=True),
                          reads=[ktri, "Adt"], writes=["psAcs"])
                    cx.op("vector", lambda e: e.tensor_copy(out=Acs[:], in_=psAcs), reads=["psAcs"], writes=["Acs"])
                    cx.op("scalar", lambda e: e.activation(out=expA[:], in_=psAcs, func=AF.Exp), reads=["psAcs"], writes=["expA"])
                    cx.op("vector", lambda e, p=p, i=i: e.tensor_tensor(
                        out=Xb[:].rearrange("p (h d) -> p h d", d=64), in0=xs_t[p][:].rearrange("p (h d) -> p h d", d=64),
                        in1=dtv[:, i, o:o + 16].unsqueeze(2).to_broadcast([128, 16, 64]), op=ALU.mult),
                        reads=[kxs, "dtv"], writes=["Xb"])
                    yd = ydir[p]; kyd = "ydir%d" % p
                    for g in range(2):
                        hs = slice(g * 8, (g + 1) * 8)
                        G = str(g)
                        cx.op("vector", lambda e, i=i, g=g: e.tensor_tensor(
                            out=rhs2[g][:], in0=tri[:].unsqueeze(1).to_broadcast([128, 8, 128]),
                            in1=Adt[:, i, o + g * 8:o + g * 8 + 8].unsqueeze(2).to_broadcast([128, 8, 128]), op=ALU.mult),
                            reads=[ktri, "Adt"], writes=["rhs2" + G])
                        for hh in range(2):
                            cx.op("tensor", lambda e, hh=hh, g=g: e.matmul(psBC[g][:, hh * 4:(hh + 1) * 4, :], lhsT=ones_f[:],
                                                                           rhs=rhs2[g][:, hh * 4:(hh + 1) * 4, :], start=True, stop=True),
                                  reads=["rhs2" + G, "ones_f"], writes=["psBC" + G])
                        cx.op("tensor", lambda e, g=g: e.matmul(psCBA[:, g, :], lhsT=BT[:, g, tsl], rhs=CT[:, g, tsl], start=True, stop=True),
                              reads=["BT", "CT"], writes=["psCB" + G])
                    for g in range(2):
                        hs = slice(g * 8, (g + 1) * 8)
                        G = str(g)
                        cx.op("vector", lambda e, g=g: e.tensor_tensor(out=cbm[g][:], in0=psCBA[:, g, :], in1=tri[:], op=ALU.mult),
                              reads=["psCB" + G, ktri], writes=["cbm" + G])
                        cx.op("vector", lambda e, hs=hs, g=g: e.tensor_tensor(
                            out=d1[g][:], in0=psBC[g][:], in1=Acs[:, hs].unsqueeze(2).to_broadcast([128, 8, 128]), op=ALU.subtract),
                            reads=["psBC" + G, "Acs"], writes=["d1" + G])
                        cx.op("scalar", lambda e, g=g: e.activation(out=LT[g][:], in_=d1[g][:], func=AF.Exp), reads=["d1" + G], writes=["LT" + G])
                        if idx < NT - 1:
                            cx.op("vector", lambda e, hs=hs, g=g: e.tensor_tensor(
                                out=dec[g][:].unsqueeze(2), in0=psBC[g][:, :, lend:lend + 1], in1=Acs[:, hs].unsqueeze(2), op=ALU.subtract),
                                reads=["psBC" + G, "Acs"], writes=["dec" + G])
                            cx.op("scalar", lambda e, g=g: e.activation(out=dec[g][:], in_=dec[g][:], func=AF.Exp), reads=["dec" + G], writes=["dec" + G])
                            cx.op("scalar", lambda e, g=g: e.activation(out=cd[g][:].unsqueeze(2), in_=psBC[g][:, :, lend:lend + 1], func=AF.Exp),
                                  reads=["psBC" + G], writes=["cd" + G])
                    for g in range(2):
                        G = str(g)
                        gsl = slice(g * 512, (g + 1) * 512)
                        cx.op("vector", lambda e, g=g: e.scalar_tensor_tensor(
                            out=Mb[g][:], in0=LT[g][:], scalar=1.0, in1=cbm[g][:].unsqueeze(1).to_broadcast([128, 8, 128]),
                            op0=ALU.min, op1=ALU.mult), reads=["LT" + G, "cbm" + G], writes=["Mb" + G])
                        for h in range(8):
                            cx.op("tensor", lambda e, h=h, g=g: e.matmul(
                                psY[g][:, h * 64:(h + 1) * 64], lhsT=Mb[g][:, h, :], rhs=Xb[:, (g * 8 + h) * 64:(g * 8 + h + 1) * 64],
                                start=True, stop=True), reads=["Mb" + G, "Xb"], writes=["psY" + G])
                        if idx < NT - 1:
                            cx.op("vector", lambda e, gsl=gsl, g=g: e.tensor_tensor(
                                out=Xd[g][:].rearrange("p (h d) -> p h d", d=64), in0=Xb[:, gsl].rearrange("p (h d) -> p h d", d=64),
                                in1=dec[g][:].unsqueeze(2).to_broadcast([128, 8, 64]), op=ALU.mult), reads=["Xb", "dec" + G], writes=["Xd" + G])
                    for g in range(2):
                        hs = slice(g * 8, (g + 1) * 8)
                        G = str(g)
                        gsl = slice(g * 512, (g + 1) * 512)
                        if idx > 0:
                            cx.op("tensor", lambda e, g=g: e.matmul(psYS[:], lhsT=CT[:, g, tsl], rhs=stbf[g][:], start=True, stop=True),
                                  reads=["CT", "stbf" + G], writes=["psYS"])
                            cx.op("vector", lambda e, hs=hs, g=g: e.tensor_tensor(
                                out=tmpy[g][:].rearrange("p (h d) -> p h d", d=64), in0=psYS[:].rearrange("p (h d) -> p h d", d=64),
                                in1=expA[:, hs].unsqueeze(2).to_broadcast([128, 8, 64]), op=ALU.mult),
                                reads=["psYS", "expA"], writes=["tmpy" + G])
                            cx.op("vector", lambda e, yd=yd, gsl=gsl, g=g: e.tensor_tensor(out=yd[:, gsl], in0=tmpy[g][:], in1=psY[g][:], op=ALU.add),
                                  reads=["tmpy" + G, "psY" + G], writes=[(kyd, g)])
                        else:
                            cx.op("vector", lambda e, yd=yd, gsl=gsl, g=g: e.tensor_copy(out=yd[:, gsl], in_=psY[g][:]),
                                  reads=["psY" + G], writes=[(kyd, g)])
                        if idx < NT - 1:
                            cx.op("tensor", lambda e, i=i, g=g: e.matmul(psYS[:], lhsT=Btm[:, i, g, :], rhs=Xd[g][:], start=True, stop=True),
                                  reads=["Btm", "Xd" + G], writes=["psYS"])
                            if idx > 0:
                                cx.op("vector", lambda e, g=g: e.tensor_tensor(
                                    out=state[g][:].rearrange("p (h d) -> p h d", d=64), in0=state[g][:].rearrange("p (h d) -> p h d", d=64),
                                    in1=cd[g][:].unsqueeze(2).to_broadcast([128, 8, 64]), op=ALU.mult),
                                    reads=["state" + G, "cd" + G], writes=["state" + G])
                                cx.op("vector", lambda e, g=g: e.tensor_tensor(out=state[g][:], in0=state[g][:], in1=psYS[:], op=ALU.add),
                                      reads=["state" + G, "psYS"], writes=["state" + G])
                            else:
                                cx.op("vector", lambda e, g=g: e.tensor_copy(out=state[g][:], in_=psYS[:]),
                                      reads=["psYS"], writes=["state" + G])
                            cx.op("scalar", lambda e, g=g: e.activation(out=stbf[g][:], in_=state[g][:], func=AF.Copy),
                                  reads=["state" + G], writes=["stbf" + G])
                    ykeys = [(kyd, 0), (kyd, 1)]
                    if not fwd:
                        cx.dma("sync", kyd, yb_d[tsl, :], yd[:], reads=ykeys, writes=[("yb_d", i)])
                    else:
                        cx.op("vector", lambda e, yd=yd, p=p: e.tensor_tensor(out=yd[:], in0=yd[:], in1=yb_t[p][:], op=ALU.add),
                              reads=ykeys + ["yb_t%d" % p], writes=ykeys)
                        cx.op("vector", lambda e, p=p: e.tensor_tensor(
                            out=gg[:].rearrange("p (h d) -> p h d", d=64), in0=xs_t[p][:].rearrange("p (h d) -> p h d", d=64),
                            in1=ssdp[:, 64:80].unsqueeze(2).to_broadcast([128, 16, 64]), op=ALU.mult),
                            reads=[kxs, "ssdp"], writes=["gg"])
                        cx.op("vector", lambda e, yd=yd: e.tensor_tensor(out=gg[:], in0=gg[:], in1=yd[:], op=ALU.add),
                              reads=["gg"] + ykeys, writes=["gg"])
                        cx.op("vector", lambda e, p=p: e.tensor_tensor(out=gg[:], in0=gg[:], in1=zs_t[p][:], op=ALU.mult),
                              reads=["gg", "zs_t%d" % p], writes=["gg"])
                        cx.op("scalar", lambda e: e.activation(out=junk2[:], in_=gg[:], func=AF.Square, accum_out=ssq[:]),
                              reads=["gg"], writes=["junk2", "ssq1"])
                        cx.op("vector", lambda e: e.tensor_scalar(out=ssq[:], in0=ssq[:], scalar1=1.0 / 1024, scalar2=EPS,
                                                                  op0=ALU.mult, op1=ALU.add), reads=["ssq1"], writes=["ssq1"])
                        cx.op("scalar", lambda e: e.activation(out=ssq[:], in_=ssq[:], func=AF.Sqrt), reads=["ssq1"], writes=["ssq1"])
                        cx.op("vector", lambda e: e.reciprocal(out=ssq[:], in_=ssq[:]), reads=["ssq1"], writes=["ssq1"])
                        cx.op("vector", lambda e, p=p: e.scalar_tensor_tensor(out=ssdtm[p][:], in0=gg[:], scalar=ssq[:, 0:1], in1=ssdnw[:],
                                                                              op0=ALU.mult, op1=ALU.mult),
                              reads=["gg", "ssq1", "ssdnw"], writes=["ssdtm0"])
                        cx.dma("sync", "ssdtm0", ssdtm_d[tsl, :], ssdtm[p][:], reads=["ssdtm0"], writes=[("ssdtm_d", i)])

            ssd_pass(False)
            ssd_pass(True)
            cx.barrier()
        with contextlib.ExitStack() as st:
            psT8 = [st.enter_context(nc.psum_tensor("psT8%d" % i, [128, 8, 128], BF16)) for i in range(2)]
            stl = [st.enter_context(nc.sbuf_tensor("y_stl%d" % i, [128, 1024], BF16)) for i in range(2)]
            for i in range(NT):
                p = i % 2
                cx.dma("sync", "stl%d" % p, stl[p][:], ssdtm_d[i * 128:(i + 1) * 128, :], reads=[("ssdtm_d", i)], writes=["stl%d" % p])
                for c in range(8):
                    cx.op("tensor", lambda e, c=c, p=p: e.transpose(out=psT8[p][:, c, :], in_=stl[p][:, c * 128:(c + 1) * 128],
                                                                    identity=identb[:]), reads=["stl%d" % p], writes=["psT8%d" % p])
                cx.op("scalar", lambda e, p=p, i=i: e.activation(out=ssdT[:, :, i * 128:(i + 1) * 128], in_=psT8[p][:], func=AF.Copy),
                      reads=["psT8%d" % p], writes=[("hT", i)])
            if "d_ssdT" in debug:
                cx.dma("sync", "dbg", dbg_out["d_ssdT"][:, :], ssdT[:].rearrange("p c s -> p (c s)"),
                       reads=[("hT", i) for i in range(NT)])
            cx.barrier()


        g2bc = sb("g2bc", [128, D], F32)
        g1scope = contextlib.ExitStack()
        g1bc = g1scope.enter_context(nc.sbuf_tensor("s_g1bc", [128, D], F32))
        with contextlib.ExitStack() as st:
            diag = [st.enter_context(nc.sbuf_tensor("diag%d" % i, [128, 128], F32)) for i in range(2)]
            psG = st.enter_context(nc.psum_tensor("psG", [128, 1024], F32))
            for (dst, kd, c0) in [(g1bc, "g1bc", 16), (g2bc, "g2bc", 40)]:
                for c in range(8):
                    dg = diag[c % 2]; kg = "diag%d" % (c % 2)
                    cx.op("vector", lambda e, dg=dg, c=c, c0=c0: e.tensor_scalar(out=dg[:], in0=ident[:], scalar1=modT[:, c0 + c:c0 + c + 1],
                                                                                 scalar2=None, op0=ALU.mult), reads=["ident", "modT"], writes=[kg])
                    cx.op("tensor", lambda e, dg=dg, c=c: e.matmul(psG[:, c * 128:(c + 1) * 128], lhsT=ones_f[:], rhs=dg[:], start=True, stop=True),
                          reads=[kg, "ones_f"], writes=["psG"])
                cx.op("vector", lambda e, dst=dst: e.tensor_copy(out=dst[:], in_=psG[:]), reads=["psG"], writes=[kd])
            cx.barrier()
        ssdT = hT
        with contextlib.ExitStack() as st:
            def T(name, shape, dt):
                return st.enter_context(nc.sbuf_tensor("z_" + name, list(shape), dt))
            wau = T("wau", [128, 4, D], BF16)
            wsu = T("wsu", [128, 8, D], BF16)
            wout = T("wout", [128, 8, D], BF16)
            attg = [T("attg%d" % i, [128, 4, 512], BF16) for i in range(2)]
            g0t = [T("g0t%d" % i, [128, 512], BF16) for i in range(2)]
            g1t = [T("g1t%d" % i, [128, 512], BF16) for i in range(2)]
            t1 = T("t1", [128, 512], F32)
            t2 = T("t2", [128, 512], F32)
            mT = T("mT", [128, 8, 512], BF16)
            xin = [T("xin%d" % i, [128, D], F32) for i in range(2)]
            x1t = [T("x1t%d" % i, [128, D], F32) for i in range(2)]
            psUa = [st.enter_context(nc.psum_tensor("psUa%d" % i, [128, 512], F32)) for i in range(2)]
            psUs = [st.enter_context(nc.psum_tensor("psUs%d" % i, [128, 512], F32)) for i in range(2)]
            psM = [st.enter_context(nc.psum_tensor("psM%d" % i, [128, 1024], F32)) for i in range(2)]
            cx.dma("gpsimd", "wau", wau[:], wau_in.rearrange("(kc p) n -> p kc n", p=128), writes=["wau"])
            cx.dma("gpsimd", "wsu", wsu[:], wsu_in.rearrange("(kc p) n -> p kc n", p=128), writes=["wsu"])
            cx.dma("gpsimd", "wout", wout[:], wout_in.rearrange("(kc p) n -> p kc n", p=128), writes=["wout"])
            n = 0
            for grp in range(8):
                gs = slice(grp * 512, (grp + 1) * 512)
                ag = attg[grp % 2]; ka = "attg%d" % (grp % 2)
                cx.dma("sync", ka, ag[:], attT_d[:, gs].rearrange("(c p) s -> p c s", p=128), reads=["attT_d"], writes=[ka])
                for dc in range(8):
                    b = n % 2; n += 1
                    dsl = slice(dc * 128, (dc + 1) * 128)
                    cx.dma("sync", "g0t%d" % b, g0t[b][:], gatesT_d[dc * 128:(dc + 1) * 128, gs], writes=["g0t%d" % b])
                    cx.dma("sync", "g1t%d" % b, g1t[b][:], gatesT_d[1024 + dc * 128:1024 + (dc + 1) * 128, gs], writes=["g1t%d" % b])
                    for kc in range(4):
                        cx.op("tensor", lambda e, b=b, kc=kc, dsl=dsl, ag=ag: e.matmul(psUa[b][:], lhsT=wau[:, kc, dsl], rhs=ag[:, kc, :],
                                                                                     start=(kc == 0), stop=(kc == 3)),
                              reads=["wau", ka], writes=["psUa%d" % b])
                    for kc in range(8):
                        cx.op("tensor", lambda e, b=b, kc=kc, dsl=dsl, gs=gs: e.matmul(psUs[b][:], lhsT=wsu[:, kc, dsl], rhs=ssdT[:, kc, gs],
                                                                                     start=(kc == 0), stop=(kc == 7)),
                              reads=["wsu"] + [("hT", grp * 4 + k) for k in range(4)], writes=["psUs%d" % b])
                    cx.op("vector", lambda e, b=b: e.tensor_tensor(out=t1[:], in0=psUa[b][:], in1=g0t[b][:], op=ALU.mult),
                          reads=["psUa%d" % b, "g0t%d" % b], writes=["t1"])
                    cx.op("vector", lambda e, b=b: e.tensor_tensor(out=t2[:], in0=psUs[b][:], in1=g1t[b][:], op=ALU.mult),
                          reads=["psUs%d" % b, "g1t%d" % b], writes=["t2"])
                    cx.op("vector", lambda e, dc=dc: e.tensor_tensor(out=mT[:, dc, :], in0=t1[:], in1=t2[:], op=ALU.add),
                          reads=["t1", "t2"], writes=[("mT", dc)])
                for sub in range(4):
                    i = grp * 4 + sub
                    p = i % 2
                    tsl = slice(i * 128, (i + 1) * 128)
                    cx.dma("sync", "xin%d" % p, xin[p][:], x[tsl, :], writes=["xin%d" % p])
                    for half in range(2):
                        for dc in range(8):
                            cx.op("tensor", lambda e, p=p, half=half, dc=dc, sub=sub: e.matmul(
                                psM[p][:, half * 512:(half + 1) * 512], lhsT=mT[:, dc, sub * 128:(sub + 1) * 128],
                                rhs=wout[:, dc, half * 512:(half + 1) * 512], start=(dc == 0), stop=(dc == 7)),
                                reads=["wout"] + [("mT", d_) for d_ in range(8)], writes=["psM%d" % p])
                    cx.op("vector", lambda e, p=p: e.tensor_tensor(out=x1t[p][:], in0=psM[p][:], in1=g1bc[:], op=ALU.mult),
                          reads=["psM%d" % p, "g1bc"], writes=["x1t%d" % p])
                    cx.op("vector", lambda e, p=p: e.tensor_tensor(out=x1t[p][:], in0=x1t[p][:], in1=xin[p][:], op=ALU.add),
                          reads=["x1t%d" % p, "xin%d" % p], writes=["x1t%d" % p])
                    cx.dma("sync", "x1t%d" % p, x1_d[tsl, :], x1t[p][:], reads=["x1t%d" % p], writes=[("x1_d", i)])
            cx.barrier()
        g1scope.close()
        with contextlib.ExitStack() as st:
            norm_mod_transpose(x1_d, gam2, modT[:, 24:32], st, "b", rkey="x1_d")
            if "d_h2T" in debug:
                cx.dma("sync", "dbg", dbg_out["d_h2T"][:, :], hT[:].rearrange("p c s -> p (c s)"),
                       reads=[("hT", i) for i in range(NT)])
            cx.barrier()


        h2T = hT
        i1T = sb("i1T", [128, S], BF16)
        i2T = sb("i2T", [128, S], BF16)
        gT = sb("gT", [128, S], BF16)
        GELU = AF.Gelu_apprx_tanh
        with contextlib.ExitStack() as st:
            def T(name, shape, dt):
                return st.enter_context(nc.sbuf_tensor("r_" + name, list(shape), dt))
            wq = T("wq", [128, 8, 2048], BF16)
            keysT = T("keysT", [128, 16, 128], BF16)
            iota16 = T("iota16", [128, 16], F32)
            qT = T("qT", [128, 16, 512], BF16)
            bufA = T("bufA", [128, 2048], F32)
            bufB = T("bufB", [128, 2048], F32)
            sc = bufA[:].rearrange("p (a b) -> p a b", b=128)
            sc2 = bufB[:].rearrange("p (a b) -> p a b", b=128)
            cand = bufA[:].rearrange("p (a b) -> p a b", b=256)
            cand2 = bufB[:].rearrange("p (a b) -> p a b", b=256)
            oh = bufA[:].rearrange("p (a b) -> p a b", b=16)
            vtop = T("vtop", [128, 16, 16], F32)
            ixu = T("ixu", [128, 16, 16], U32)
            ixf = T("ixf", [128, 16, 16], F32)
            sc16 = T("sc16", [128, 8, 16], F32)
            posu = T("posu", [128, 8, 16], U32)
            au = T("au", [128, 8, 16], U32)
            bu = T("bu", [128, 8, 16], U32)
            af = T("af", [128, 8, 16], F32)
            bf = T("bf", [128, 8, 16], F32)
            esum = T("esum", [128, 8], F32)
            gf = T("gf", [128, 8, 16], F32)
            idf = [T("idf%d" % m, [128, 128], F32) for m in range(2)]
            psQ = st.enter_context(nc.psum_tensor("psQ", [128, 512], F32))
            psSc = st.enter_context(nc.psum_tensor("psSc", [128, 16, 128], F32))
            psT3 = st.enter_context(nc.psum_tensor("psT3", [128, 3, 128], F32))
            cx.dma("gpsimd", "wq", wq[:], wq_in.rearrange("(kc p) n -> p kc n", p=128), writes=["wq"])
            cx.dma("gpsimd", "r0", keysT[:].rearrange("p a b -> p (a b)"), keysT_in[:, :], writes=["keysT"])
            cx.dma("sync", "r1", iota16[:], iota16_in[:, :], writes=["iota16"])
            for grp in range(8):
                gs = slice(grp * 512, (grp + 1) * 512)
                for j in range(16):
                    for kc in range(8):
                        cx.op("tensor", lambda e, j=j, kc=kc, gs=gs: e.matmul(psQ[:], lhsT=wq[:, kc, j * 128:(j + 1) * 128], rhs=h2T[:, kc, gs],
                                                                             start=(kc == 0), stop=(kc == 7)), reads=["wq"], writes=["psQ"])
                    cx.op("scalar", lambda e, j=j: e.activation(out=qT[:, j, :], in_=psQ[:], func=AF.Copy), reads=["psQ"], writes=[("qT", j)])
                for sub in range(4):
                    i = grp * 4 + sub
                    tsl = slice(i * 128, (i + 1) * 128)
                    for j in range(16):
                        cx.op("tensor", lambda e, j=j, sub=sub: e.matmul(psSc[:, j, :], lhsT=qT[:, j, sub * 128:(sub + 1) * 128], rhs=keysT[:, j, :],
                                                                         start=True, stop=True), reads=[("qT", j), "keysT"], writes=["psSc"])
                    cx.op("scalar", lambda e: e.activation(out=sc, in_=psSc[:], func=AF.Copy), reads=["psSc"], writes=["bufA"])
                    for j in range(16):
                        cx.op("vector", lambda e, j=j: e.max(out=vtop[:, j, 0:8], in_=sc[:, j, :]), reads=["bufA"], writes=[("vtop", j)])
                    for j in range(16):
                        cx.op("vector", lambda e, j=j: e.max_index(out=ixu[:, j, 0:8], in_max=vtop[:, j, 0:8], in_values=sc[:, j, :]),
                              reads=["bufA", ("vtop", j)], writes=[("ixu", j)])
                    for j in range(16):
                        cx.op("vector", lambda e, j=j: e.match_replace(out=sc2[:, j, :], in_to_replace=vtop[:, j, 0:8], in_values=sc[:, j, :],
                                                                       imm_value=-1e30), reads=["bufA", ("vtop", j)], writes=[("bufB", j)])
                    for j in range(16):
                        cx.op("vector", lambda e, j=j: e.max(out=vtop[:, j, 8:16], in_=sc2[:, j, :]), reads=[("bufB", j)], writes=[("vtop", j)])
                    for j in range(16):
                        cx.op("vector", lambda e, j=j: e.max_index(out=ixu[:, j, 8:16], in_max=vtop[:, j, 8:16], in_values=sc2[:, j, :]),
                              reads=[("bufB", j), ("vtop", j)], writes=[("ixu", j)])
                    vk = [("vtop", j) for j in range(16)]
                    ik = [("ixu", j) for j in range(16)]
                    cx.op("vector", lambda e: e.tensor_copy(out=ixf[:], in_=ixu[:]), reads=ik, writes=["ixf"])
                    vv = vtop[:].rearrange("p (h two) k -> p h two k", two=2)
                    cx.op("vector", lambda e, vv=vv: e.tensor_tensor(
                        out=cand.rearrange("p h (a b) -> p h a b", b=16), in0=vv[:, :, 0, :].unsqueeze(3).to_broadcast([128, 8, 16, 16]),
                        in1=vv[:, :, 1, :].unsqueeze(2).to_broadcast([128, 8, 16, 16]), op=ALU.add), reads=vk, writes=["bufA"])
                    for h in range(8):
                        cx.op("vector", lambda e, h=h: e.max(out=sc16[:, h, 0:8], in_=cand[:, h, :]), reads=["bufA"], writes=[("sc16", h)])
                    for h in range(8):
                        cx.op("vector", lambda e, h=h: e.max_index(out=posu[:, h, 0:8], in_max=sc16[:, h, 0:8], in_values=cand[:, h, :]),
                              reads=["bufA", ("sc16", h)], writes=[("posu", h)])
                    for h in range(8):
                        cx.op("vector", lambda e, h=h: e.match_replace(out=cand2[:, h, :], in_to_replace=sc16[:, h, 0:8], in_values=cand[:, h, :],
                                                                       imm_value=-1e30), reads=["bufA", ("sc16", h)],
                              writes=[("bufB", 2 * h), ("bufB", 2 * h + 1)])
                    for h in range(8):
                        cx.op("vector", lambda e, h=h: e.max(out=sc16[:, h, 8:16], in_=cand2[:, h, :]),
                              reads=[("bufB", 2 * h), ("bufB", 2 * h + 1)], writes=[("sc16", h)])
                    for h in range(8):
                        cx.op("vector", lambda e, h=h: e.max_index(out=posu[:, h, 8:16], in_max=sc16[:, h, 8:16], in_values=cand2[:, h, :]),
                              reads=[("bufB", 2 * h), ("bufB", 2 * h + 1), ("sc16", h)], writes=[("posu", h)])
                    sk = [("sc16", h) for h in range(8)]
                    pk = [("posu", h) for h in range(8)]
                    cx.op("vector", lambda e: e.tensor_tensor(out=gf[:], in0=sc16[:], in1=sc16[:, :, 0:1].to_broadcast([128, 8, 16]),
                                                              op=ALU.subtract), reads=sk, writes=["gf"])
                    cx.op("scalar", lambda e: e.activation(out=gf[:], in_=gf[:], func=AF.Exp), reads=["gf"], writes=["gf"])
                    cx.op("vector", lambda e: e.tensor_reduce(out=esum[:], in_=gf[:], axis=AX.X, op=ALU.add), reads=["gf"], writes=["esum"])
                    cx.op("vector", lambda e: e.reciprocal(out=esum[:], in_=esum[:]), reads=["esum"], writes=["esum"])
                    cx.op("vector", lambda e: e.tensor_tensor(out=gf[:], in0=gf[:], in1=esum[:].unsqueeze(2).to_broadcast([128, 8, 16]),
                                                              op=ALU.mult), reads=["gf", "esum"], writes=["gf"])
                    cx.op("vector", lambda e: e.tensor_single_scalar(out=au[:], in_=posu[:], scalar=4, op=ALU.logical_shift_right),
                          reads=pk, writes=["au"])
                    cx.op("vector", lambda e: e.tensor_single_scalar(out=bu[:], in_=posu[:], scalar=15, op=ALU.bitwise_and),
                          reads=pk, writes=["bu"])
                    cx.op("vector", lambda e: e.tensor_copy(out=af[:], in_=au[:]), reads=["au"], writes=["af"])
                    cx.op("vector", lambda e: e.tensor_copy(out=bf[:], in_=bu[:]), reads=["bu"], writes=["bf"])
                    ixv = ixf[:].rearrange("p (h two) k -> p h two k", two=2)
                    for m, (sel, ksel) in enumerate([(af, "af"), (bf, "bf")]):
                        cx.op("vector", lambda e, sel=sel: e.tensor_tensor(
                            out=oh, in0=sel[:].rearrange("p h k -> p (h k)").unsqueeze(2).to_broadcast([128, 128, 16]),
                            in1=iota16[:].unsqueeze(1).to_broadcast([128, 128, 16]), op=ALU.is_equal), reads=[ksel, "iota16"], writes=["bufA"])
                        cx.op("vector", lambda e, m=m, ixv=ixv: e.tensor_tensor(
                            out=oh.rearrange("p (h k) a -> p h k a", k=16), in0=oh.rearrange("p (h k) a -> p h k a", k=16),
                            in1=ixv[:, :, m, :].unsqueeze(2).to_broadcast([128, 8, 16, 16]), op=ALU.mult), reads=["bufA", "ixf"], writes=["bufA"])
                        cx.op("vector", lambda e, m=m: e.tensor_reduce(out=idf[m][:], in_=oh, axis=AX.X, op=ALU.add),
                              reads=["bufA"], writes=["idf%d" % m])
                    cx.op("tensor", lambda e: e.transpose(out=psT3[:, 0, :], in_=idf[0][:], identity=ident[:]), reads=["idf0"], writes=["psT3"])
                    cx.op("tensor", lambda e: e.transpose(out=psT3[:, 1, :], in_=idf[1][:], identity=ident[:]), reads=["idf1"], writes=["psT3"])
                    cx.op("tensor", lambda e: e.transpose(out=psT3[:, 2, :], in_=gf[:].rearrange("p h k -> p (h k)"), identity=ident[:]),
                          reads=["gf"], writes=["psT3"])
                    cx.op("scalar", lambda e: e.activation(out=i1T[:, tsl], in_=psT3[:, 0, :], func=AF.Copy), reads=["psT3"], writes=[("rt", i)])
                    cx.op("scalar", lambda e: e.activation(out=i2T[:, tsl], in_=psT3[:, 1, :], func=AF.Identity, scale=-1.0), reads=["psT3"], writes=[("rt", i)])
                    cx.op("scalar", lambda e: e.activation(out=gT[:, tsl], in_=psT3[:, 2, :], func=AF.Copy), reads=["psT3"], writes=[("rt", i)])
            for kc in range(8):
                cx.dma("sync", "h2Td", h2T_d[kc * 128:(kc + 1) * 128, :], h2T[:, kc, :])
            for nm, t in [("d_i1T", i1T), ("d_i2T", i2T), ("d_gT", gT)]:
                if nm in debug:
                    cx.dma("sync", "dbg", dbg_out[nm][:, :], t[:])
            cx.barrier()

        with contextlib.ExitStack() as st:
            def T(name, shape, dt):
                return st.enter_context(nc.sbuf_tensor("p_" + name, list(shape), dt))
            TG = 256
            iota128f = T("iota128f", [128, 128], F32)
            iota128 = T("iota128", [128, 128], BF16)
            niota128 = T("niota128", [128, 128], BF16)
            Btmp = [T("Btmp%d" % i, [128, 128], BF16) for i in range(4)]
            fnw = T("fnw", [128, D], F32)
            gwb = [T("gw", [128, 128, TG], BF16), hT[:].rearrange("p c (a b) -> p (c a) b", b=TG)]
            h2g = [T("h2g%d" % i, [128, 8, TG], BF16) for i in range(2)]
            Aoh = [T("Aoh%d" % i, [128, 128], BF16) for i in range(4)]
            Boh = [T("Boh%d" % i, [128, 128], BF16) for i in range(4)]
            ut = [T("ut%d" % i, [128, 2, 8, 128], BF16) for i in range(2)]
            vt = [T("vt%d" % i, [128, 2, D], BF16) for i in range(2)]
            gel = [T("gel%d" % i, [128, TG], F32) for i in range(2)]
            Pb = [T("Pb%d" % i, [128, TG], BF16) for i in range(2)]
            x1s = [T("x1s%d" % i, [128, D], F32) for i in range(1)] * 2
            xo = [T("xo%d" % i, [128, D], F32) for i in range(1)] * 2
            ssq = T("ssq3", [128, 2], F32)
            psW = [st.enter_context(nc.psum_tensor("psW%d" % i, [128, 4, 128], F32)) for i in range(2)]
            psA = [st.enter_context(nc.psum_tensor("psA_%d" % i, [128, 512], F32)) for i in range(2)]
            psO = [st.enter_context(nc.psum_tensor("psO_%d" % i, [128, 1024], F32)) for i in range(2)]
            cx.dma("sync", "p0", iota128f[:], iota128_in[:, :], writes=["iota128f"])
            cx.op("vector", lambda e: e.tensor_copy(out=iota128[:], in_=iota128f[:]), reads=["iota128f"], writes=["iota128"])
            cx.op("vector", lambda e: e.tensor_scalar(out=niota128[:], in0=iota128f[:], scalar1=-1.0, scalar2=None, op0=ALU.mult),
                  reads=["iota128f"], writes=["niota128"])
            cx.dma("sync", "p1", fnw[:], fnw_in.partition_broadcast(128), writes=["fnw"])
            NG = S // TG
            nWc = [0]

            def gw_onehots(grp, q4, toks):
                t0 = grp * TG
                for tq in toks:
                    col = t0 + q4 * 4 + tq
                    r = tq
                    cx.op("vector", lambda e, r=r, col=col: e.tensor_scalar(
                        out=Aoh[r][:], in0=iota128[:], scalar1=i1T[:, col:col + 1], scalar2=gT[:, col:col + 1],
                        op0=ALU.is_equal, op1=ALU.mult), reads=["iota128"], writes=["Aoh%d" % r])
                    if tq % 2 == 1:
                        cx.op("vector", lambda e, r=r, col=col: e.tensor_scalar(
                            out=Boh[r][:], in0=niota128[:], scalar1=i2T[:, col:col + 1], scalar2=None,
                            op0=ALU.is_equal), reads=["niota128"], writes=["Boh%d" % r])
                    else:
                        cx.op("scalar", lambda e, r=r, col=col: e.activation(out=Btmp[r][:], in_=iota128[:], func=AF.Abs,
                                                                              bias=i2T[:, col:col + 1], scale=1.0),
                              reads=["iota128"], writes=["Btmp%d" % r])
                        cx.op("scalar", lambda e, r=r: e.activation(out=Boh[r][:], in_=Btmp[r][:], func=AF.Relu, bias=1.0, scale=-1.0),
                              reads=["Btmp%d" % r], writes=["Boh%d" % r])

            def gw_mm(grp, q4):
                b = q4 % 2
                for tq in range(4):
                    cx.op("tensor", lambda e, b=b, tq=tq: e.matmul(psW[b][:, tq, :], lhsT=Aoh[tq][:], rhs=Boh[tq][:], start=True, stop=True),
                          reads=["Aoh%d" % tq, "Boh%d" % tq], writes=["psW%d" % b])

            def gw_evac(grp, q4):
                b = q4 % 2
                g_ = gwb[grp % 2]
                cx.op("scalar", lambda e: e.activation(out=g_[:, :, q4 * 4:(q4 + 1) * 4],
                                                       in_=psW[b][:].rearrange("p t j -> p j t"), func=AF.Copy),
                      reads=["psW%d" % b], writes=["gw%d" % (grp % 2)])

            def emit_gw(grp, q4):
                gw_onehots(grp, q4, (0, 1, 2, 3))
                gw_mm(grp, q4)
                gw_evac(grp, q4)

            def load_h2(grp):
                cx.dma("sync", "h2g%d" % (grp % 2), h2g[grp % 2][:], h2T_d[:, grp * TG:(grp + 1) * TG].rearrange("(c p) t -> p c t", p=128),
                       writes=["h2g%d" % (grp % 2)])

            def load_w(gb):
                jb = (gb % 64) * 2
                pb = gb % 2
                rs = slice(jb * 128, (jb + 2) * 128)
                bs = slice((gb % 64) * 128, (gb % 64 + 1) * 128)
                cx.dma("sync", "ut%d" % pb, ut[pb][:].rearrange("p t a b -> p (t a b)"), u_bf[bs, :],
                       reads=[("u_bf", jb), ("u_bf", jb + 1)], writes=["ut%d" % pb])
                cx.dma("sync", "vt%d" % pb, vt[pb][:].rearrange("p t n -> p (t n)"), v_bf[bs, :],
                       reads=[("v_bf", jb), ("v_bf", jb + 1)], writes=["vt%d" % pb])

            load_h2(0)
            load_w(0)
            load_w(1)
            for q4 in range(TG // 4):
                emit_gw(0, q4)
            for grp in range(NG):
                t0 = grp * TG
                gw = gwb[grp % 2]
                kgw = "gw%d" % (grp % 2)
                hg = h2g[grp % 2]
                khg = "h2g%d" % (grp % 2)
                if grp + 1 < NG:
                    load_h2(grp + 1)

                def emit_u(j):
                    p3 = (j // 2) % 2
                    ja = j % 2
                    b = j % 2
                    for kc in range(8):
                        cx.op("tensor", lambda e, kc=kc: e.matmul(psA[b][:, 0:TG], lhsT=ut[p3][:, ja, kc, :], rhs=hg[:, kc, :],
                                                                  start=(kc == 0), stop=(kc == 7)),
                              reads=["ut%d" % p3, khg], writes=["psA_%d" % b])

                emit_u(0)
                for j in range(128):
                    p3 = (j // 2) % 2
                    ja = j % 2
                    b = j % 2
                    if j + 1 < 128:
                        emit_u(j + 1)
                    cx.op("scalar", lambda e, b=b: e.activation(out=gel[b][:], in_=psA[b][:, 0:TG], func=GELU),
                          reads=["psA_%d" % b], writes=["gel%d" % b])
                    cx.op("vector", lambda e, b=b, j=j: e.tensor_tensor(out=Pb[b][:], in0=gel[b][:], in1=gw[:, j, :], op=ALU.mult),
                          reads=["gel%d" % b, kgw], writes=["Pb%d" % b])
                    for sub in range(2):
                        for half in range(2):
                            cx.op("tensor", lambda e, b=b, sub=sub, half=half, p3=p3, j=j, ja=ja: e.matmul(
                                psO[sub][:, half * 512:(half + 1) * 512], lhsT=Pb[b][:, sub * 128:(sub + 1) * 128],
                                rhs=vt[p3][:, ja, half * 512:(half + 1) * 512], start=(j == 0), stop=(j == 127)),
                                reads=["Pb%d" % b, "vt%d" % p3], writes=["psO_%d" % sub])
                    if j % 2 == 1:
                        gb = grp * 64 + j // 2 + 2
                        if gb < NG * 64:
                            load_w(gb)
                    if grp + 1 < NG:
                        q4 = j // 2
                        if j % 2 == 0:
                            gw_onehots(grp + 1, q4, (0, 1))
                        else:
                            gw_onehots(grp + 1, q4, (2, 3))
                            gw_mm(grp + 1, q4)
                            if q4 > 0:
                                gw_evac(grp + 1, q4 - 1)
                if grp + 1 < NG:
                    gw_evac(grp + 1, 63)
                for sub in range(2):
                    i = grp * 2 + sub
                    tsl = slice(i * 128, (i + 1) * 128)
                    cx.dma("sync", "x1s0", x1s[sub][:], x1_d[tsl, :], reads=[("x1_d", i)], writes=["x1s0"])
                    X = xo[sub]; kx = "xo0"
                    cx.op("vector", lambda e, X=X, sub=sub: e.tensor_tensor(out=X[:], in0=psO[sub][:], in1=g2bc[:], op=ALU.mult),
                          reads=["psO_%d" % sub], writes=[kx])
                    cx.op("vector", lambda e, X=X, sub=sub: e.tensor_tensor(out=X[:], in0=X[:], in1=x1s[sub][:], op=ALU.add),
                          reads=[kx, "x1s0"], writes=[kx])
                    cx.op("scalar", lambda e, X=X, sub=sub: e.activation(out=x1s[sub][:], in_=X[:], func=AF.Square, accum_out=ssq[:, sub:sub + 1]),
                          reads=[kx], writes=["x1s0", ("ssq3", sub)])
                    cx.op("vector", lambda e, sub=sub: e.tensor_scalar(out=ssq[:, sub:sub + 1], in0=ssq[:, sub:sub + 1], scalar1=1.0 / D, scalar2=EPS,
                                                                       op0=ALU.mult, op1=ALU.add), reads=[("ssq3", sub)], writes=[("ssq3", sub)])
                    cx.op("scalar", lambda e, sub=sub: e.activation(out=ssq[:, sub:sub + 1], in_=ssq[:, sub:sub + 1], func=AF.Sqrt),
                          reads=[("ssq3", sub)], writes=[("ssq3", sub)])
                    cx.op("vector", lambda e, sub=sub: e.reciprocal(out=ssq[:, sub:sub + 1], in_=ssq[:, sub:sub + 1]),
                          reads=[("ssq3", sub)], writes=[("ssq3", sub)])
                    cx.op("vector", lambda e, X=X, sub=sub: e.scalar_tensor_tensor(out=X[:], in0=X[:], scalar=ssq[:, sub:sub + 1], in1=fnw[:],
                                                                                  op0=ALU.mult, op1=ALU.mult),
                          reads=[kx, ("ssq3", sub), "fnw"], writes=[kx])
                    cx.dma("sync", kx, out[tsl, :], X[:], reads=[kx], writes=[("out", i)])
            cx.barrier()

        cx.barrier()
    print("instructions:", cx.ninst)
    return nc


def make_in_maps(inputs, ncores=8):
    f = np.float32
    C, Sg = rope_tables()
    ident = np.eye(128, dtype=f)
    qperm = np.concatenate([np.arange(h * 64, (h + 1) * 64) for h in [0, 4, 1, 5, 2, 6, 3, 7]])
    cols = np.concatenate([qperm, np.arange(512, 768), np.arange(3328, 3360), np.arange(768, 1792),
                           np.arange(1792, 3328), np.arange(3360, 5408)])
    w_in_p = np.ascontiguousarray(inputs["w_in"][0][:, cols])
    qkgain = np.concatenate([np.tile(inputs["q_gain"][0], 8), np.tile(inputs["k_gain"][0], 2)])[None, :].astype(f)
    convwT = np.ascontiguousarray(inputs["conv_w"][0].reshape(5, 12, 128).transpose(2, 1, 0).reshape(128, 60))
    convbT = np.ascontiguousarray(inputs["conv_b"][0].reshape(12, 128).T)
    wau = np.ascontiguousarray(inputs["w_attn_up"][0][qperm, :])
    wq_h = np.ascontiguousarray(inputs["peer_w_query"][0])
    keysT_h = np.ascontiguousarray(np.stack([inputs["peer_keys1"][0], inputs["peer_keys2"][0]], axis=1)
                                   .transpose(3, 0, 1, 2).reshape(128, 2048))
    iota128 = np.tile(np.arange(128, dtype=f)[None, :], (128, 1))
    iota16 = np.tile(np.arange(16, dtype=f)[None, :], (128, 1))
    fnw_h = inputs["final_norm_w"][None, :].astype(f)
    U_h = np.ascontiguousarray(inputs["peer_u"][0].reshape(128, 128, 8, 128).transpose(1, 3, 2, 0)).reshape(128 * 128, D)
    V_h = np.ascontiguousarray(inputs["peer_v"][0].reshape(128, 128, D).transpose(1, 0, 2)).reshape(128 * 128, D)
    ii = np.arange(128)
    trif = (ii[:, None] <= ii[None, :]).astype(f)
    maskf = np.where(ii[None, :] >= ii[:, None], 0.0, -30000.0).astype(f)
    ssdp = np.concatenate([inputs["dt_bias_f"][0], inputs["dt_bias_b"][0], inputs["a_log_f"][0], inputs["a_log_b"][0],
                           inputs["d_skip"][0]])[None, :].astype(f)
    maps = []
    for b in range(ncores):
        m = {
            "x": np.ascontiguousarray(inputs["x"][b]),
            "c_col": np.ascontiguousarray(inputs["c"][b].reshape(8, 128).T),
            "ada_w": np.ascontiguousarray(inputs["ada_w"][0]),
            "ada_bT": np.ascontiguousarray(inputs["ada_b"][0].reshape(48, 128).T),
            "n1T": np.ascontiguousarray(inputs["norm1_w"][0].reshape(8, 128).T),
            "n2T": np.ascontiguousarray(inputs["norm2_w"][0].reshape(8, 128).T),
            "w_in": w_in_p,
            "trif": trif, "trib": np.ascontiguousarray(trif.T), "maskf": maskf, "maskb": np.ascontiguousarray(maskf.T),
            "ssdp": ssdp, "ssdnw": inputs["ssd_norm_w"][0][None, :].astype(f),
            "wau": wau, "wsu": np.ascontiguousarray(inputs["w_ssd_up"][0]), "wout": np.ascontiguousarray(inputs["w_out"][0]),
            "wq": wq_h, "keysT": keysT_h, "iota128": iota128, "iota16": iota16, "fnw": fnw_h, "U_h": U_h, "V_h": V_h,
            "ropeC": C, "ropeS": Sg, "qkgain": qkgain, "convwT": convwT, "convbT": convbT,
            "ident": ident,
        }
        maps.append(m)
    return maps


def kernel(**inputs):
    inputs = {k: np.asarray(v) for k, v in inputs.items()}
    nc = build()
    maps = make_in_maps(inputs)
    res = run_bass_kernel_spmd(nc, maps, core_ids=list(range(8)))
    return np.stack([r["out"] for r in res.results], axis=0).astype(np.float32)
```

```python
import contextlib
import numpy as np
import concourse.bass as bass
import concourse.mybir as mybir
from concourse.bass_utils import run_bass_kernel_spmd

F32 = mybir.dt.float32
BF16 = mybir.dt.bfloat16
U32 = mybir.dt.uint32
I32 = mybir.dt.int32
AF = mybir.ActivationFunctionType
ALU = mybir.AluOpType
AX = mybir.AxisListType

S = 4096
D = 1024
NT = S // 128
EPS = 1e-6
IN_W = 5408
STRICT = True


class Sem:
    def __init__(self, h):
        self.h = h
        self.cnt = 0


class Eng:
    def __init__(self, name, h, sem):
        self.name = name
        self.h = h
        self.sem = sem
        self.seen = {}


class Buf:
    __slots__ = ("w", "rs")

    def __init__(self):
        self.w = None
        self.rs = []


class Ctx:
    def __init__(self, nc, es):
        self.nc = nc
        self.es = es
        self.engs = {}
        for name in ["tensor", "vector", "scalar", "gpsimd", "sync"]:
            sem = Sem(es.enter_context(nc.semaphore("e_" + name)))
            self.engs[name] = Eng(name, getattr(nc, name), sem)
        self.bufs = {}
        self.dsems = {}
        self.ninst = 0

    def buf(self, key):
        b = self.bufs.get(key)
        if b is None:
            b = Buf()
            self.bufs[key] = b
        return b

    def dsem(self, key):
        s = self.dsems.get(key)
        if s is None:
            s = Sem(self.es.enter_context(self.nc.semaphore("d%d" % len(self.dsems))))
            self.dsems[key] = s
        return s

    def _waits(self, E, reads, writes, skip_self=False):
        need = {}

        def add(st):
            sem, val = st
            if need.get(sem, 0) < val:
                need[sem] = val

        for k in reads:
            b = self.bufs.get(k)
            if b is not None and b.w is not None:
                add(b.w)
        for k in writes:
            b = self.buf(k)
            if b.w is not None:
                add(b.w)
            for r in b.rs:
                add(r)
        for sem, val in need.items():
            if sem is E.sem and (skip_self or not STRICT):
                continue
            if E.seen.get(sem, 0) >= val:
                continue
            E.h.wait_ge(sem.h, val)
            E.seen[sem] = val
            self.ninst += 1

    def _stamp(self, stamp, reads, writes):
        for k in reads:
            self.buf(k).rs.append(stamp)
        for k in writes:
            b = self.buf(k)
            b.w = stamp
            b.rs = []

    def op(self, eng, fn, reads=(), writes=()):
        E = self.engs[eng]
        self._waits(E, reads, writes, skip_self=(eng == "tensor"))
        inst = fn(E.h)
        E.sem.cnt += 1
        inst.then_inc(E.sem.h, 1)
        self.ninst += 1
        self._stamp((E.sem, E.sem.cnt), reads, writes)
        return inst

    def dma(self, queue, semkey, out, in_, reads=(), writes=(), **kw):
        E = self.engs[queue]
        sem = self.dsem(semkey)
        self._waits(E, reads, writes)
        if E.seen.get(sem, 0) < sem.cnt:
            E.h.wait_ge(sem.h, sem.cnt)
            E.seen[sem] = sem.cnt
        inst = E.h.dma_start(out=out, in_=in_, **kw)
        sem.cnt += 16
        inst.then_inc(sem.h, 16)
        self.ninst += 1
        self._stamp((sem, sem.cnt), reads, writes)
        return inst

    def barrier(self):
        sems = [e.sem for e in self.engs.values()] + list(self.dsems.values())
        for E in self.engs.values():
            for s in sems:
                if s is E.sem and not STRICT:
                    continue
                if s.cnt > 0 and E.seen.get(s, 0) < s.cnt:
                    E.h.wait_ge(s.h, s.cnt)
                    E.seen[s] = s.cnt
        self.bufs = {k: v for k, v in self.bufs.items() if isinstance(k, tuple) and k[0] in KEEP}


KEEP = ("u_bf", "v_bf", "x1_d", "ssdtm_d")


def rope_tables():
    half = 32
    inv = (10000.0 ** (-np.arange(0, half, 2, dtype=np.float32) / half)).astype(np.float32)
    t = np.arange(S)
    row = (t // 64).astype(np.float32)
    col = (t % 64).astype(np.float32)
    ar = row[:, None] * inv
    ac = col[:, None] * inv
    C = np.concatenate([np.cos(ar), np.cos(ar), np.cos(ac), np.cos(ac)], axis=1).astype(np.float32)
    Sg = np.concatenate([-np.sin(ar), np.sin(ar), -np.sin(ac), np.sin(ac)], axis=1).astype(np.float32)
    return C, Sg


def build(debug=None):
    debug = debug or ()
    nc = bass.Bass("TRN2", target_bir_lowering=False)
    es = contextlib.ExitStack()

    def din(name, shape, dt=F32):
        return nc.dram_tensor(name, list(shape), dt, kind="ExternalInput").ap()

    def dscratch(name, shape, dt):
        kind = "ExternalOutput" if name in debug else "Internal"
        return nc.dram_tensor(name, list(shape), dt, kind=kind).ap()

    x = din("x", [S, D])
    c_col = din("c_col", [128, 8])
    ada_w = din("ada_w", [D, 6 * D])
    ada_bT = din("ada_bT", [128, 48])
    n1T = din("n1T", [128, 8])
    n2T = din("n2T", [128, 8])
    w_in = din("w_in", [D, IN_W])
    ident_in = din("ident", [128, 128])
    ropeC_in = din("ropeC", [S, 64])
    ropeS_in = din("ropeS", [S, 64])
    qkgain_in = din("qkgain", [1, 640])
    convwT_in = din("convwT", [128, 12 * 5])
    convbT_in = din("convbT", [128, 12])
    trif_in = din("trif", [128, 128])
    trib_in = din("trib", [128, 128])
    maskf_in = din("maskf", [128, 128])
    maskb_in = din("maskb", [128, 128])
    ssdp_in = din("ssdp", [1, 80])
    ssdnw_in = din("ssdnw", [1, 1024])
    wq_in = din("wq", [D, 2048])
    keysT_in = din("keysT", [128, 2048])
    iota128_in = din("iota128", [128, 128])
    iota16_in = din("iota16", [128, 16])
    fnw_in = din("fnw", [1, D])
    U_in = din("U_h", [128 * 128, D])
    V_in = din("V_h", [128 * 128, D])
    h2T_d = dscratch("h2T_d", [D, S], BF16)
    u_bf = dscratch("u_bf", [64 * 128, 2 * D], BF16)
    v_bf = dscratch("v_bf", [64 * 128, 2 * D], BF16)
    wau_in = din("wau", [512, D])
    wsu_in = din("wsu", [D, D])
    wout_in = din("wout", [D, D])
    x1_d = dscratch("x1_d", [S, D], F32)
    attT_d = dscratch("attT_d", [512, S], BF16)
    xstm_d = dscratch("xstm_d", [S, 1024], BF16)
    yb_d = dscratch("yb_d", [S, 1024], F32)
    ssdtm_d = dscratch("ssdtm_d", [S, 1024], BF16)
    zs_d = dscratch("zs_d", [S, 1024], BF16)
    xbcT_d = dscratch("xbcT_d", [1536, S], BF16)
    gatesT_d = dscratch("gatesT_d", [2048, S], BF16)
    out = nc.dram_tensor("out", [S, D], F32, kind="ExternalOutput").ap()
    dbg_out = {}
    for name, shape, dt in [("d_modT", [128, 48], F32), ("d_hT", [128, 8 * S], BF16),
                            ("d_QT", [128, 4 * S], BF16), ("d_KT", [128, S], BF16), ("d_V", [128, NT * 130], BF16),
                            ("d_dtraw", [128, NT * 32], F32),
                            ("d_attm", [128, NT * 512], BF16),
                            ("d_ssdT", [128, 8 * S], BF16), ("d_h2T", [128, 8 * S], BF16), ("d_i1T", [128, S], BF16),
                            ("d_i2T", [128, S], BF16), ("d_gT", [128, S], BF16)]:
        if name in debug:
            dbg_out[name] = nc.dram_tensor(name, shape, dt, kind="ExternalOutput").ap()

    with es:
        cx = Ctx(nc, es)

        def sb(name, shape, dt):
            return es.enter_context(nc.sbuf_tensor("s_" + name, list(shape), dt))

        ident = sb("ident", [128, 128], F32)
        identb = sb("identb", [128, 128], BF16)
        ones_f = sb("ones_f", [128, 128], F32)
        cact = sb("cact", [128, 8], F32)
        modT = sb("modT", [128, 48], F32)
        abT = sb("abT", [128, 48], F32)
        n1 = sb("n1", [128, 8], F32)
        n2 = sb("n2", [128, 8], F32)
        gam1 = sb("gam1", [128, 8], F32)
        gam2 = sb("gam2", [128, 8], F32)
        hT = sb("hT", [128, 8, S], BF16)
        dtraw = sb("dtraw", [128, NT, 32], F32)
        mid = contextlib.ExitStack()

        def sbm(name, shape, dt):
            return mid.enter_context(nc.sbuf_tensor("m_" + name, list(shape), dt))
        QT = sbm("QT", [128, 4, S], BF16)
        KT = sbm("KT", [128, S], BF16)
        Vsb = sbm("Vsb", [128, NT, 2, 65], BF16)
        ropeC = sbm("ropeC", [128, NT, 64], F32)
        ropeS = sbm("ropeS", [128, NT, 64], F32)
        qkgain = sbm("qkgain", [128, 640], F32)
        convwT = sbm("convwT", [128, 12, 5], F32)
        convbT = sbm("convbT", [128, 12], F32)

        cx.dma("sync", "c0", ident[:], ident_in[:, :], writes=["ident"])
        cx.dma("sync", "c1", cact[:], c_col[:, :], writes=["cact"])
        cx.dma("sync", "c2", abT[:], ada_bT[:, :], writes=["abT"])
        cx.dma("sync", "c3", n1[:], n1T[:, :], writes=["n1"])
        cx.dma("sync", "c4", n2[:], n2T[:, :], writes=["n2"])
        cx.dma("sync", "c5", ropeC[:], ropeC_in.rearrange("(i p) j -> p i j", p=128), writes=["ropeC"])
        cx.dma("sync", "c6", ropeS[:], ropeS_in.rearrange("(i p) j -> p i j", p=128), writes=["ropeS"])
        cx.dma("sync", "c7", qkgain[:], qkgain_in.partition_broadcast(128), writes=["qkgain"])
        cx.dma("sync", "c8", convwT[:].rearrange("p a b -> p (a b)"), convwT_in[:, :], writes=["convwT"])
        cx.dma("sync", "c9", convbT[:], convbT_in[:, :], writes=["convbT"])
        cx.op("vector", lambda e: e.tensor_scalar(out=qkgain[:, 0:512], in0=qkgain[:, 0:512], scalar1=0.125, scalar2=None,
                                                  op0=ALU.mult), reads=["qkgain"], writes=["qkgain"])
        cx.op("vector", lambda e: e.memset(Vsb[:].rearrange("p a b c -> p (a b c)"), 1.0), writes=["Vsb"])
        cx.op("vector", lambda e: e.tensor_copy(out=identb[:], in_=ident[:]), reads=["ident"], writes=["identb"])
        cx.op("vector", lambda e: e.memset(ones_f[:], 1.0), writes=["ones_f"])
        cx.op("scalar", lambda e: e.activation(out=cact[:], in_=cact[:], func=AF.Silu), reads=["cact"], writes=["cact"])

        with contextlib.ExitStack() as st:
            aw = [st.enter_context(nc.sbuf_tensor("aw%d" % i, [128, 8, 512], F32)) for i in range(2)]
            psm = st.enter_context(nc.psum_tensor("psm", [128, 48], F32))
            awv = ada_w.rearrange("(kc p) n -> p kc n", p=128)
            for blk in range(12):
                t = aw[blk % 2]
                key = "aw%d" % (blk % 2)
                cx.dma("sync", key, t[:], awv[:, :, blk * 512:(blk + 1) * 512], writes=[key])
                for jj in range(4):
                    j = blk * 4 + jj
                    for kc in range(8):
                        cx.op("tensor", lambda e, t=t, jj=jj, kc=kc, j=j: e.matmul(
                            psm[:, j:j + 1], lhsT=t[:, kc, jj * 128:(jj + 1) * 128], rhs=cact[:, kc:kc + 1],
                            start=(kc == 0), stop=(kc == 7)), reads=[key, "cact"], writes=["psm"])
            cx.op("vector", lambda e: e.tensor_tensor(out=modT[:], in0=psm[:], in1=abT[:], op=ALU.add),
                  reads=["psm", "abT"], writes=["modT"])
            cx.op("vector", lambda e: e.scalar_tensor_tensor(out=gam1[:], in0=modT[:, 8:16], scalar=1.0, in1=n1[:],
                                                             op0=ALU.add, op1=ALU.mult),
                  reads=["modT", "n1"], writes=["gam1"])
            cx.op("vector", lambda e: e.scalar_tensor_tensor(out=gam2[:], in0=modT[:, 32:40], scalar=1.0, in1=n2[:],
                                                             op0=ALU.add, op1=ALU.mult),
                  reads=["modT", "n2"], writes=["gam2"])
            if "d_modT" in debug:
                cx.dma("sync", "dbg", dbg_out["d_modT"][:, :], modT[:], reads=["modT"])
            s0_scope = st.pop_all()

        def norm_mod_transpose(src, gam, shT, st, pfx, rkey=None, gkeys=()):
            xt = [st.enter_context(nc.sbuf_tensor(pfx + "xt%d" % i, [128, D], F32)) for i in range(2)]
            xn = [st.enter_context(nc.sbuf_tensor(pfx + "xn%d" % i, [128, D], BF16)) for i in range(2)]
            junk = st.enter_context(nc.sbuf_tensor(pfx + "junk", [128, D], BF16))
            ssq = st.enter_context(nc.sbuf_tensor(pfx + "ssq", [128, NT], F32))
            rstd = st.enter_context(nc.sbuf_tensor(pfx + "rstd", [128, NT], F32))
            pst = [st.enter_context(nc.psum_tensor(pfx + "pst%d" % i, [128, 8, 128], BF16)) for i in range(2)]
            for i in range(NT):
                p = i % 2
                kx, kn, kp = pfx + "xt%d" % p, pfx + "xn%d" % p, pfx + "pst%d" % p
                cx.dma("sync", kx, xt[p][:], src[i * 128:(i + 1) * 128, :], reads=([(rkey, i)] if rkey else []), writes=[kx])
                cx.op("scalar", lambda e, p=p, i=i: e.activation(out=junk[:], in_=xt[p][:], func=AF.Square,
                                                                 accum_out=ssq[:, i:i + 1]),
                      reads=[kx], writes=[pfx + "junk", (pfx + "ssq", i)])
                cx.op("vector", lambda e, i=i: e.tensor_scalar(out=rstd[:, i:i + 1], in0=ssq[:, i:i + 1],
                                                               scalar1=1.0 / D, scalar2=EPS, op0=ALU.mult, op1=ALU.add),
                      reads=[(pfx + "ssq", i)], writes=[(pfx + "rstd", i)])
                cx.op("scalar", lambda e, i=i: e.activation(out=rstd[:, i:i + 1], in_=rstd[:, i:i + 1], func=AF.Sqrt),
                      reads=[(pfx + "rstd", i)], writes=[(pfx + "rstd", i)])
                cx.op("vector", lambda e, i=i: e.reciprocal(out=rstd[:, i:i + 1], in_=rstd[:, i:i + 1]),
                      reads=[(pfx + "rstd", i)], writes=[(pfx + "rstd", i)])
                cx.op("vector", lambda e, p=p, i=i: e.tensor_scalar(out=xn[p][:], in0=xt[p][:], scalar1=rstd[:, i:i + 1],
                                                                    scalar2=None, op0=ALU.mult),
                      reads=[kx, (pfx + "rstd", i)], writes=[kn])
                for c in range(8):
                    cx.op("tensor", lambda e, p=p, c=c: e.transpose(out=pst[p][:, c, :], in_=xn[p][:, c * 128:(c + 1) * 128],
                                                                    identity=identb[:]),
                          reads=[kn, "identb"], writes=[kp])
                for c in range(8):
                    cx.op("scalar", lambda e, p=p, c=c, i=i: e.activation(
                        out=hT[:, c, i * 128:(i + 1) * 128], in_=pst[p][:, c, :], func=AF.Identity,
                        scale=gam[:, c:c + 1], bias=shT[:, c:c + 1]),
                        reads=[kp] + list(gkeys), writes=[("hT", i)])

        with contextlib.ExitStack() as st:
            norm_mod_transpose(x, gam1, modT[:, 0:8], st, "a", gkeys=["gam1", "modT"])
            if "d_hT" in debug:
                cx.dma("sync", "dbg", dbg_out["d_hT"][:, :], hT[:].rearrange("p c s -> p (c s)"),
                       reads=[("hT", i) for i in range(NT)])
            cx.barrier()
        s0_scope.close()


        wv = w_in.rearrange("(kc p) n -> p kc n", p=128)
        with contextlib.ExitStack() as st:
            wA = st.enter_context(nc.sbuf_tensor("wA", [128, 8, 800], BF16))
            wZ = st.enter_context(nc.sbuf_tensor("wZ", [128, 8, 1024], BF16))
            psA = [st.enter_context(nc.psum_tensor("psA%d" % i, [128, 1024], F32)) for i in range(2)]
            psZ = st.enter_context(nc.psum_tensor("psZ", [128, 1024], F32))
            pT = st.enter_context(nc.psum_tensor("pT", [128, 5, 128], BF16))
            qkv = [st.enter_context(nc.sbuf_tensor("qkv%d" % i, [128, 800], F32)) for i in range(2)]
            sq = st.enter_context(nc.sbuf_tensor("sq", [128, 640], F32))
            qn = st.enter_context(nc.sbuf_tensor("qn", [128, 640], F32))
            qa = st.enter_context(nc.sbuf_tensor("qa", [128, 640], F32))
            qb = st.enter_context(nc.sbuf_tensor("qb", [128, 640], F32))
            qr = st.enter_context(nc.sbuf_tensor("qr", [128, 640], BF16))
            ss = st.enter_context(nc.sbuf_tensor("ss10", [128, 10], F32))
            zst = [st.enter_context(nc.sbuf_tensor("zst%d" % i, [128, 1024], BF16)) for i in range(2)]
            cx.dma("gpsimd", "wA", wA[:], wv[:, :, 0:800], writes=["wA"])
            cx.dma("gpsimd", "wZ", wZ[:], wv[:, :, 800:1824], writes=["wZ"])
            for i in range(NT):
                p = i % 2
                tsl = slice(i * 128, (i + 1) * 128)
                kA, kq = "psA%d" % p, "qkv%d" % p
                for (c0, c1) in [(0, 512), (512, 800)]:
                    for kc in range(8):
                        cx.op("tensor", lambda e, p=p, c0=c0, c1=c1, kc=kc: e.matmul(
                            psA[p][:, c0:c1], lhsT=hT[:, kc, tsl], rhs=wA[:, kc, c0:c1], start=(kc == 0), stop=(kc == 7)),
                            reads=[("hT", i), "wA"], writes=[kA])
                for (c0, c1) in [(0, 512), (512, 1024)]:
                    for kc in range(8):
                        cx.op("tensor", lambda e, c0=c0, c1=c1, kc=kc: e.matmul(
                            psZ[:, c0:c1], lhsT=hT[:, kc, tsl], rhs=wZ[:, kc, c0:c1], start=(kc == 0), stop=(kc == 7)),
                            reads=[("hT", i), "wZ"], writes=["psZ"])
                cx.op("scalar", lambda e, p=p: e.activation(out=qkv[p][:], in_=psA[p][:, 0:800], func=AF.Copy),
                      reads=[kA], writes=[kq])
                kz = "zst%d" % p
                cx.op("scalar", lambda e, p=p: e.activation(out=zst[p][:], in_=psZ[:], func=AF.Silu),
                      reads=["psZ"], writes=[kz])
                cx.dma("sync", kz, zs_d[tsl, :], zst[p][:], reads=[kz], writes=[("zs_d", i)])
                Q = qkv[p]
                cx.op("vector", lambda e, Q=Q: e.tensor_tensor(out=sq[:], in0=Q[:, 0:640], in1=Q[:, 0:640], op=ALU.mult),
                      reads=[kq], writes=["sq"])
                cx.op("vector", lambda e: e.tensor_reduce(out=ss[:], in_=sq[:].rearrange("p (h d) -> p h d", d=64),
                                                          axis=AX.X, op=ALU.add), reads=["sq"], writes=["ss10"])
                cx.op("vector", lambda e: e.tensor_scalar(out=ss[:], in0=ss[:], scalar1=1.0 / 64, scalar2=EPS,
                                                          op0=ALU.mult, op1=ALU.add), reads=["ss10"], writes=["ss10"])
                cx.op("scalar", lambda e: e.activation(out=ss[:], in_=ss[:], func=AF.Sqrt), reads=["ss10"], writes=["ss10"])
                cx.op("vector", lambda e: e.reciprocal(out=ss[:], in_=ss[:]), reads=["ss10"], writes=["ss10"])
                cx.op("vector", lambda e, Q=Q: e.tensor_tensor(
                    out=qn[:].rearrange("p (h d) -> p h d", d=64), in0=Q[:, 0:640].rearrange("p (h d) -> p h d", d=64),
                    in1=ss[:].unsqueeze(2).to_broadcast([128, 10, 64]), op=ALU.mult), reads=[kq, "ss10"], writes=["qn"])
                cx.op("vector", lambda e: e.tensor_tensor(out=qn[:], in0=qn[:], in1=qkgain[:], op=ALU.mult),
                      reads=["qn", "qkgain"], writes=["qn"])
                cx.op("vector", lambda e, i=i: e.tensor_tensor(
                    out=qa[:].rearrange("p (h d) -> p h d", d=64), in0=qn[:].rearrange("p (h d) -> p h d", d=64),
                    in1=ropeC[:, i, :].unsqueeze(1).to_broadcast([128, 10, 64]), op=ALU.mult),
                    reads=["qn", "ropeC"], writes=["qa"])
                for blk in range(2):
                    for hh in range(2):
                        o0 = blk * 32 + hh * 16
                        i0 = blk * 32 + (1 - hh) * 16
                        cx.op("vector", lambda e, i=i, o0=o0, i0=i0: e.tensor_tensor(
                            out=qb[:].rearrange("p (h d) -> p h d", d=64)[:, :, o0:o0 + 16],
                            in0=qn[:].rearrange("p (h d) -> p h d", d=64)[:, :, i0:i0 + 16],
                            in1=ropeS[:, i, o0:o0 + 16].unsqueeze(1).to_broadcast([128, 10, 16]), op=ALU.mult),
                            reads=["qn", "ropeS"], writes=["qb"])
                cx.op("vector", lambda e: e.tensor_tensor(out=qr[:], in0=qa[:], in1=qb[:], op=ALU.add),
                      reads=["qa", "qb"], writes=["qr"])
                for j in range(5):
                    cx.op("tensor", lambda e, j=j: e.transpose(out=pT[:, j, :], in_=qr[:, j * 128:(j + 1) * 128],
                                                               identity=identb[:]), reads=["qr", "identb"], writes=["pT"])
                cx.op("scalar", lambda e: e.activation(out=QT[:, :, tsl], in_=pT[:, 0:4, :], func=AF.Copy),
                      reads=["pT"], writes=[("QT", i)])
                cx.op("scalar", lambda e: e.activation(out=KT[:, tsl], in_=pT[:, 4, :], func=AF.Copy),
                      reads=["pT"], writes=[("KT", i)])
                cx.op("vector", lambda e, Q=Q, i=i: e.tensor_copy(
                    out=Vsb[:, i, :, 0:64], in_=Q[:, 640:768].rearrange("p (h d) -> p h d", d=64)),
                    reads=[kq, "Vsb"], writes=[("Vsb", i)])
                cx.op("vector", lambda e, Q=Q, i=i: e.tensor_copy(out=dtraw[:, i, :], in_=Q[:, 768:800]),
                      reads=[kq], writes=[("dtraw", i)])
            cx.barrier()
        with contextlib.ExitStack() as st:
            wF = [st.enter_context(nc.sbuf_tensor("wF%d" % i, [128, 8, 512], BF16)) for i in range(2)]
            psF = [st.enter_context(nc.psum_tensor("psF%d" % i, [128, 512], F32)) for i in range(4)]
            stage = st.enter_context(nc.sbuf_tensor("stage", [128, S + 4], F32))
            acc = st.enter_context(nc.sbuf_tensor("acc", [128, S], F32))
            ob = [st.enter_context(nc.sbuf_tensor("ob%d" % i, [128, S], BF16)) for i in range(2)]
            cx.op("vector", lambda e: e.memset(stage[:], 0.0), writes=["stage"])
            n = 0
            for j in range(28):
                isx = j < 12
                jj = j if isx else j - 12
                bb = j // 4
                W = wF[bb % 2]
                kW = "wF%d" % (bb % 2)
                if j % 4 == 0:
                    cx.dma("gpsimd", kW, W[:], wv[:, :, 1824 + bb * 512:1824 + (bb + 1) * 512], writes=[kW])
                jw = j % 4
                o = ob[j % 2]
                ko = "ob%d" % (j % 2)
                for g in range(8):
                    b = n % 4
                    n += 1
                    gs = slice(g * 512, (g + 1) * 512)
                    for kc in range(8):
                        cx.op("tensor", lambda e, b=b, kc=kc, W=W, jw=jw, gs=gs: e.matmul(
                            psF[b][:], lhsT=W[:, kc, jw * 128:(jw + 1) * 128], rhs=hT[:, kc, gs],
                            start=(kc == 0), stop=(kc == 7)),
                            reads=["hTall", kW], writes=["psF%d" % b])
                    if isx:
                        cx.op("scalar", lambda e, b=b, g=g: e.activation(out=stage[:, 2 + g * 512:2 + (g + 1) * 512],
                                                                         in_=psF[b][:], func=AF.Copy),
                              reads=["psF%d" % b, "acc0", "stage"], writes=[("stage", g)])
                    else:
                        cx.op("scalar", lambda e, b=b, gs=gs, o=o: e.activation(out=o[:, gs], in_=psF[b][:], func=AF.Sigmoid),
                              reads=["psF%d" % b], writes=[ko])
                if isx:
                    srd = [("stage", g) for g in range(8)] + ["stage"]
                    cx.op("vector", lambda e, jj=jj: e.tensor_scalar(
                        out=acc[:], in0=stage[:, 0:S], scalar1=convwT[:, jj, 0:1], scalar2=convbT[:, jj:jj + 1],
                        op0=ALU.mult, op1=ALU.add), reads=srd + ["convwT", "convbT"], writes=["acc"])
                    for k in range(1, 5):
                        cx.op("vector", lambda e, jj=jj, k=k: e.scalar_tensor_tensor(
                            out=acc[:], in0=stage[:, k:k + S], scalar=convwT[:, jj, k:k + 1], in1=acc[:],
                            op0=ALU.mult, op1=ALU.add), reads=srd + ["acc"], writes=["acc"] + (["acc0"] if k == 4 else []))
                    cx.op("scalar", lambda e, o=o: e.activation(out=o[:], in_=acc[:], func=AF.Silu),
                          reads=["acc"], writes=[ko])
                    cx.dma("sync", ko, xbcT_d[jj * 128:(jj + 1) * 128, :], o[:], reads=[ko], writes=[("xbcT_d", jj)])
                else:
                    cx.dma("sync", ko, gatesT_d[jj * 128:(jj + 1) * 128, :], o[:], reads=[ko], writes=[("gatesT_d", jj)])
            for nm, t in [("d_QT", QT), ("d_KT", KT), ("d_V", Vsb), ("d_dtraw", dtraw)]:
                if nm in debug:
                    flat = t[:] if len(t.shape) == 2 else (
                        t[:].rearrange("p a b -> p (a b)") if len(t.shape) == 3 else t[:].rearrange("p a b c -> p (a b c)"))
                    cx.dma("sync", "dbg", dbg_out[nm][:, :], flat)
            cx.barrier()


        with contextlib.ExitStack() as st:
            psS = [st.enter_context(nc.psum_tensor("psS%d" % i, [128, 512], F32)) for i in range(4)]
            psO = [st.enter_context(nc.psum_tensor("psO%d" % i, [128, 512], F32)) for i in range(2)]
            pTr = st.enter_context(nc.psum_tensor("pTr", [128, 4, 65], F32))
            PT = [st.enter_context(nc.sbuf_tensor("PT%d" % i, [128, 512], BF16)) for i in range(4)]
            oT = [st.enter_context(nc.sbuf_tensor("oT%d" % i, [128, 512], F32)) for i in range(2)]
            attm = st.enter_context(nc.sbuf_tensor("attm", [128, NT, 512], BF16))
            rden = st.enter_context(nc.sbuf_tensor("rden", [128, 4], F32))
            pTa = st.enter_context(nc.psum_tensor("pTa", [128, 4, 128], BF16))
            attT = hT
            cu = [st.enter_context(nc.sbuf_tensor("cu%d" % i, [128, 2, D], BF16)) for i in range(2)]
            cv = [st.enter_context(nc.sbuf_tensor("cv%d" % i, [128, 2, D], BF16)) for i in range(2)]
            for jj in range(64):
                p = jj % 2
                rs = slice(jj * 256, (jj + 1) * 256)
                cx.dma("gpsimd", "cu%d" % p, cu[p][:], U_in[rs, :].rearrange("(a p) n -> p a n", p=128), writes=["cu%d" % p])
                cx.dma("sync", "cuo%d" % p, u_bf[jj * 128:(jj + 1) * 128, :], cu[p][:].rearrange("p a n -> p (a n)"), reads=["cu%d" % p],
                       writes=[("u_bf", 2 * jj), ("u_bf", 2 * jj + 1)])
                cx.dma("gpsimd", "cv%d" % p, cv[p][:], V_in[rs, :].rearrange("(a p) n -> p a n", p=128), writes=["cv%d" % p])
                cx.dma("sync", "cvo%d" % p, v_bf[jj * 128:(jj + 1) * 128, :], cv[p][:].rearrange("p a n -> p (a n)"), reads=["cv%d" % p],
                       writes=[("v_bf", 2 * jj), ("v_bf", 2 * jj + 1)])
            steps = [(j, qg, kt) for j in range(4) for qg in range(8) for kt in range(NT)]

            def emit_qk(n):
                j, qg, kt = steps[n]
                for grp in range(2):
                    b = 2 * (n % 2) + grp
                    ps_ = slice(grp * 64, (grp + 1) * 64)
                    cx.op("tensor", lambda e, b=b, ps_=ps_: e.matmul(
                        psS[b][:], lhsT=KT[ps_, kt * 128:(kt + 1) * 128], rhs=QT[ps_, j, qg * 512:(qg + 1) * 512],
                        start=True, stop=True), reads=["QT", "KT"], writes=["psS%d" % b])

            emit_qk(0)
            for n, (j, qg, kt) in enumerate(steps):
                if n + 1 < len(steps):
                    emit_qk(n + 1)
                for grp in range(2):
                    b = 2 * (n % 2) + grp
                    cx.op("scalar", lambda e, b=b: e.activation(out=PT[b][:], in_=psS[b][:], func=AF.Exp),
                          reads=["psS%d" % b], writes=["PT%d" % b])
                for grp in range(2):
                    b = 2 * (n % 2) + grp
                    cx.op("tensor", lambda e, b=b, kt=kt, grp=grp: e.matmul(
                        psO[grp][0:65, :], lhsT=Vsb[:, kt, grp, :], rhs=PT[b][:],
                        start=(kt == 0), stop=(kt == NT - 1)), reads=["PT%d" % b, "Vsb"], writes=["psO%d" % grp])
                if kt == NT - 1:
                    for grp in range(2):
                        pos = 2 * j + grp
                        cx.op("scalar", lambda e, grp=grp: e.activation(out=oT[grp][0:65, :], in_=psO[grp][0:65, :], func=AF.Copy),
                              reads=["psO%d" % grp], writes=["oT%d" % grp])
                        for sub in range(4):
                            cx.op("tensor", lambda e, grp=grp, sub=sub: e.transpose(
                                out=pTr[:, sub, :], in_=oT[grp][0:65, sub * 128:(sub + 1) * 128], identity=ident[0:65, 0:65]),
                                reads=["oT%d" % grp, "ident"], writes=["pTr"])
                        cx.op("vector", lambda e: e.reciprocal(out=rden[:].unsqueeze(2), in_=pTr[:, :, 64:65]),
                              reads=["pTr"], writes=["rden"])
                        cx.op("vector", lambda e, qg=qg, pos=pos: e.tensor_tensor(
                            out=attm[:, qg * 4:(qg + 1) * 4, pos * 64:(pos + 1) * 64], in0=pTr[:, :, 0:64],
                            in1=rden[:].unsqueeze(2).to_broadcast([128, 4, 64]), op=ALU.mult),
                            reads=["pTr", "rden"], writes=[("attm", qg * 4 + k) for k in range(4)])
            if "d_attm" in debug:
                cx.dma("sync", "dbg", dbg_out["d_attm"][:, :], attm[:].rearrange("p a b -> p (a b)"),
                       reads=[("attm", ti) for ti in range(NT)])
            for i in range(NT):
                for c in range(4):
                    cx.op("tensor", lambda e, i=i, c=c: e.transpose(out=pTa[:, c, :], in_=attm[:, i, c * 128:(c + 1) * 128],
                                                                    identity=identb[:]), reads=[("attm", i)], writes=["pTa"])
                cx.op("scalar", lambda e, i=i: e.activation(out=attT[:, 0:4, i * 128:(i + 1) * 128], in_=pTa[:], func=AF.Copy),
                      reads=["pTa"], writes=[("attT", i)])
            for c in range(4):
                cx.dma("sync", "attTd", attT_d[c * 128:(c + 1) * 128, :], attT[:, c, :],
                       reads=[("attT", i) for i in range(NT)], writes=["attT_d"])
            cx.barrier()
        mid.close()


        ssdT = hT
        with contextlib.ExitStack() as st:
            def T(name, shape, dt):
                return st.enter_context(nc.sbuf_tensor("y_" + name, list(shape), dt))
            trif = T("trif", [128, 128], F32); trib = T("trib", [128, 128], F32)
            ssdp = T("ssdp", [128, 80], F32)
            ssdnw = T("ssdnw", [128, 1024], F32)
            dtv = T("dtv", [128, NT, 32], F32)
            Adt = T("Adt", [128, NT, 32], F32)
            BT = T("BT", [128, 2, S], BF16)
            CT = T("CT", [128, 2, S], BF16)
            Btm = T("Btm", [128, NT, 2, 128], BF16)
            cx.dma("sync", "k0", trif[:], trif_in[:, :], writes=["trif"])
            cx.dma("sync", "k1", trib[:], trib_in[:, :], writes=["trib"])
            cx.dma("sync", "k4", ssdp[:], ssdp_in.partition_broadcast(128), writes=["ssdp"])
            cx.dma("sync", "k5", ssdnw[:], ssdnw_in.partition_broadcast(128), writes=["ssdnw"])
            for g in range(2):
                cx.dma("sync", "k6", BT[:, g, :], xbcT_d[1024 + g * 128:1024 + (g + 1) * 128, :], writes=["BT"])
                cx.dma("sync", "k7", CT[:, g, :], xbcT_d[1280 + g * 128:1280 + (g + 1) * 128, :], writes=["CT"])
            cx.op("vector", lambda e: e.tensor_tensor(out=dtv[:], in0=dtraw[:], in1=ssdp[:, 0:32].unsqueeze(1).to_broadcast([128, NT, 32]),
                                                      op=ALU.add), reads=["ssdp"], writes=["dtv"])
            cx.op("scalar", lambda e: e.activation(out=dtv[:], in_=dtv[:], func=AF.Exp), reads=["dtv"], writes=["dtv"])
            cx.op("scalar", lambda e: e.activation(out=dtv[:], in_=dtv[:], func=AF.Ln, bias=1.0), reads=["dtv"], writes=["dtv"])
            cx.op("scalar", lambda e: e.activation(out=ssdp[:, 32:64], in_=ssdp[:, 32:64], func=AF.Exp), reads=["ssdp"], writes=["ssdp"])
            cx.op("vector", lambda e: e.scalar_tensor_tensor(out=Adt[:], in0=dtv[:], scalar=-1.0,
                                                             in1=ssdp[:, 32:64].unsqueeze(1).to_broadcast([128, NT, 32]),
                                                             op0=ALU.mult, op1=ALU.mult), reads=["dtv", "ssdp"], writes=["Adt"])
            with contextlib.ExitStack() as st2:
                pT4 = [st2.enter_context(nc.psum_tensor("pT4%d" % i, [128, 4, 128], BF16)) for i in range(2)]
                xch = [st2.enter_context(nc.sbuf_tensor("y_xch%d" % i, [128, S], BF16)) for i in range(2)]
                xst = [st2.enter_context(nc.sbuf_tensor("y_xst%d" % i, [128, 1024], BF16)) for i in range(2)]
                n = 0
                for g in range(2):
                    for i0 in range(0, NT, 4):
                        b = n % 2; n += 1
                        for k in range(4):
                            cx.op("tensor", lambda e, b=b, k=k, g=g, i0=i0: e.transpose(
                                out=pT4[b][:, k, :], in_=BT[:, g, (i0 + k) * 128:(i0 + k + 1) * 128], identity=identb[:]),
                                reads=["BT"], writes=["pT4%d" % b])
                        cx.op("scalar", lambda e, b=b, g=g, i0=i0: e.activation(out=Btm[:, i0:i0 + 4, g, :], in_=pT4[b][:], func=AF.Copy),
                              reads=["pT4%d" % b], writes=["Btm"])
                for i in range(NT):
                    cx.buf(("xstm", i))
                for c in range(8):
                    X = xch[c % 2]; kx = "xch%d" % (c % 2)
                    cx.dma("sync", kx, X[:], xbcT_d[c * 128:(c + 1) * 128, :], writes=[kx])
                    for i0 in range(0, NT, 4):
                        b = n % 2; n += 1
                        for k in range(4):
                            cx.op("tensor", lambda e, b=b, k=k, X=X, i0=i0: e.transpose(
                                out=pT4[b][:, k, :], in_=X[:, (i0 + k) * 128:(i0 + k + 1) * 128], identity=identb[:]),
                                reads=[kx], writes=["pT4%d" % b])
                        q = (i0 // 4) % 2
                        cx.op("scalar", lambda e, b=b, q=q: e.activation(out=xst[q][:, 0:512].rearrange("p (a b) -> p a b", a=4),
                                                                         in_=pT4[b][:], func=AF.Copy),
                              reads=["pT4%d" % b], writes=["xst%d" % q])
                        cx.dma("sync", "xst%d" % q,
                               xstm_d[i0 * 128:(i0 + 4) * 128, c * 128:(c + 1) * 128].rearrange("(a p) n -> p a n", p=128),
                               xst[q][:, 0:512].rearrange("p (a b) -> p a b", a=4),
                               reads=["xst%d" % q], writes=[("xstm", i0 + k) for k in range(4)])
                cx.barrier()

            psCBA = st.enter_context(nc.psum_tensor("psCBA", [128, 3, 128], F32))
            psBC = [st.enter_context(nc.psum_tensor("psBC%d" % g, [128, 8, 128], F32)) for g in range(2)]
            psY = [st.enter_context(nc.psum_tensor("psY%d" % g, [128, 512], F32)) for g in range(2)]
            psYS = st.enter_context(nc.psum_tensor("psYS", [128, 512], F32))
            psAcs = psCBA[:, 2, 0:16]
            Acs = T("Acs", [128, 16], F32)
            expA = T("expA", [128, 16], F32)
            rhs2 = [T("rhs2%d" % g, [128, 8, 128], F32) for g in range(2)]
            d1 = [T("d1%d" % g, [128, 8, 128], F32) for g in range(2)]
            LT = [T("LT%d" % g, [128, 8, 128], F32) for g in range(2)]
            Mb = [T("Mb%d" % g, [128, 8, 128], BF16) for g in range(2)]
            cbm = [T("cbm%d" % g, [128, 128], F32) for g in range(2)]
            Xb = T("Xb", [128, 1024], BF16)
            Xd = [T("Xd%d" % g, [128, 512], BF16) for g in range(2)]
            dec = [T("dec%d" % g, [128, 8], F32) for g in range(2)]
            cd = [T("cd%d" % g, [128, 8], F32) for g in range(2)]
            tmpy = [T("tmpy%d" % g, [128, 512], F32) for g in range(2)]
            ydir = [T("ydir%d" % i, [128, 1024], F32) for i in range(2)]
            state = [T("state%d" % g, [128, 512], F32) for g in range(2)]
            stbf = [T("stbf%d" % g, [128, 512], BF16) for g in range(2)]
            xs_t = [T("xs_t%d" % i, [128, 1024], BF16) for i in range(2)]
            zs_t = [T("zs_t%d" % i, [128, 1024], BF16) for i in range(2)]
            yb_t = [T("yb_t%d" % i, [128, 1024], F32) for i in range(2)]
            gg = T("gg", [128, 1024], F32)
            junk2 = T("junk2", [128, 1024], BF16)
            ssq = T("ssq1", [128, 1], F32)
            ssdtm = [T("ssdtm0", [128, 1024], BF16)] * 2

            def ssd_pass(fwd):
                o = 0 if fwd else 16
                tri = trif if fwd else trib
                ktri = "trif" if fwd else "trib"
                lend = 127 if fwd else 0
                order = list(range(NT)) if fwd else list(range(NT - 1, -1, -1))
                for idx, i in enumerate(order):
                    p = idx % 2
                    tsl = slice(i * 128, (i + 1) * 128)
                    kxs = "xs_t%d" % p
                    cx.dma("sync", kxs, xs_t[p][:], xstm_d[tsl, :], reads=[("xstm", i)], writes=[kxs])
                    if fwd:
                        cx.dma("sync", "zs_t%d" % p, zs_t[p][:], zs_d[tsl, :], writes=["zs_t%d" % p])
                        cx.dma("sync", "yb_t%d" % p, yb_t[p][:], yb_d[tsl, :], reads=[("yb_d", i)], writes=["yb_t%d" % p])
                    cx.op("tensor", lambda e, i=i: e.matmul(psAcs, lhsT=tri[:], rhs=Adt[:, i, o:o + 16], start=True, stop=True),
                          reads=[ktri, "Adt"], writes=["psAcs"])
                    cx.op("vector", lambda e: e.tensor_copy(out=Acs[:], in_=psAcs), reads=["psAcs"], writes=["Acs"])
                    cx.op("scalar", lambda e: e.activation(out=expA[:], in_=psAcs, func=AF.Exp), reads=["psAcs"], writes=["expA"])
                    cx.op("vector", lambda e, p=p, i=i: e.tensor_tensor(
                        out=Xb[:].rearrange("p (h d) -> p h d", d=64), in0=xs_t[p][:].rearrange("p (h d) -> p h d", d=64),
                        in1=dtv[:, i, o:o + 16].unsqueeze(2).to_broadcast([128, 16, 64]), op=ALU.mult),
                        reads=[kxs, "dtv"], writes=["Xb"])
                    yd = ydir[p]; kyd = "ydir%d" % p
                    for g in range(2):
                        hs = slice(g * 8, (g + 1) * 8)
                        G = str(g)
                        cx.op("vector", lambda e, i=i, g=g: e.tensor_tensor(
                            out=rhs2[g][:], in0=tri[:].unsqueeze(1).to_broadcast([128, 8, 128]),
                            in1=Adt[:, i, o + g * 8:o + g * 8 + 8].unsqueeze(2).to_broadcast([128, 8, 128]), op=ALU.mult),
                            reads=[ktri, "Adt"], writes=["rhs2" + G])
                        for hh in range(2):
                            cx.op("tensor", lambda e, hh=hh, g=g: e.matmul(psBC[g][:, hh * 4:(hh + 1) * 4, :], lhsT=ones_f[:],
                                                                           rhs=rhs2[g][:, hh * 4:(hh + 1) * 4, :], start=True, stop=True),
                                  reads=["rhs2" + G, "ones_f"], writes=["psBC" + G])
                        cx.op("tensor", lambda e, g=g: e.matmul(psCBA[:, g, :], lhsT=BT[:, g, tsl], rhs=CT[:, g, tsl], start=True, stop=True),
                              reads=["BT", "CT"], writes=["psCB" + G])
                    for g in range(2):
                        hs = slice(g * 8, (g + 1) * 8)
                        G = str(g)
                        cx.op("vector", lambda e, g=g: e.tensor_tensor(out=cbm[g][:], in0=psCBA[:, g, :], in1=tri[:], op=ALU.mult),
                              reads=["psCB" + G, ktri], writes=["cbm" + G])
                        cx.op("vector", lambda e, hs=hs, g=g: e.tensor_tensor(
                            out=d1[g][:], in0=psBC[g][:], in1=Acs[:, hs].unsqueeze(2).to_broadcast([128, 8, 128]), op=ALU.subtract),
                            reads=["psBC" + G, "Acs"], writes=["d1" + G])
                        cx.op("scalar", lambda e, g=g: e.activation(out=LT[g][:], in_=d1[g][:], func=AF.Exp), reads=["d1" + G], writes=["LT" + G])
                        if idx < NT - 1:
                            cx.op("vector", lambda e, hs=hs, g=g: e.tensor_tensor(
                                out=dec[g][:].unsqueeze(2), in0=psBC[g][:, :, lend:lend + 1], in1=Acs[:, hs].unsqueeze(2), op=ALU.subtract),
                                reads=["psBC" + G, "Acs"], writes=["dec" + G])
                            cx.op("scalar", lambda e, g=g: e.activation(out=dec[g][:], in_=dec[g][:], func=AF.Exp), reads=["dec" + G], writes=["dec" + G])
                            cx.op("scalar", lambda e, g=g: e.activation(out=cd[g][:].unsqueeze(2), in_=psBC[g][:, :, lend:lend + 1], func=AF.Exp),
                                  reads=["psBC" + G], writes=["cd" + G])
                    for g in range(2):
                        G = str(g)
                        gsl = slice(g * 512, (g + 1) * 512)
                        cx.op("vector", lambda e, g=g: e.scalar_tensor_tensor(
                            out=Mb[g][:], in0=LT[g][:], scalar=1.0, in1=cbm[g][:].unsqueeze(1).to_broadcast([128, 8, 128]),
                            op0=ALU.min, op1=ALU.mult), reads=["LT" + G, "cbm" + G], writes=["Mb" + G])
                        for h in range(8):
                            cx.op("tensor", lambda e, h=h, g=g: e.matmul(
                                psY[g][:, h * 64:(h + 1) * 64], lhsT=Mb[g][:, h, :], rhs=Xb[:, (g * 8 + h) * 64:(g * 8 + h + 1) * 64],
                                start=True, stop=True), reads=["Mb" + G, "Xb"], writes=["psY" + G])
                        if idx < NT - 1:
                            cx.op("vector", lambda e, gsl=gsl, g=g: e.tensor_tensor(
                                out=Xd[g][:].rearrange("p (h d) -> p h d", d=64), in0=Xb[:, gsl].rearrange("p (h d) -> p h d", d=64),
                                in1=dec[g][:].unsqueeze(2).to_broadcast([128, 8, 64]), op=ALU.mult), reads=["Xb", "dec" + G], writes=["Xd" + G])
                    for g in range(2):
                        hs = slice(g * 8, (g + 1) * 8)
                        G = str(g)
                        gsl = slice(g * 512, (g + 1) * 512)
                        if idx > 0:
                            cx.op("tensor", lambda e, g=g: e.matmul(psYS[:], lhsT=CT[:, g, tsl], rhs=stbf[g][:], start=True, stop=True),
                                  reads=["CT", "stbf" + G], writes=["psYS"])
                            cx.op("vector", lambda e, hs=hs, g=g: e.tensor_tensor(
                                out=tmpy[g][:].rearrange("p (h d) -> p h d", d=64), in0=psYS[:].rearrange("p (h d) -> p h d", d=64),
                                in1=expA[:, hs].unsqueeze(2).to_broadcast([128, 8, 64]), op=ALU.mult),
                                reads=["psYS", "expA"], writes=["tmpy" + G])
                            cx.op("vector", lambda e, yd=yd, gsl=gsl, g=g: e.tensor_tensor(out=yd[:, gsl], in0=tmpy[g][:], in1=psY[g][:], op=ALU.add),
                                  reads=["tmpy" + G, "psY" + G], writes=[(kyd, g)])
                        else:
                            cx.op("vector", lambda e, yd=yd, gsl=gsl, g=g: e.tensor_copy(out=yd[:, gsl], in_=psY[g][:]),
                                  reads=["psY" + G], writes=[(kyd, g)])
                        if idx < NT - 1:
                            cx.op("tensor", lambda e, i=i, g=g: e.matmul(psYS[:], lhsT=Btm[:, i, g, :], rhs=Xd[g][:], start=True, stop=True),
                                  reads=["Btm", "Xd" + G], writes=["psYS"])
                            if idx > 0:
                                cx.op("vector", lambda e, g=g: e.tensor_tensor(
                                    out=state[g][:].rearrange("p (h d) -> p h d", d=64), in0=state[g][:].rearrange("p (h d) -> p h d", d=64),
                                    in1=cd[g][:].unsqueeze(2).to_broadcast([128, 8, 64]), op=ALU.mult),
                                    reads=["state" + G, "cd" + G], writes=["state" + G])
                                cx.op("vector", lambda e, g=g: e.tensor_tensor(out=state[g][:], in0=state[g][:], in1=psYS[:], op=ALU.add),
                                      reads=["state" + G, "psYS"], writes=["state" + G])
                            else:
                                cx.op("vector", lambda e, g=g: e.tensor_copy(out=state[g][:], in_=psYS[:]),
                                      reads=["psYS"], writes=["state" + G])
                            cx.op("scalar", lambda e, g=g: e.activation(out=stbf[g][:], in_=state[g][:], func=AF.Copy),
                                  reads=["state" + G], writes=["stbf" + G])
                    ykeys = [(kyd, 0), (kyd, 1)]
                    if not fwd:
                        cx.dma("sync", kyd, yb_d[tsl, :], yd[:], reads=ykeys, writes=[("yb_d", i)])
                    else:
                        cx.op("vector", lambda e, yd=yd, p=p: e.tensor_tensor(out=yd[:], in0=yd[:], in1=yb_t[p][:], op=ALU.add),
                              reads=ykeys + ["yb_t%d" % p], writes=ykeys)
                        cx.op("vector", lambda e, p=p: e.tensor_tensor(
                            out=gg[:].rearrange("p (h d) -> p h d", d=64), in0=xs_t[p][:].rearrange("p (h d) -> p h d", d=64),
                            in1=ssdp[:, 64:80].unsqueeze(2).to_broadcast([128, 16, 64]), op=ALU.mult),
                            reads=[kxs, "ssdp"], writes=["gg"])
                        cx.op("vector", lambda e, yd=yd: e.tensor_tensor(out=gg[:], in0=gg[:], in1=yd[:], op=ALU.add),
                              reads=["gg"] + ykeys, writes=["gg"])
                        cx.op("vector", lambda e, p=p: e.tensor_tensor(out=gg[:], in0=gg[:], in1=zs_t[p][:], op=ALU.mult),
                              reads=["gg", "zs_t%d" % p], writes=["gg"])
                        cx.op("scalar", lambda e: e.activation(out=junk2[:], in_=gg[:], func=AF.Square, accum_out=ssq[:]),
                              reads=["gg"], writes=["junk2", "ssq1"])
                        cx.op("vector", lambda e: e.tensor_scalar(out=ssq[:], in0=ssq[:], scalar1=1.0 / 1024, scalar2=EPS,
                                                                  op0=ALU.mult, op1=ALU.add), reads=["ssq1"], writes=["ssq1"])
                        cx.op("scalar", lambda e: e.activation(out=ssq[:], in_=ssq[:], func=AF.Sqrt), reads=["ssq1"], writes=["ssq1"])
                        cx.op("vector", lambda e: e.reciprocal(out=ssq[:], in_=ssq[:]), reads=["ssq1"], writes=["ssq1"])
                        cx.op("vector", lambda e, p=p: e.scalar_tensor_tensor(out=ssdtm[p][:], in0=gg[:], scalar=ssq[:, 0:1], in1=ssdnw[:],
                                                                              op0=ALU.mult, op1=ALU.mult),
                              reads=["gg", "ssq1", "ssdnw"], writes=["ssdtm0"])
                        cx.dma("sync", "ssdtm0", ssdtm_d[tsl, :], ssdtm[p][:], reads=["ssdtm0"], writes=[("ssdtm_d", i)])

            ssd_pass(False)
            ssd_pass(True)
            cx.barrier()
        with contextlib.ExitStack() as st:
            psT8 = [st.enter_context(nc.psum_tensor("psT8%d" % i, [128, 8, 128], BF16)) for i in range(2)]
            stl = [st.enter_context(nc.sbuf_tensor("y_stl%d" % i, [128, 1024], BF16)) for i in range(2)]
            for i in range(NT):
                p = i % 2
                cx.dma("sync", "stl%d" % p, stl[p][:], ssdtm_d[i * 128:(i + 1) * 128, :], reads=[("ssdtm_d", i)], writes=["stl%d" % p])
                for c in range(8):
                    cx.op("tensor", lambda e, c=c, p=p: e.transpose(out=psT8[p][:, c, :], in_=stl[p][:, c * 128:(c + 1) * 128],
                                                                    identity=identb[:]), reads=["stl%d" % p], writes=["psT8%d" % p])
                cx.op("scalar", lambda e, p=p, i=i: e.activation(out=ssdT[:, :, i * 128:(i + 1) * 128], in_=psT8[p][:], func=AF.Copy),
                      reads=["psT8%d" % p], writes=[("hT", i)])
            if "d_ssdT" in debug:
                cx.dma("sync", "dbg", dbg_out["d_ssdT"][:, :], ssdT[:].rearrange("p c s -> p (c s)"),
                       reads=[("hT", i) for i in range(NT)])
            cx.barrier()


        g2bc = sb("g2bc", [128, D], F32)
        g1scope = contextlib.ExitStack()
        g1bc = g1scope.enter_context(nc.sbuf_tensor("s_g1bc", [128, D], F32))
        with contextlib.ExitStack() as st:
            diag = [st.enter_context(nc.sbuf_tensor("diag%d" % i, [128, 128], F32)) for i in range(2)]
            psG = st.enter_context(nc.psum_tensor("psG", [128, 1024], F32))
            for (dst, kd, c0) in [(g1bc, "g1bc", 16), (g2bc, "g2bc", 40)]:
                for c in range(8):
                    dg = diag[c % 2]; kg = "diag%d" % (c % 2)
                    cx.op("vector", lambda e, dg=dg, c=c, c0=c0: e.tensor_scalar(out=dg[:], in0=ident[:], scalar1=modT[:, c0 + c:c0 + c + 1],
                                                                                 scalar2=None, op0=ALU.mult), reads=["ident", "modT"], writes=[kg])
                    cx.op("tensor", lambda e, dg=dg, c=c: e.matmul(psG[:, c * 128:(c + 1) * 128], lhsT=ones_f[:], rhs=dg[:], start=True, stop=True),
                          reads=[kg, "ones_f"], writes=["psG"])
                cx.op("vector", lambda e, dst=dst: e.tensor_copy(out=dst[:], in_=psG[:]), reads=["psG"], writes=[kd])
            cx.barrier()
        ssdT = hT
        with contextlib.ExitStack() as st:
            def T(name, shape, dt):
                return st.enter_context(nc.sbuf_tensor("z_" + name, list(shape), dt))
            wau = T("wau", [128, 4, D], BF16)
            wsu = T("wsu", [128, 8, D], BF16)
            wout = T("wout", [128, 8, D], BF16)
            attg = [T("attg%d" % i, [128, 4, 512], BF16) for i in range(2)]
            g0t = [T("g0t%d" % i, [128, 512], BF16) for i in range(2)]
            g1t = [T("g1t%d" % i, [128, 512], BF16) for i in range(2)]
            t1 = T("t1", [128, 512], F32)
            t2 = T("t2", [128, 512], F32)
            mT = T("mT", [128, 8, 512], BF16)
            xin = [T("xin%d" % i, [128, D], F32) for i in range(2)]
            x1t = [T("x1t%d" % i, [128, D], F32) for i in range(2)]
            psUa = [st.enter_context(nc.psum_tensor("psUa%d" % i, [128, 512], F32)) for i in range(2)]
            psUs = [st.enter_context(nc.psum_tensor("psUs%d" % i, [128, 512], F32)) for i in range(2)]
            psM = [st.enter_context(nc.psum_tensor("psM%d" % i, [128, 1024], F32)) for i in range(2)]
            cx.dma("gpsimd", "wau", wau[:], wau_in.rearrange("(kc p) n -> p kc n", p=128), writes=["wau"])
            cx.dma("gpsimd", "wsu", wsu[:], wsu_in.rearrange("(kc p) n -> p kc n", p=128), writes=["wsu"])
            cx.dma("gpsimd", "wout", wout[:], wout_in.rearrange("(kc p) n -> p kc n", p=128), writes=["wout"])
            n = 0
            for grp in range(8):
                gs = slice(grp * 512, (grp + 1) * 512)
                ag = attg[grp % 2]; ka = "attg%d" % (grp % 2)
                cx.dma("sync", ka, ag[:], attT_d[:, gs].rearrange("(c p) s -> p c s", p=128), reads=["attT_d"], writes=[ka])
                for dc in range(8):
                    b = n % 2; n += 1
                    dsl = slice(dc * 128, (dc + 1) * 128)
                    cx.dma("sync", "g0t%d" % b, g0t[b][:], gatesT_d[dc * 128:(dc + 1) * 128, gs], writes=["g0t%d" % b])
                    cx.dma("sync", "g1t%d" % b, g1t[b][:], gatesT_d[1024 + dc * 128:1024 + (dc + 1) * 128, gs], writes=["g1t%d" % b])
                    for kc in range(4):
                        cx.op("tensor", lambda e, b=b, kc=kc, dsl=dsl, ag=ag: e.matmul(psUa[b][:], lhsT=wau[:, kc, dsl], rhs=ag[:, kc, :],
                                                                                     start=(kc == 0), stop=(kc == 3)),
                              reads=["wau", ka], writes=["psUa%d" % b])
                    for kc in range(8):
                        cx.op("tensor", lambda e, b=b, kc=kc, dsl=dsl, gs=gs: e.matmul(psUs[b][:], lhsT=wsu[:, kc, dsl], rhs=ssdT[:, kc, gs],
                                                                                     start=(kc == 0), stop=(kc == 7)),
                              reads=["wsu"] + [("hT", grp * 4 + k) for k in range(4)], writes=["psUs%d" % b])
                    cx.op("vector", lambda e, b=b: e.tensor_tensor(out=t1[:], in0=psUa[b][:], in1=g0t[b][:], op=ALU.mult),
                          reads=["psUa%d" % b, "g0t%d" % b], writes=["t1"])
                    cx.op("vector", lambda e, b=b: e.tensor_tensor(out=t2[:], in0=psUs[b][:], in1=g1t[b][:], op=ALU.mult),
                          reads=["psUs%d" % b, "g1t%d" % b], writes=["t2"])
                    cx.op("vector", lambda e, dc=dc: e.tensor_tensor(out=mT[:, dc, :], in0=t1[:], in1=t2[:], op=ALU.add),
                          reads=["t1", "t2"], writes=[("mT", dc)])
                for sub in range(4):
                    i = grp * 4 + sub
                    p = i % 2
                    tsl = slice(i * 128, (i + 1) * 128)
                    cx.dma("sync", "xin%d" % p, xin[p][:], x[tsl, :], writes=["xin%d" % p])
                    for half in range(2):
                        for dc in range(8):
                            cx.op("tensor", lambda e, p=p, half=half, dc=dc, sub=sub: e.matmul(
                                psM[p][:, half * 512:(half + 1) * 512], lhsT=mT[:, dc, sub * 128:(sub + 1) * 128],
                                rhs=wout[:, dc, half * 512:(half + 1) * 512], start=(dc == 0), stop=(dc == 7)),
                                reads=["wout"] + [("mT", d_) for d_ in range(8)], writes=["psM%d" % p])
                    cx.op("vector", lambda e, p=p: e.tensor_tensor(out=x1t[p][:], in0=psM[p][:], in1=g1bc[:], op=ALU.mult),
                          reads=["psM%d" % p, "g1bc"], writes=["x1t%d" % p])
                    cx.op("vector", lambda e, p=p: e.tensor_tensor(out=x1t[p][:], in0=x1t[p][:], in1=xin[p][:], op=ALU.add),
                          reads=["x1t%d" % p, "xin%d" % p], writes=["x1t%d" % p])
                    cx.dma("sync", "x1t%d" % p, x1_d[tsl, :], x1t[p][:], reads=["x1t%d" % p], writes=[("x1_d", i)])
            cx.barrier()
        g1scope.close()
        with contextlib.ExitStack() as st:
            norm_mod_transpose(x1_d, gam2, modT[:, 24:32], st, "b", rkey="x1_d")
            if "d_h2T" in debug:
                cx.dma("sync", "dbg", dbg_out["d_h2T"][:, :], hT[:].rearrange("p c s -> p (c s)"),
                       reads=[("hT", i) for i in range(NT)])
            cx.barrier()


        h2T = hT
        i1T = sb("i1T", [128, S], BF16)
        i2T = sb("i2T", [128, S], BF16)
        gT = sb("gT", [128, S], BF16)
        GELU = AF.Gelu_apprx_tanh
        with contextlib.ExitStack() as st:
            def T(name, shape, dt):
                return st.enter_context(nc.sbuf_tensor("r_" + name, list(shape), dt))
            wq = T("wq", [128, 8, 2048], BF16)
            keysT = T("keysT", [128, 16, 128], BF16)
            iota16 = T("iota16", [128, 16], F32)
            qT = T("qT", [128, 16, 512], BF16)
            bufA = T("bufA", [128, 2048], F32)
            bufB = T("bufB", [128, 2048], F32)
            sc = bufA[:].rearrange("p (a b) -> p a b", b=128)
            sc2 = bufB[:].rearrange("p (a b) -> p a b", b=128)
            cand = bufA[:].rearrange("p (a b) -> p a b", b=256)
            cand2 = bufB[:].rearrange("p (a b) -> p a b", b=256)
            oh = bufA[:].rearrange("p (a b) -> p a b", b=16)
            vtop = T("vtop", [128, 16, 16], F32)
            ixu = T("ixu", [128, 16, 16], U32)
            ixf = T("ixf", [128, 16, 16], F32)
            sc16 = T("sc16", [128, 8, 16], F32)
            posu = T("posu", [128, 8, 16], U32)
            au = T("au", [128, 8, 16], U32)
            bu = T("bu", [128, 8, 16], U32)
            af = T("af", [128, 8, 16], F32)
            bf = T("bf", [128, 8, 16], F32)
            esum = T("esum", [128, 8], F32)
            gf = T("gf", [128, 8, 16], F32)
            idf = [T("idf%d" % m, [128, 128], F32) for m in range(2)]
            psQ = st.enter_context(nc.psum_tensor("psQ", [128, 512], F32))
            psSc = st.enter_context(nc.psum_tensor("psSc", [128, 16, 128], F32))
            psT3 = st.enter_context(nc.psum_tensor("psT3", [128, 3, 128], F32))
            cx.dma("gpsimd", "wq", wq[:], wq_in.rearrange("(kc p) n -> p kc n", p=128), writes=["wq"])
            cx.dma("gpsimd", "r0", keysT[:].rearrange("p a b -> p (a b)"), keysT_in[:, :], writes=["keysT"])
            cx.dma("sync", "r1", iota16[:], iota16_in[:, :], writes=["iota16"])
            for grp in range(8):
                gs = slice(grp * 512, (grp + 1) * 512)
                for j in range(16):
                    for kc in range(8):
                        cx.op("tensor", lambda e, j=j, kc=kc, gs=gs: e.matmul(psQ[:], lhsT=wq[:, kc, j * 128:(j + 1) * 128], rhs=h2T[:, kc, gs],
                                                                             start=(kc == 0), stop=(kc == 7)), reads=["wq"], writes=["psQ"])
                    cx.op("scalar", lambda e, j=j: e.activation(out=qT[:, j, :], in_=psQ[:], func=AF.Copy), reads=["psQ"], writes=[("qT", j)])
                for sub in range(4):
                    i = grp * 4 + sub
                    tsl = slice(i * 128, (i + 1) * 128)
                    for j in range(16):
                        cx.op("tensor", lambda e, j=j, sub=sub: e.matmul(psSc[:, j, :], lhsT=qT[:, j, sub * 128:(sub + 1) * 128], rhs=keysT[:, j, :],
                                                                         start=True, stop=True), reads=[("qT", j), "keysT"], writes=["psSc"])
                    cx.op("scalar", lambda e: e.activation(out=sc, in_=psSc[:], func=AF.Copy), reads=["psSc"], writes=["bufA"])
                    for j in range(16):
                        cx.op("vector", lambda e, j=j: e.max(out=vtop[:, j, 0:8], in_=sc[:, j, :]), reads=["bufA"], writes=[("vtop", j)])
                    for j in range(16):
                        cx.op("vector", lambda e, j=j: e.max_index(out=ixu[:, j, 0:8], in_max=vtop[:, j, 0:8], in_values=sc[:, j, :]),
                              reads=["bufA", ("vtop", j)], writes=[("ixu", j)])
                    for j in range(16):
                        cx.op("vector", lambda e, j=j: e.match_replace(out=sc2[:, j, :], in_to_replace=vtop[:, j, 0:8], in_values=sc[:, j, :],
                                                                       imm_value=-1e30), reads=["bufA", ("vtop", j)], writes=[("bufB", j)])
                    for j in range(16):
                        cx.op("vector", lambda e, j=j: e.max(out=vtop[:, j, 8:16], in_=sc2[:, j, :]), reads=[("bufB", j)], writes=[("vtop", j)])
                    for j in range(16):
                        cx.op("vector", lambda e, j=j: e.max_index(out=ixu[:, j, 8:16], in_max=vtop[:, j, 8:16], in_values=sc2[:, j, :]),
                              reads=[("bufB", j), ("vtop", j)], writes=[("ixu", j)])
                    vk = [("vtop", j) for j in range(16)]
                    ik = [("ixu", j) for j in range(16)]
                    cx.op("vector", lambda e: e.tensor_copy(out=ixf[:], in_=ixu[:]), reads=ik, writes=["ixf"])
                    vv = vtop[:].rearrange("p (h two) k -> p h two k", two=2)
                    cx.op("vector", lambda e, vv=vv: e.tensor_tensor(
                        out=cand.rearrange("p h (a b) -> p h a b", b=16), in0=vv[:, :, 0, :].unsqueeze(3).to_broadcast([128, 8, 16, 16]),
                        in1=vv[:, :, 1, :].unsqueeze(2).to_broadcast([128, 8, 16, 16]), op=ALU.add), reads=vk, writes=["bufA"])
                    for h in range(8):
                        cx.op("vector", lambda e, h=h: e.max(out=sc16[:, h, 0:8], in_=cand[:, h, :]), reads=["bufA"], writes=[("sc16", h)])
                    for h in range(8):
                        cx.op("vector", lambda e, h=h: e.max_index(out=posu[:, h, 0:8], in_max=sc16[:, h, 0:8], in_values=cand[:, h, :]),
                              reads=["bufA", ("sc16", h)], writes=[("posu", h)])
                    for h in range(8):
                        cx.op("vector", lambda e, h=h: e.match_replace(out=cand2[:, h, :], in_to_replace=sc16[:, h, 0:8], in_values=cand[:, h, :],
                                                                       imm_value=-1e30), reads=["bufA", ("sc16", h)],
                              writes=[("bufB", 2 * h), ("bufB", 2 * h + 1)])
                    for h in range(8):
                        cx.op("vector", lambda e, h=h: e.max(out=sc16[:, h, 8:16], in_=cand2[:, h, :]),
                              reads=[("bufB", 2 * h), ("bufB", 2 * h + 1)], writes=[("sc16", h)])
                    for h in range(8):
                        cx.op("vector", lambda e, h=h: e.max_index(out=posu[:, h, 8:16], in_max=sc16[:, h, 8:16], in_values=cand2[:, h, :]),
                              reads=[("bufB", 2 * h), ("bufB", 2 * h + 1), ("sc16", h)], writes=[("posu", h)])
                    sk = [("sc16", h) for h in range(8)]
                    pk = [("posu", h) for h in range(8)]
                    cx.op("vector", lambda e: e.tensor_tensor(out=gf[:], in0=sc16[:], in1=sc16[:, :, 0:1].to_broadcast([128, 8, 16]),
                                                              op=ALU.subtract), reads=sk, writes=["gf"])
                    cx.op("scalar", lambda e: e.activation(out=gf[:], in_=gf[:], func=AF.Exp), reads=["gf"], writes=["gf"])
                    cx.op("vector", lambda e: e.tensor_reduce(out=esum[:], in_=gf[:], axis=AX.X, op=ALU.add), reads=["gf"], writes=["esum"])
                    cx.op("vector", lambda e: e.reciprocal(out=esum[:], in_=esum[:]), reads=["esum"], writes=["esum"])
                    cx.op("vector", lambda e: e.tensor_tensor(out=gf[:], in0=gf[:], in1=esum[:].unsqueeze(2).to_broadcast([128, 8, 16]),
                                                              op=ALU.mult), reads=["gf", "esum"], writes=["gf"])
                    cx.op("vector", lambda e: e.tensor_single_scalar(out=au[:], in_=posu[:], scalar=4, op=ALU.logical_shift_right),
                          reads=pk, writes=["au"])
                    cx.op("vector", lambda e: e.tensor_single_scalar(out=bu[:], in_=posu[:], scalar=15, op=ALU.bitwise_and),
                          reads=pk, writes=["bu"])
                    cx.op("vector", lambda e: e.tensor_copy(out=af[:], in_=au[:]), reads=["au"], writes=["af"])
                    cx.op("vector", lambda e: e.tensor_copy(out=bf[:], in_=bu[:]), reads=["bu"], writes=["bf"])
                    ixv = ixf[:].rearrange("p (h two) k -> p h two k", two=2)
                    for m, (sel, ksel) in enumerate([(af, "af"), (bf, "bf")]):
                        cx.op("vector", lambda e, sel=sel: e.tensor_tensor(
                            out=oh, in0=sel[:].rearrange("p h k -> p (h k)").unsqueeze(2).to_broadcast([128, 128, 16]),
                            in1=iota16[:].unsqueeze(1).to_broadcast([128, 128, 16]), op=ALU.is_equal), reads=[ksel, "iota16"], writes=["bufA"])
                        cx.op("vector", lambda e, m=m, ixv=ixv: e.tensor_tensor(
                            out=oh.rearrange("p (h k) a -> p h k a", k=16), in0=oh.rearrange("p (h k) a -> p h k a", k=16),
                            in1=ixv[:, :, m, :].unsqueeze(2).to_broadcast([128, 8, 16, 16]), op=ALU.mult), reads=["bufA", "ixf"], writes=["bufA"])
                        cx.op("vector", lambda e, m=m: e.tensor_reduce(out=idf[m][:], in_=oh, axis=AX.X, op=ALU.add),
                              reads=["bufA"], writes=["idf%d" % m])
                    cx.op("tensor", lambda e: e.transpose(out=psT3[:, 0, :], in_=idf[0][:], identity=ident[:]), reads=["idf0"], writes=["psT3"])
                    cx.op("tensor", lambda e: e.transpose(out=psT3[:, 1, :], in_=idf[1][:], identity=ident[:]), reads=["idf1"], writes=["psT3"])
                    cx.op("tensor", lambda e: e.transpose(out=psT3[:, 2, :], in_=gf[:].rearrange("p h k -> p (h k)"), identity=ident[:]),
                          reads=["gf"], writes=["psT3"])
                    cx.op("scalar", lambda e: e.activation(out=i1T[:, tsl], in_=psT3[:, 0, :], func=AF.Copy), reads=["psT3"], writes=[("rt", i)])
                    cx.op("scalar", lambda e: e.activation(out=i2T[:, tsl], in_=psT3[:, 1, :], func=AF.Identity, scale=-1.0), reads=["psT3"], writes=[("rt", i)])
                    cx.op("scalar", lambda e: e.activation(out=gT[:, tsl], in_=psT3[:, 2, :], func=AF.Copy), reads=["psT3"], writes=[("rt", i)])
            for kc in range(8):
                cx.dma("sync", "h2Td", h2T_d[kc * 128:(kc + 1) * 128, :], h2T[:, kc, :])
            for nm, t in [("d_i1T", i1T), ("d_i2T", i2T), ("d_gT", gT)]:
                if nm in debug:
                    cx.dma("sync", "dbg", dbg_out[nm][:, :], t[:])
            cx.barrier()

        with contextlib.ExitStack() as st:
            def T(name, shape, dt):
                return st.enter_context(nc.sbuf_tensor("p_" + name, list(shape), dt))
            TG = 256
            iota128f = T("iota128f", [128, 128], F32)
            iota128 = T("iota128", [128, 128], BF16)
            niota128 = T("niota128", [128, 128], BF16)
            Btmp = [T("Btmp%d" % i, [128, 128], BF16) for i in range(4)]
            fnw = T("fnw", [128, D], F32)
            gwb = [T("gw", [128, 128, TG], BF16), hT[:].rearrange("p c (a b) -> p (c a) b", b=TG)]
            h2g = [T("h2g%d" % i, [128, 8, TG], BF16) for i in range(2)]
            Aoh = [T("Aoh%d" % i, [128, 128], BF16) for i in range(4)]
            Boh = [T("Boh%d" % i, [128, 128], BF16) for i in range(4)]
            ut = [T("ut%d" % i, [128, 2, 8, 128], BF16) for i in range(2)]
            vt = [T("vt%d" % i, [128, 2, D], BF16) for i in range(2)]
            gel = [T("gel%d" % i, [128, TG], F32) for i in range(2)]
            Pb = [T("Pb%d" % i, [128, TG], BF16) for i in range(2)]
            x1s = [T("x1s%d" % i, [128, D], F32) for i in range(1)] * 2
            xo = [T("xo%d" % i, [128, D], F32) for i in range(1)] * 2
            ssq = T("ssq3", [128, 2], F32)
            psW = [st.enter_context(nc.psum_tensor("psW%d" % i, [128, 4, 128], F32)) for i in range(2)]
            psA = [st.enter_context(nc.psum_tensor("psA_%d" % i, [128, 512], F32)) for i in range(2)]
            psO = [st.enter_context(nc.psum_tensor("psO_%d" % i, [128, 1024], F32)) for i in range(2)]
            cx.dma("sync", "p0", iota128f[:], iota128_in[:, :], writes=["iota128f"])
            cx.op("vector", lambda e: e.tensor_copy(out=iota128[:], in_=iota128f[:]), reads=["iota128f"], writes=["iota128"])
            cx.op("vector", lambda e: e.tensor_scalar(out=niota128[:], in0=iota128f[:], scalar1=-1.0, scalar2=None, op0=ALU.mult),
                  reads=["iota128f"], writes=["niota128"])
            cx.dma("sync", "p1", fnw[:], fnw_in.partition_broadcast(128), writes=["fnw"])
            NG = S // TG
            nWc = [0]

            def gw_onehots(grp, q4, toks):
                t0 = grp * TG
                for tq in toks:
                    col = t0 + q4 * 4 + tq
                    r = tq
                    cx.op("vector", lambda e, r=r, col=col: e.tensor_scalar(
                        out=Aoh[r][:], in0=iota128[:], scalar1=i1T[:, col:col + 1], scalar2=gT[:, col:col + 1],
                        op0=ALU.is_equal, op1=ALU.mult), reads=["iota128"], writes=["Aoh%d" % r])
                    if tq % 2 == 1:
                        cx.op("vector", lambda e, r=r, col=col: e.tensor_scalar(
                            out=Boh[r][:], in0=niota128[:], scalar1=i2T[:, col:col + 1], scalar2=None,
                            op0=ALU.is_equal), reads=["niota128"], writes=["Boh%d" % r])
                    else:
                        cx.op("scalar", lambda e, r=r, col=col: e.activation(out=Btmp[r][:], in_=iota128[:], func=AF.Abs,
                                                                              bias=i2T[:, col:col + 1], scale=1.0),
                              reads=["iota128"], writes=["Btmp%d" % r])
                        cx.op("scalar", lambda e, r=r: e.activation(out=Boh[r][:], in_=Btmp[r][:], func=AF.Relu, bias=1.0, scale=-1.0),
                              reads=["Btmp%d" % r], writes=["Boh%d" % r])

            def gw_mm(grp, q4):
                b = q4 % 2
                for tq in range(4):
                    cx.op("tensor", lambda e, b=b, tq=tq: e.matmul(psW[b][:, tq, :], lhsT=Aoh[tq][:], rhs=Boh[tq][:], start=True, stop=True),
                          reads=["Aoh%d" % tq, "Boh%d" % tq], writes=["psW%d" % b])

            def gw_evac(grp, q4):
                b = q4 % 2
                g_ = gwb[grp % 2]
                cx.op("scalar", lambda e: e.activation(out=g_[:, :, q4 * 4:(q4 + 1) * 4],
                                                       in_=psW[b][:].rearrange("p t j -> p j t"), func=AF.Copy),
                      reads=["psW%d" % b], writes=["gw%d" % (grp % 2)])

            def emit_gw(grp, q4):
                gw_onehots(grp, q4, (0, 1, 2, 3))
                gw_mm(grp, q4)
                gw_evac(grp, q4)

            def load_h2(grp):
                cx.dma("sync", "h2g%d" % (grp % 2), h2g[grp % 2][:], h2T_d[:, grp * TG:(grp + 1) * TG].rearrange("(c p) t -> p c t", p=128),
                       writes=["h2g%d" % (grp % 2)])

            def load_w(gb):
                jb = (gb % 64) * 2
                pb = gb % 2
                rs = slice(jb * 128, (jb + 2) * 128)
                bs = slice((gb % 64) * 128, (gb % 64 + 1) * 128)
                cx.dma("sync", "ut%d" % pb, ut[pb][:].rearrange("p t a b -> p (t a b)"), u_bf[bs, :],
                       reads=[("u_bf", jb), ("u_bf", jb + 1)], writes=["ut%d" % pb])
                cx.dma("sync", "vt%d" % pb, vt[pb][:].rearrange("p t n -> p (t n)"), v_bf[bs, :],
                       reads=[("v_bf", jb), ("v_bf", jb + 1)], writes=["vt%d" % pb])

            load_h2(0)
            load_w(0)
            load_w(1)
            for q4 in range(TG // 4):
                emit_gw(0, q4)
            for grp in range(NG):
                t0 = grp * TG
                gw = gwb[grp % 2]
                kgw = "gw%d" % (grp % 2)
                hg = h2g[grp % 2]
                khg = "h2g%d" % (grp % 2)
                if grp + 1 < NG:
                    load_h2(grp + 1)

                def emit_u(j):
                    p3 = (j // 2) % 2
                    ja = j % 2
                    b = j % 2
                    for kc in range(8):
                        cx.op("tensor", lambda e, kc=kc: e.matmul(psA[b][:, 0:TG], lhsT=ut[p3][:, ja, kc, :], rhs=hg[:, kc, :],
                                                                  start=(kc == 0), stop=(kc == 7)),
                              reads=["ut%d" % p3, khg], writes=["psA_%d" % b])

                emit_u(0)
                for j in range(128):
                    p3 = (j // 2) % 2
                    ja = j % 2
                    b = j % 2
                    if j + 1 < 128:
                        emit_u(j + 1)
                    cx.op("scalar", lambda e, b=b: e.activation(out=gel[b][:], in_=psA[b][:, 0:TG], func=GELU),
                          reads=["psA_%d" % b], writes=["gel%d" % b])
                    cx.op("vector", lambda e, b=b, j=j: e.tensor_tensor(out=Pb[b][:], in0=gel[b][:], in1=gw[:, j, :], op=ALU.mult),
                          reads=["gel%d" % b, kgw], writes=["Pb%d" % b])
                    for sub in range(2):
                        for half in range(2):
                            cx.op("tensor", lambda e, b=b, sub=sub, half=half, p3=p3, j=j, ja=ja: e.matmul(
                                psO[sub][:, half * 512:(half + 1) * 512], lhsT=Pb[b][:, sub * 128:(sub + 1) * 128],
                                rhs=vt[p3][:, ja, half * 512:(half + 1) * 512], start=(j == 0), stop=(j == 127)),
                                reads=["Pb%d" % b, "vt%d" % p3], writes=["psO_%d" % sub])
                    if j % 2 == 1:
                        gb = grp * 64 + j // 2 + 2
                        if gb < NG * 64:
                            load_w(gb)
                    if grp + 1 < NG:
                        q4 = j // 2
                        if j % 2 == 0:
                            gw_onehots(grp + 1, q4, (0, 1))
                        else:
                            gw_onehots(grp + 1, q4, (2, 3))
                            gw_mm(grp + 1, q4)
                            if q4 > 0:
                                gw_evac(grp + 1, q4 - 1)
                if grp + 1 < NG:
                    gw_evac(grp + 1, 63)
                for sub in range(2):
                    i = grp * 2 + sub
                    tsl = slice(i * 128, (i + 1) * 128)
                    cx.dma("sync", "x1s0", x1s[sub][:], x1_d[tsl, :], reads=[("x1_d", i)], writes=["x1s0"])
                    X = xo[sub]; kx = "xo0"
                    cx.op("vector", lambda e, X=X, sub=sub: e.tensor_tensor(out=X[:], in0=psO[sub][:], in1=g2bc[:], op=ALU.mult),
                          reads=["psO_%d" % sub], writes=[kx])
                    cx.op("vector", lambda e, X=X, sub=sub: e.tensor_tensor(out=X[:], in0=X[:], in1=x1s[sub][:], op=ALU.add),
                          reads=[kx, "x1s0"], writes=[kx])
                    cx.op("scalar", lambda e, X=X, sub=sub: e.activation(out=x1s[sub][:], in_=X[:], func=AF.Square, accum_out=ssq[:, sub:sub + 1]),
                          reads=[kx], writes=["x1s0", ("ssq3", sub)])
                    cx.op("vector", lambda e, sub=sub: e.tensor_scalar(out=ssq[:, sub:sub + 1], in0=ssq[:, sub:sub + 1], scalar1=1.0 / D, scalar2=EPS,
                                                                       op0=ALU.mult, op1=ALU.add), reads=[("ssq3", sub)], writes=[("ssq3", sub)])
                    cx.op("scalar", lambda e, sub=sub: e.activation(out=ssq[:, sub:sub + 1], in_=ssq[:, sub:sub + 1], func=AF.Sqrt),
                          reads=[("ssq3", sub)], writes=[("ssq3", sub)])
                    cx.op("vector", lambda e, sub=sub: e.reciprocal(out=ssq[:, sub:sub + 1], in_=ssq[:, sub:sub + 1]),
                          reads=[("ssq3", sub)], writes=[("ssq3", sub)])
                    cx.op("vector", lambda e, X=X, sub=sub: e.scalar_tensor_tensor(out=X[:], in0=X[:], scalar=ssq[:, sub:sub + 1], in1=fnw[:],
                                                                                  op0=ALU.mult, op1=ALU.mult),
                          reads=[kx, ("ssq3", sub), "fnw"], writes=[kx])
                    cx.dma("sync", kx, out[tsl, :], X[:], reads=[kx], writes=[("out", i)])
            cx.barrier()

        cx.barrier()
    print("instructions:", cx.ninst)
    return nc


def make_in_maps(inputs, ncores=8):
    f = np.float32
    C, Sg = rope_tables()
    ident = np.eye(128, dtype=f)
    qperm = np.concatenate([np.arange(h * 64, (h + 1) * 64) for h in [0, 4, 1, 5, 2, 6, 3, 7]])
    cols = np.concatenate([qperm, np.arange(512, 768), np.arange(3328, 3360), np.arange(768, 1792),
                           np.arange(1792, 3328), np.arange(3360, 5408)])
    w_in_p = np.ascontiguousarray(inputs["w_in"][0][:, cols])
    qkgain = np.concatenate([np.tile(inputs["q_gain"][0], 8), np.tile(inputs["k_gain"][0], 2)])[None, :].astype(f)
    convwT = np.ascontiguousarray(inputs["conv_w"][0].reshape(5, 12, 128).transpose(2, 1, 0).reshape(128, 60))
    convbT = np.ascontiguousarray(inputs["conv_b"][0].reshape(12, 128).T)
    wau = np.ascontiguousarray(inputs["w_attn_up"][0][qperm, :])
    wq_h = np.ascontiguousarray(inputs["peer_w_query"][0])
    keysT_h = np.ascontiguousarray(np.stack([inputs["peer_keys1"][0], inputs["peer_keys2"][0]], axis=1)
                                   .transpose(3, 0, 1, 2).reshape(128, 2048))
    iota128 = np.tile(np.arange(128, dtype=f)[None, :], (128, 1))
    iota16 = np.tile(np.arange(16, dtype=f)[None, :], (128, 1))
    fnw_h = inputs["final_norm_w"][None, :].astype(f)
    U_h = np.ascontiguousarray(inputs["peer_u"][0].reshape(128, 128, 8, 128).transpose(1, 3, 2, 0)).reshape(128 * 128, D)
    V_h = np.ascontiguousarray(inputs["peer_v"][0].reshape(128, 128, D).transpose(1, 0, 2)).reshape(128 * 128, D)
    ii = np.arange(128)
    trif = (ii[:, None] <= ii[None, :]).astype(f)
    maskf = np.where(ii[None, :] >= ii[:, None], 0.0, -30000.0).astype(f)
    ssdp = np.concatenate([inputs["dt_bias_f"][0], inputs["dt_bias_b"][0], inputs["a_log_f"][0], inputs["a_log_b"][0],
                           inputs["d_skip"][0]])[None, :].astype(f)
    maps = []
    for b in range(ncores):
        m = {
            "x": np.ascontiguousarray(inputs["x"][b]),
            "c_col": np.ascontiguousarray(inputs["c"][b].reshape(8, 128).T),
            "ada_w": np.ascontiguousarray(inputs["ada_w"][0]),
            "ada_bT": np.ascontiguousarray(inputs["ada_b"][0].reshape(48, 128).T),
            "n1T": np.ascontiguousarray(inputs["norm1_w"][0].reshape(8, 128).T),
            "n2T": np.ascontiguousarray(inputs["norm2_w"][0].reshape(8, 128).T),
            "w_in": w_in_p,
            "trif": trif, "trib": np.ascontiguousarray(trif.T), "maskf": maskf, "maskb": np.ascontiguousarray(maskf.T),
            "ssdp": ssdp, "ssdnw": inputs["ssd_norm_w"][0][None, :].astype(f),
            "wau": wau, "wsu": np.ascontiguousarray(inputs["w_ssd_up"][0]), "wout": np.ascontiguousarray(inputs["w_out"][0]),
            "wq": wq_h, "keysT": keysT_h, "iota128": iota128, "iota16": iota16, "fnw": fnw_h, "U_h": U_h, "V_h": V_h,
            "ropeC": C, "ropeS": Sg, "qkgain": qkgain, "convwT": convwT, "convbT": convbT,
            "ident": ident,
        }
        maps.append(m)
    return maps


def kernel(**inputs):
    inputs = {k: np.asarray(v) for k, v in inputs.items()}
    nc = build()
    maps = make_in_maps(inputs)
    res = run_bass_kernel_spmd(nc, maps, core_ids=list(range(8)))
    return np.stack([r["out"] for r in res.results], axis=0).astype(np.float32)
```

```python
import contextlib
import numpy as np
import concourse.bass as bass
import concourse.mybir as mybir
from concourse.bass_utils import run_bass_kernel_spmd

F32 = mybir.dt.float32
BF16 = mybir.dt.bfloat16
U32 = mybir.dt.uint32
I32 = mybir.dt.int32
AF = mybir.ActivationFunctionType
ALU = mybir.AluOpType
AX = mybir.AxisListType

S = 4096
D = 1024
NT = S // 128
EPS = 1e-6
IN_W = 5408
STRICT = True


class Sem:
    def __init__(self, h):
        self.h = h
        self.cnt = 0


class Eng:
    def __init__(self, name, h, sem):
        self.name = name
        self.h = h
        self.sem = sem
        self.seen = {}


class Buf:
    __slots__ = ("w", "rs")

    def __init__(self):
        self.w = None
        self.rs = []


class Ctx:
    def __init__(self, nc, es):
        self.nc = nc
        self.es = es
        self.engs = {}
        for name in ["tensor", "vector", "scalar", "gpsimd", "sync"]:
            sem = Sem(es.enter_context(nc.semaphore("e_" + name)))
            self.engs[name] = Eng(name, getattr(nc, name), sem)
        self.bufs = {}
        self.dsems = {}
        self.ninst = 0

    def buf(self, key):
        b = self.bufs.get(key)
        if b is None:
            b = Buf()
            self.bufs[key] = b
        return b

    def dsem(self, key):
        s = self.dsems.get(key)
        if s is None:
            s = Sem(self.es.enter_context(self.nc.semaphore("d%d" % len(self.dsems))))
            self.dsems[key] = s
        return s

    def _waits(self, E, reads, writes, skip_self=False):
        need = {}

        def add(st):
            sem, val = st
            if need.get(sem, 0) < val:
                need[sem] = val

        for k in reads:
            b = self.bufs.get(k)
            if b is not None and b.w is not None:
                add(b.w)
        for k in writes:
            b = self.buf(k)
            if b.w is not None:
                add(b.w)
            for r in b.rs:
                add(r)
        for sem, val in need.items():
            if sem is E.sem and (skip_self or not STRICT):
                continue
            if E.seen.get(sem, 0) >= val:
                continue
            E.h.wait_ge(sem.h, val)
            E.seen[sem] = val
            self.ninst += 1

    def _stamp(self, stamp, reads, writes):
        for k in reads:
            self.buf(k).rs.append(stamp)
        for k in writes:
            b = self.buf(k)
            b.w = stamp
            b.rs = []

    def op(self, eng, fn, reads=(), writes=()):
        E = self.engs[eng]
        self._waits(E, reads, writes, skip_self=(eng == "tensor"))
        inst = fn(E.h)
        E.sem.cnt += 1
        inst.then_inc(E.sem.h, 1)
        self.ninst += 1
        self._stamp((E.sem, E.sem.cnt), reads, writes)
        return inst

    def dma(self, queue, semkey, out, in_, reads=(), writes=(), **kw):
        E = self.engs[queue]
        sem = self.dsem(semkey)
        self._waits(E, reads, writes)
        if E.seen.get(sem, 0) < sem.cnt:
            E.h.wait_ge(sem.h, sem.cnt)
            E.seen[sem] = sem.cnt
        inst = E.h.dma_start(out=out, in_=in_, **kw)
        sem.cnt += 16
        inst.then_inc(sem.h, 16)
        self.ninst += 1
        self._stamp((sem, sem.cnt), reads, writes)
        return inst

    def barrier(self):
        sems = [e.sem for e in self.engs.values()] + list(self.dsems.values())
        for E in self.engs.values():
            for s in sems:
                if s is E.sem and not STRICT:
                    continue
                if s.cnt > 0 and E.seen.get(s, 0) < s.cnt:
                    E.h.wait_ge(s.h, s.cnt)
                    E.seen[s] = s.cnt
        self.bufs = {k: v for k, v in self.bufs.items() if isinstance(k, tuple) and k[0] in KEEP}


KEEP = ("u_bf", "v_bf", "x1_d", "ssdtm_d")


def rope_tables():
    half = 32
    inv = (10000.0 ** (-np.arange(0, half, 2, dtype=np.float32) / half)).astype(np.float32)
    t = np.arange(S)
    row = (t // 64).astype(np.float32)
    col = (t % 64).astype(np.float32)
    ar = row[:, None] * inv
    ac = col[:, None] * inv
    C = np.concatenate([np.cos(ar), np.cos(ar), np.cos(ac), np.cos(ac)], axis=1).astype(np.float32)
    Sg = np.concatenate([-np.sin(ar), np.sin(ar), -np.sin(ac), np.sin(ac)], axis=1).astype(np.float32)
    return C, Sg


def build(debug=None):
    debug = debug or ()
    nc = bass.Bass("TRN2", target_bir_lowering=False)
    es = contextlib.ExitStack()

    def din(name, shape, dt=F32):
        return nc.dram_tensor(name, list(shape), dt, kind="ExternalInput").ap()

    def dscratch(name, shape, dt):
        kind = "ExternalOutput" if name in debug else "Internal"
        return nc.dram_tensor(name, list(shape), dt, kind=kind).ap()

    x = din("x", [S, D])
    c_col = din("c_col", [128, 8])
    ada_w = din("ada_w", [D, 6 * D])
    ada_bT = din("ada_bT", [128, 48])
    n1T = din("n1T", [128, 8])
    n2T = din("n2T", [128, 8])
    w_in = din("w_in", [D, IN_W])
    ident_in = din("ident", [128, 128])
    ropeC_in = din("ropeC", [S, 64])
    ropeS_in = din("ropeS", [S, 64])
    qkgain_in = din("qkgain", [1, 640])
    convwT_in = din("convwT", [128, 12 * 5])
    convbT_in = din("convbT", [128, 12])
    trif_in = din("trif", [128, 128])
    trib_in = din("trib", [128, 128])
    maskf_in = din("maskf", [128, 128])
    maskb_in = din("maskb", [128, 128])
    ssdp_in = din("ssdp", [1, 80])
    ssdnw_in = din("ssdnw", [1, 1024])
    wq_in = din("wq", [D, 2048])
    keysT_in = din("keysT", [128, 2048])
    iota128_in = din("iota128", [128, 128])
    iota16_in = din("iota16", [128, 16])
    fnw_in = din("fnw", [1, D])
    U_in = din("U_h", [128 * 128, D])
    V_in = din("V_h", [128 * 128, D])
    h2T_d = dscratch("h2T_d", [D, S], BF16)
    u_bf = dscratch("u_bf", [64 * 128, 2 * D], BF16)
    v_bf = dscratch("v_bf", [64 * 128, 2 * D], BF16)
    wau_in = din("wau", [512, D])
    wsu_in = din("wsu", [D, D])
    wout_in = din("wout", [D, D])
    x1_d = dscratch("x1_d", [S, D], F32)
    attT_d = dscratch("attT_d", [512, S], BF16)
    xstm_d = dscratch("xstm_d", [S, 1024], BF16)
    yb_d = dscratch("yb_d", [S, 1024], F32)
    ssdtm_d = dscratch("ssdtm_d", [S, 1024], BF16)
    zs_d = dscratch("zs_d", [S, 1024], BF16)
    xbcT_d = dscratch("xbcT_d", [1536, S], BF16)
    gatesT_d = dscratch("gatesT_d", [2048, S], BF16)
    out = nc.dram_tensor("out", [S, D], F32, kind="ExternalOutput").ap()
    dbg_out = {}
    for name, shape, dt in [("d_modT", [128, 48], F32), ("d_hT", [128, 8 * S], BF16),
                            ("d_QT", [128, 4 * S], BF16), ("d_KT", [128, S], BF16), ("d_V", [128, NT * 130], BF16),
                            ("d_dtraw", [128, NT * 32], F32),
                            ("d_attm", [128, NT * 512], BF16),
                            ("d_ssdT", [128, 8 * S], BF16), ("d_h2T", [128, 8 * S], BF16), ("d_i1T", [128, S], BF16),
                            ("d_i2T", [128, S], BF16), ("d_gT", [128, S], BF16)]:
        if name in debug:
            dbg_out[name] = nc.dram_tensor(name, shape, dt, kind="ExternalOutput").ap()

    with es:
        cx = Ctx(nc, es)

        def sb(name, shape, dt):
            return es.enter_context(nc.sbuf_tensor("s_" + name, list(shape), dt))

        ident = sb("ident", [128, 128], F32)
        identb = sb("identb", [128, 128], BF16)
        ones_f = sb("ones_f", [128, 128], F32)
        cact = sb("cact", [128, 8], F32)
        modT = sb("modT", [128, 48], F32)
        abT = sb("abT", [128, 48], F32)
        n1 = sb("n1", [128, 8], F32)
        n2 = sb("n2", [128, 8], F32)
        gam1 = sb("gam1", [128, 8], F32)
        gam2 = sb("gam2", [128, 8], F32)
        hT = sb("hT", [128, 8, S], BF16)
        dtraw = sb("dtraw", [128, NT, 32], F32)
        mid = contextlib.ExitStack()

        def sbm(name, shape, dt):
            return mid.enter_context(nc.sbuf_tensor("m_" + name, list(shape), dt))
        QT = sbm("QT", [128, 4, S], BF16)
        KT = sbm("KT", [128, S], BF16)
        Vsb = sbm("Vsb", [128, NT, 2, 65], BF16)
        ropeC = sbm("ropeC", [128, NT, 64], F32)
        ropeS = sbm("ropeS", [128, NT, 64], F32)
        qkgain = sbm("qkgain", [128, 640], F32)
        convwT = sbm("convwT", [128, 12, 5], F32)
        convbT = sbm("convbT", [128, 12], F32)

        cx.dma("sync", "c0", ident[:], ident_in[:, :], writes=["ident"])
        cx.dma("sync", "c1", cact[:], c_col[:, :], writes=["cact"])
        cx.dma("sync", "c2", abT[:], ada_bT[:, :], writes=["abT"])
        cx.dma("sync", "c3", n1[:], n1T[:, :], writes=["n1"])
        cx.dma("sync", "c4", n2[:], n2T[:, :], writes=["n2"])
        cx.dma("sync", "c5", ropeC[:], ropeC_in.rearrange("(i p) j -> p i j", p=128), writes=["ropeC"])
        cx.dma("sync", "c6", ropeS[:], ropeS_in.rearrange("(i p) j -> p i j", p=128), writes=["ropeS"])
        cx.dma("sync", "c7", qkgain[:], qkgain_in.partition_broadcast(128), writes=["qkgain"])
        cx.dma("sync", "c8", convwT[:].rearrange("p a b -> p (a b)"), convwT_in[:, :], writes=["convwT"])
        cx.dma("sync", "c9", convbT[:], convbT_in[:, :], writes=["convbT"])
        cx.op("vector", lambda e: e.tensor_scalar(out=qkgain[:, 0:512], in0=qkgain[:, 0:512], scalar1=0.125, scalar2=None,
                                                  op0=ALU.mult), reads=["qkgain"], writes=["qkgain"])
        cx.op("vector", lambda e: e.memset(Vsb[:].rearrange("p a b c -> p (a b c)"), 1.0), writes=["Vsb"])
        cx.op("vector", lambda e: e.tensor_copy(out=identb[:], in_=ident[:]), reads=["ident"], writes=["identb"])
        cx.op("vector", lambda e: e.memset(ones_f[:], 1.0), writes=["ones_f"])
        cx.op("scalar", lambda e: e.activation(out=cact[:], in_=cact[:], func=AF.Silu), reads=["cact"], writes=["cact"])

        with contextlib.ExitStack() as st:
            aw = [st.enter_context(nc.sbuf_tensor("aw%d" % i, [128, 8, 512], F32)) for i in range(2)]
            psm = st.enter_context(nc.psum_tensor("psm", [128, 48], F32))
            awv = ada_w.rearrange("(kc p) n -> p kc n", p=128)
            for blk in range(12):
                t = aw[blk % 2]
                key = "aw%d" % (blk % 2)
                cx.dma("sync", key, t[:], awv[:, :, blk * 512:(blk + 1) * 512], writes=[key])
                for jj in range(4):
                    j = blk * 4 + jj
                    for kc in range(8):
                        cx.op("tensor", lambda e, t=t, jj=jj, kc=kc, j=j: e.matmul(
                            psm[:, j:j + 1], lhsT=t[:, kc, jj * 128:(jj + 1) * 128], rhs=cact[:, kc:kc + 1],
                            start=(kc == 0), stop=(kc == 7)), reads=[key, "cact"], writes=["psm"])
            cx.op("vector", lambda e: e.tensor_tensor(out=modT[:], in0=psm[:], in1=abT[:], op=ALU.add),
                  reads=["psm", "abT"], writes=["modT"])
            cx.op("vector", lambda e: e.scalar_tensor_tensor(out=gam1[:], in0=modT[:, 8:16], scalar=1.0, in1=n1[:],
                                                             op0=ALU.add, op1=ALU.mult),
                  reads=["modT", "n1"], writes=["gam1"])
            cx.op("vector", lambda e: e.scalar_tensor_tensor(out=gam2[:], in0=modT[:, 32:40], scalar=1.0, in1=n2[:],
                                                             op0=ALU.add, op1=ALU.mult),
                  reads=["modT", "n2"], writes=["gam2"])
            if "d_modT" in debug:
                cx.dma("sync", "dbg", dbg_out["d_modT"][:, :], modT[:], reads=["modT"])
            s0_scope = st.pop_all()

        def norm_mod_transpose(src, gam, shT, st, pfx, rkey=None, gkeys=()):
            xt = [st.enter_context(nc.sbuf_tensor(pfx + "xt%d" % i, [128, D], F32)) for i in range(2)]
            xn = [st.enter_context(nc.sbuf_tensor(pfx + "xn%d" % i, [128, D], BF16)) for i in range(2)]
            junk = st.enter_context(nc.sbuf_tensor(pfx + "junk", [128, D], BF16))
            ssq = st.enter_context(nc.sbuf_tensor(pfx + "ssq", [128, NT], F32))
            rstd = st.enter_context(nc.sbuf_tensor(pfx + "rstd", [128, NT], F32))
            pst = [st.enter_context(nc.psum_tensor(pfx + "pst%d" % i, [128, 8, 128], BF16)) for i in range(2)]
            for i in range(NT):
                p = i % 2
                kx, kn, kp = pfx + "xt%d" % p, pfx + "xn%d" % p, pfx + "pst%d" % p
                cx.dma("sync", kx, xt[p][:], src[i * 128:(i + 1) * 128, :], reads=([(rkey, i)] if rkey else []), writes=[kx])
                cx.op("scalar", lambda e, p=p, i=i: e.activation(out=junk[:], in_=xt[p][:], func=AF.Square,
                                                                 accum_out=ssq[:, i:i + 1]),
                      reads=[kx], writes=[pfx + "junk", (pfx + "ssq", i)])
                cx.op("vector", lambda e, i=i: e.tensor_scalar(out=rstd[:, i:i + 1], in0=ssq[:, i:i + 1],
                                                               scalar1=1.0 / D, scalar2=EPS, op0=ALU.mult, op1=ALU.add),
                      reads=[(pfx + "ssq", i)], writes=[(pfx + "rstd", i)])
                cx.op("scalar", lambda e, i=i: e.activation(out=rstd[:, i:i + 1], in_=rstd[:, i:i + 1], func=AF.Sqrt),
                      reads=[(pfx + "rstd", i)], writes=[(pfx + "rstd", i)])
                cx.op("vector", lambda e, i=i: e.reciprocal(out=rstd[:, i:i + 1], in_=rstd[:, i:i + 1]),
                      reads=[(pfx + "rstd", i)], writes=[(pfx + "rstd", i)])
                cx.op("vector", lambda e, p=p, i=i: e.tensor_scalar(out=xn[p][:], in0=xt[p][:], scalar1=rstd[:, i:i + 1],
                                                                    scalar2=None, op0=ALU.mult),
                      reads=[kx, (pfx + "rstd", i)], writes=[kn])
                for c in range(8):
                    cx.op("tensor", lambda e, p=p, c=c: e.transpose(out=pst[p][:, c, :], in_=xn[p][:, c * 128:(c + 1) * 128],
                                                                    identity=identb[:]),
                          reads=[kn, "identb"], writes=[kp])
                for c in range(8):
                    cx.op("scalar", lambda e, p=p, c=c, i=i: e.activation(
                        out=hT[:, c, i * 128:(i + 1) * 128], in_=pst[p][:, c, :], func=AF.Identity,
                        scale=gam[:, c:c + 1], bias=shT[:, c:c + 1]),
                        reads=[kp] + list(gkeys), writes=[("hT", i)])

        with contextlib.ExitStack() as st:
            norm_mod_transpose(x, gam1, modT[:, 0:8], st, "a", gkeys=["gam1", "modT"])
            if "d_hT" in debug:
                cx.dma("sync", "dbg", dbg_out["d_hT"][:, :], hT[:].rearrange("p c s -> p (c s)"),
                       reads=[("hT", i) for i in range(NT)])
            cx.barrier()
        s0_scope.close()


        wv = w_in.rearrange("(kc p) n -> p kc n", p=128)
        with contextlib.ExitStack() as st:
            wA = st.enter_context(nc.sbuf_tensor("wA", [128, 8, 800], BF16))
            wZ = st.enter_context(nc.sbuf_tensor("wZ", [128, 8, 1024], BF16))
            psA = [st.enter_context(nc.psum_tensor("psA%d" % i, [128, 1024], F32)) for i in range(2)]
            psZ = st.enter_context(nc.psum_tensor("psZ", [128, 1024], F32))
            pT = st.enter_context(nc.psum_tensor("pT", [128, 5, 128], BF16))
            qkv = [st.enter_context(nc.sbuf_tensor("qkv%d" % i, [128, 800], F32)) for i in range(2)]
            sq = st.enter_context(nc.sbuf_tensor("sq", [128, 640], F32))
            qn = st.enter_context(nc.sbuf_tensor("qn", [128, 640], F32))
            qa = st.enter_context(nc.sbuf_tensor("qa", [128, 640], F32))
            qb = st.enter_context(nc.sbuf_tensor("qb", [128, 640], F32))
            qr = st.enter_context(nc.sbuf_tensor("qr", [128, 640], BF16))
            ss = st.enter_context(nc.sbuf_tensor("ss10", [128, 10], F32))
            zst = [st.enter_context(nc.sbuf_tensor("zst%d" % i, [128, 1024], BF16)) for i in range(2)]
            cx.dma("gpsimd", "wA", wA[:], wv[:, :, 0:800], writes=["wA"])
            cx.dma("gpsimd", "wZ", wZ[:], wv[:, :, 800:1824], writes=["wZ"])
            for i in range(NT):
                p = i % 2
                tsl = slice(i * 128, (i + 1) * 128)
                kA, kq = "psA%d" % p, "qkv%d" % p
                for (c0, c1) in [(0, 512), (512, 800)]:
                    for kc in range(8):
                        cx.op("tensor", lambda e, p=p, c0=c0, c1=c1, kc=kc: e.matmul(
                            psA[p][:, c0:c1], lhsT=hT[:, kc, tsl], rhs=wA[:, kc, c0:c1], start=(kc == 0), stop=(kc == 7)),
                            reads=[("hT", i), "wA"], writes=[kA])
                for (c0, c1) in [(0, 512), (512, 1024)]:
                    for kc in range(8):
                        cx.op("tensor", lambda e, c0=c0, c1=c1, kc=kc: e.matmul(
                            psZ[:, c0:c1], lhsT=hT[:, kc, tsl], rhs=wZ[:, kc, c0:c1], start=(kc == 0), stop=(kc == 7)),
                            reads=[("hT", i), "wZ"], writes=["psZ"])
                cx.op("scalar", lambda e, p=p: e.activation(out=qkv[p][:], in_=psA[p][:, 0:800], func=AF.Copy),
                      reads=[kA], writes=[kq])
                kz = "zst%d" % p
                cx.op("scalar", lambda e, p=p: e.activation(out=zst[p][:], in_=psZ[:], func=AF.Silu),
                      reads=["psZ"], writes=[kz])
                cx.dma("sync", kz, zs_d[tsl, :], zst[p][:], reads=[kz], writes=[("zs_d", i)])
                Q = qkv[p]
                cx.op("vector", lambda e, Q=Q: e.tensor_tensor(out=sq[:], in0=Q[:, 0:640], in1=Q[:, 0:640], op=ALU.mult),
                      reads=[kq], writes=["sq"])
                cx.op("vector", lambda e: e.tensor_reduce(out=ss[:], in_=sq[:].rearrange("p (h d) -> p h d", d=64),
                                                          axis=AX.X, op=ALU.add), reads=["sq"], writes=["ss10"])
                cx.op("vector", lambda e: e.tensor_scalar(out=ss[:], in0=ss[:], scalar1=1.0 / 64, scalar2=EPS,
                                                          op0=ALU.mult, op1=ALU.add), reads=["ss10"], writes=["ss10"])
                cx.op("scalar", lambda e: e.activation(out=ss[:], in_=ss[:], func=AF.Sqrt), reads=["ss10"], writes=["ss10"])
                cx.op("vector", lambda e: e.reciprocal(out=ss[:], in_=ss[:]), reads=["ss10"], writes=["ss10"])
                cx.op("vector", lambda e, Q=Q: e.tensor_tensor(
                    out=qn[:].rearrange("p (h d) -> p h d", d=64), in0=Q[:, 0:640].rearrange("p (h d) -> p h d", d=64),
                    in1=ss[:].unsqueeze(2).to_broadcast([128, 10, 64]), op=ALU.mult), reads=[kq, "ss10"], writes=["qn"])
                cx.op("vector", lambda e: e.tensor_tensor(out=qn[:], in0=qn[:], in1=qkgain[:], op=ALU.mult),
                      reads=["qn", "qkgain"], writes=["qn"])
                cx.op("vector", lambda e, i=i: e.tensor_tensor(
                    out=qa[:].rearrange("p (h d) -> p h d", d=64), in0=qn[:].rearrange("p (h d) -> p h d", d=64),
                    in1=ropeC[:, i, :].unsqueeze(1).to_broadcast([128, 10, 64]), op=ALU.mult),
                    reads=["qn", "ropeC"], writes=["qa"])
                for blk in range(2):
                    for hh in range(2):
                        o0 = blk * 32 + hh * 16
                        i0 = blk * 32 + (1 - hh) * 16
                        cx.op("vector", lambda e, i=i, o0=o0, i0=i0: e.tensor_tensor(
                            out=qb[:].rearrange("p (h d) -> p h d", d=64)[:, :, o0:o0 + 16],
                            in0=qn[:].rearrange("p (h d) -> p h d", d=64)[:, :, i0:i0 + 16],
                            in1=ropeS[:, i, o0:o0 + 16].unsqueeze(1).to_broadcast([128, 10, 16]), op=ALU.mult),
                            reads=["qn", "ropeS"], writes=["qb"])
                cx.op("vector", lambda e: e.tensor_tensor(out=qr[:], in0=qa[:], in1=qb[:], op=ALU.add),
                      reads=["qa", "qb"], writes=["qr"])
                for j in range(5):
                    cx.op("tensor", lambda e, j=j: e.transpose(out=pT[:, j, :], in_=qr[:, j * 128:(j + 1) * 128],
                                                               identity=identb[:]), reads=["qr", "identb"], writes=["pT"])
                cx.op("scalar", lambda e: e.activation(out=QT[:, :, tsl], in_=pT[:, 0:4, :], func=AF.Copy),
                      reads=["pT"], writes=[("QT", i)])
                cx.op("scalar", lambda e: e.activation(out=KT[:, tsl], in_=pT[:, 4, :], func=AF.Copy),
                      reads=["pT"], writes=[("KT", i)])
                cx.op("vector", lambda e, Q=Q, i=i: e.tensor_copy(
                    out=Vsb[:, i, :, 0:64], in_=Q[:, 640:768].rearrange("p (h d) -> p h d", d=64)),
                    reads=[kq, "Vsb"], writes=[("Vsb", i)])
                cx.op("vector", lambda e, Q=Q, i=i: e.tensor_copy(out=dtraw[:, i, :], in_=Q[:, 768:800]),
                      reads=[kq], writes=[("dtraw", i)])
            cx.barrier()
        with contextlib.ExitStack() as st:
            wF = [st.enter_context(nc.sbuf_tensor("wF%d" % i, [128, 8, 512], BF16)) for i in range(2)]
            psF = [st.enter_context(nc.psum_tensor("psF%d" % i, [128, 512], F32)) for i in range(4)]
            stage = st.enter_context(nc.sbuf_tensor("stage", [128, S + 4], F32))
            acc = st.enter_context(nc.sbuf_tensor("acc", [128, S], F32))
            ob = [st.enter_context(nc.sbuf_tensor("ob%d" % i, [128, S], BF16)) for i in range(2)]
            cx.op("vector", lambda e: e.memset(stage[:], 0.0), writes=["stage"])
            n = 0
            for j in range(28):
                isx = j < 12
                jj = j if isx else j - 12
                bb = j // 4
                W = wF[bb % 2]
                kW = "wF%d" % (bb % 2)
                if j % 4 == 0:
                    cx.dma("gpsimd", kW, W[:], wv[:, :, 1824 + bb * 512:1824 + (bb + 1) * 512], writes=[kW])
                jw = j % 4
                o = ob[j % 2]
                ko = "ob%d" % (j % 2)
                for g in range(8):
                    b = n % 4
                    n += 1
                    gs = slice(g * 512, (g + 1) * 512)
                    for kc in range(8):
                        cx.op("tensor", lambda e, b=b, kc=kc, W=W, jw=jw, gs=gs: e.matmul(
                            psF[b][:], lhsT=W[:, kc, jw * 128:(jw + 1) * 128], rhs=hT[:, kc, gs],
                            start=(kc == 0), stop=(kc == 7)),
                            reads=["hTall", kW], writes=["psF%d" % b])
                    if isx:
                        cx.op("scalar", lambda e, b=b, g=g: e.activation(out=stage[:, 2 + g * 512:2 + (g + 1) * 512],
                                                                         in_=psF[b][:], func=AF.Copy),
                              reads=["psF%d" % b, "acc0", "stage"], writes=[("stage", g)])
                    else:
                        cx.op("scalar", lambda e, b=b, gs=gs, o=o: e.activation(out=o[:, gs], in_=psF[b][:], func=AF.Sigmoid),
                              reads=["psF%d" % b], writes=[ko])
                if isx:
                    srd = [("stage", g) for g in range(8)] + ["stage"]
                    cx.op("vector", lambda e, jj=jj: e.tensor_scalar(
                        out=acc[:], in0=stage[:, 0:S], scalar1=convwT[:, jj, 0:1], scalar2=convbT[:, jj:jj + 1],
                        op0=ALU.mult, op1=ALU.add), reads=srd + ["convwT", "convbT"], writes=["acc"])
                    for k in range(1, 5):
                        cx.op("vector", lambda e, jj=jj, k=k: e.scalar_tensor_tensor(
                            out=acc[:], in0=stage[:, k:k + S], scalar=convwT[:, jj, k:k + 1], in1=acc[:],
                            op0=ALU.mult, op1=ALU.add), reads=srd + ["acc"], writes=["acc"] + (["acc0"] if k == 4 else []))
                    cx.op("scalar", lambda e, o=o: e.activation(out=o[:], in_=acc[:], func=AF.Silu),
                          reads=["acc"], writes=[ko])
                    cx.dma("sync", ko, xbcT_d[jj * 128:(jj + 1) * 128, :], o[:], reads=[ko], writes=[("xbcT_d", jj)])
                else:
                    cx.dma("sync", ko, gatesT_d[jj * 128:(jj + 1) * 128, :], o[:], reads=[ko], writes=[("gatesT_d", jj)])
            for nm, t in [("d_QT", QT), ("d_KT", KT), ("d_V", Vsb), ("d_dtraw", dtraw)]:
                if nm in debug:
                    flat = t[:] if len(t.shape) == 2 else (
                        t[:].rearrange("p a b -> p (a b)") if len(t.shape) == 3 else t[:].rearrange("p a b c -> p (a b c)"))
                    cx.dma("sync", "dbg", dbg_out[nm][:, :], flat)
            cx.barrier()


        with contextlib.ExitStack() as st:
            psS = [st.enter_context(nc.psum_tensor("psS%d" % i, [128, 512], F32)) for i in range(4)]
            psO = [st.enter_context(nc.psum_tensor("psO%d" % i, [128, 512], F32)) for i in range(2)]
            pTr = st.enter_context(nc.psum_tensor("pTr", [128, 4, 65], F32))
            PT = [st.enter_context(nc.sbuf_tensor("PT%d" % i, [128, 512], BF16)) for i in range(4)]
            oT = [st.enter_context(nc.sbuf_tensor("oT%d" % i, [128, 512], F32)) for i in range(2)]
            attm = st.enter_context(nc.sbuf_tensor("attm", [128, NT, 512], BF16))
            rden = st.enter_context(nc.sbuf_tensor("rden", [128, 4], F32))
            pTa = st.enter_context(nc.psum_tensor("pTa", [128, 4, 128], BF16))
            attT = hT
            cu = [st.enter_context(nc.sbuf_tensor("cu%d" % i, [128, 2, D], BF16)) for i in range(2)]
            cv = [st.enter_context(nc.sbuf_tensor("cv%d" % i, [128, 2, D], BF16)) for i in range(2)]
            for jj in range(64):
                p = jj % 2
                rs = slice(jj * 256, (jj + 1) * 256)
                cx.dma("gpsimd", "cu%d" % p, cu[p][:], U_in[rs, :].rearrange("(a p) n -> p a n", p=128), writes=["cu%d" % p])
                cx.dma("sync", "cuo%d" % p, u_bf[jj * 128:(jj + 1) * 128, :], cu[p][:].rearrange("p a n -> p (a n)"), reads=["cu%d" % p],
                       writes=[("u_bf", 2 * jj), ("u_bf", 2 * jj + 1)])
                cx.dma("gpsimd", "cv%d" % p, cv[p][:], V_in[rs, :].rearrange("(a p) n -> p a n", p=128), writes=["cv%d" % p])
                cx.dma("sync", "cvo%d" % p, v_bf[jj * 128:(jj + 1) * 128, :], cv[p][:].rearrange("p a n -> p (a n)"), reads=["cv%d" % p],
                       writes=[("v_bf", 2 * jj), ("v_bf", 2 * jj + 1)])
            steps = [(j, qg, kt) for j in range(4) for qg in range(8) for kt in range(NT)]

            def emit_qk(n):
                j, qg, kt = steps[n]
                for grp in range(2):
                    b = 2 * (n % 2) + grp
                    ps_ = slice(grp * 64, (grp + 1) * 64)
                    cx.op("tensor", lambda e, b=b, ps_=ps_: e.matmul(
                        psS[b][:], lhsT=KT[ps_, kt * 128:(kt + 1) * 128], rhs=QT[ps_, j, qg * 512:(qg + 1) * 512],
                        start=True, stop=True), reads=["QT", "KT"], writes=["psS%d" % b])

            emit_qk(0)
            for n, (j, qg, kt) in enumerate(steps):
                if n + 1 < len(steps):
                    emit_qk(n + 1)
                for grp in range(2):
                    b = 2 * (n % 2) + grp
                    cx.op("scalar", lambda e, b=b: e.activation(out=PT[b][:], in_=psS[b][:], func=AF.Exp),
                          reads=["psS%d" % b], writes=["PT%d" % b])
                for grp in range(2):
                    b = 2 * (n % 2) + grp
                    cx.op("tensor", lambda e, b=b, kt=kt, grp=grp: e.matmul(
                        psO[grp][0:65, :], lhsT=Vsb[:, kt, grp, :], rhs=PT[b][:],
                        start=(kt == 0), stop=(kt == NT - 1)), reads=["PT%d" % b, "Vsb"], writes=["psO%d" % grp])
                if kt == NT - 1:
                    for grp in range(2):
                        pos = 2 * j + grp
                        cx.op("scalar", lambda e, grp=grp: e.activation(out=oT[grp][0:65, :], in_=psO[grp][0:65, :], func=AF.Copy),
                              reads=["psO%d" % grp], writes=["oT%d" % grp])
                        for sub in range(4):
                            cx.op("tensor", lambda e, grp=grp, sub=sub: e.transpose(
                                out=pTr[:, sub, :], in_=oT[grp][0:65, sub * 128:(sub + 1) * 128], identity=ident[0:65, 0:65]),
                                reads=["oT%d" % grp, "ident"], writes=["pTr"])
                        cx.op("vector", lambda e: e.reciprocal(out=rden[:].unsqueeze(2), in_=pTr[:, :, 64:65]),
                              reads=["pTr"], writes=["rden"])
                        cx.op("vector", lambda e, qg=qg, pos=pos: e.tensor_tensor(
                            out=attm[:, qg * 4:(qg + 1) * 4, pos * 64:(pos + 1) * 64], in0=pTr[:, :, 0:64],
                            in1=rden[:].unsqueeze(2).to_broadcast([128, 4, 64]), op=ALU.mult),
                            reads=["pTr", "rden"], writes=[("attm", qg * 4 + k) for k in range(4)])
            if "d_attm" in debug:
                cx.dma("sync", "dbg", dbg_out["d_attm"][:, :], attm[:].rearrange("p a b -> p (a b)"),
                       reads=[("attm", ti) for ti in range(NT)])
            for i in range(NT):
                for c in range(4):
                    cx.op("tensor", lambda e, i=i, c=c: e.transpose(out=pTa[:, c, :], in_=attm[:, i, c * 128:(c + 1) * 128],
                                                                    identity=identb[:]), reads=[("attm", i)], writes=["pTa"])
                cx.op("scalar", lambda e, i=i: e.activation(out=attT[:, 0:4, i * 128:(i + 1) * 128], in_=pTa[:], func=AF.Copy),
                      reads=["pTa"], writes=[("attT", i)])
            for c in range(4):
                cx.dma("sync", "attTd", attT_d[c * 128:(c + 1) * 128, :], attT[:, c, :],
                       reads=[("attT", i) for i in range(NT)], writes=["attT_d"])
            cx.barrier()
        mid.close()


        ssdT = hT
        with contextlib.ExitStack() as st:
            def T(name, shape, dt):
                return st.enter_context(nc.sbuf_tensor("y_" + name, list(shape), dt))
            trif = T("trif", [128, 128], F32); trib = T("trib", [128, 128], F32)
            ssdp = T("ssdp", [128, 80], F32)
            ssdnw = T("ssdnw", [128, 1024], F32)
            dtv = T("dtv", [128, NT, 32], F32)
            Adt = T("Adt", [128, NT, 32], F32)
            BT = T("BT", [128, 2, S], BF16)
            CT = T("CT", [128, 2, S], BF16)
            Btm = T("Btm", [128, NT, 2, 128], BF16)
            cx.dma("sync", "k0", trif[:], trif_in[:, :], writes=["trif"])
            cx.dma("sync", "k1", trib[:], trib_in[:, :], writes=["trib"])
            cx.dma("sync", "k4", ssdp[:], ssdp_in.partition_broadcast(128), writes=["ssdp"])
            cx.dma("sync", "k5", ssdnw[:], ssdnw_in.partition_broadcast(128), writes=["ssdnw"])
            for g in range(2):
                cx.dma("sync", "k6", BT[:, g, :], xbcT_d[1024 + g * 128:1024 + (g + 1) * 128, :], writes=["BT"])
                cx.dma("sync", "k7", CT[:, g, :], xbcT_d[1280 + g * 128:1280 + (g + 1) * 128, :], writes=["CT"])
            cx.op("vector", lambda e: e.tensor_tensor(out=dtv[:], in0=dtraw[:], in1=ssdp[:, 0:32].unsqueeze(1).to_broadcast([128, NT, 32]),
                                                      op=ALU.add), reads=["ssdp"], writes=["dtv"])
            cx.op("scalar", lambda e: e.activation(out=dtv[:], in_=dtv[:], func=AF.Exp), reads=["dtv"], writes=["dtv"])
            cx.op("scalar", lambda e: e.activation(out=dtv[:], in_=dtv[:], func=AF.Ln, bias=1.0), reads=["dtv"], writes=["dtv"])
            cx.op("scalar", lambda e: e.activation(out=ssdp[:, 32:64], in_=ssdp[:, 32:64], func=AF.Exp), reads=["ssdp"], writes=["ssdp"])
            cx.op("vector", lambda e: e.scalar_tensor_tensor(out=Adt[:], in0=dtv[:], scalar=-1.0,
                                                             in1=ssdp[:, 32:64].unsqueeze(1).to_broadcast([128, NT, 32]),
                                                             op0=ALU.mult, op1=ALU.mult), reads=["dtv", "ssdp"], writes=["Adt"])
            with contextlib.ExitStack() as st2:
                pT4 = [st2.enter_context(nc.psum_tensor("pT4%d" % i, [128, 4, 128], BF16)) for i in range(2)]
                xch = [st2.enter_context(nc.sbuf_tensor("y_xch%d" % i, [128, S], BF16)) for i in range(2)]
                xst = [st2.enter_context(nc.sbuf_tensor("y_xst%d" % i, [128, 1024], BF16)) for i in range(2)]
                n = 0
                for g in range(2):
                    for i0 in range(0, NT, 4):
                        b = n % 2; n += 1
                        for k in range(4):
                            cx.op("tensor", lambda e, b=b, k=k, g=g, i0=i0: e.transpose(
                                out=pT4[b][:, k, :], in_=BT[:, g, (i0 + k) * 128:(i0 + k + 1) * 128], identity=identb[:]),
                                reads=["BT"], writes=["pT4%d" % b])
                        cx.op("scalar", lambda e, b=b, g=g, i0=i0: e.activation(out=Btm[:, i0:i0 + 4, g, :], in_=pT4[b][:], func=AF.Copy),
                              reads=["pT4%d" % b], writes=["Btm"])
                for i in range(NT):
                    cx.buf(("xstm", i))
                for c in range(8):
                    X = xch[c % 2]; kx = "xch%d" % (c % 2)
                    cx.dma("sync", kx, X[:], xbcT_d[c * 128:(c + 1) * 128, :], writes=[kx])
                    for i0 in range(0, NT, 4):
                        b = n % 2; n += 1
                        for k in range(4):
                            cx.op("tensor", lambda e, b=b, k=k, X=X, i0=i0: e.transpose(
                                out=pT4[b][:, k, :], in_=X[:, (i0 + k) * 128:(i0 + k + 1) * 128], identity=identb[:]),
                                reads=[kx], writes=["pT4%d" % b])
                        q = (i0 // 4) % 2
                        cx.op("scalar", lambda e, b=b, q=q: e.activation(out=xst[q][:, 0:512].rearrange("p (a b) -> p a b", a=4),
                                                                         in_=pT4[b][:], func=AF.Copy),
                              reads=["pT4%d" % b], writes=["xst%d" % q])
                        cx.dma("sync", "xst%d" % q,
                               xstm_d[i0 * 128:(i0 + 4) * 128, c * 128:(c + 1) * 128].rearrange("(a p) n -> p a n", p=128),
                               xst[q][:, 0:512].rearrange("p (a b) -> p a b", a=4),
                               reads=["xst%d" % q], writes=[("xstm", i0 + k) for k in range(4)])
                cx.barrier()

            psCBA = st.enter_context(nc.psum_tensor("psCBA", [128, 3, 128], F32))
            psBC = [st.enter_context(nc.psum_tensor("psBC%d" % g, [128, 8, 128], F32)) for g in range(2)]
            psY = [st.enter_context(nc.psum_tensor("psY%d" % g, [128, 512], F32)) for g in range(2)]
            psYS = st.enter_context(nc.psum_tensor("psYS", [128, 512], F32))
            psAcs = psCBA[:, 2, 0:16]
            Acs = T("Acs", [128, 16], F32)
            expA = T("expA", [128, 16], F32)
            rhs2 = [T("rhs2%d" % g, [128, 8, 128], F32) for g in range(2)]
            d1 = [T("d1%d" % g, [128, 8, 128], F32) for g in range(2)]
            LT = [T("LT%d" % g, [128, 8, 128], F32) for g in range(2)]
            Mb = [T("Mb%d" % g, [128, 8, 128], BF16) for g in range(2)]
            cbm = [T("cbm%d" % g, [128, 128], F32) for g in range(2)]
            Xb = T("Xb", [128, 1024], BF16)
            Xd = [T("Xd%d" % g, [128, 512], BF16) for g in range(2)]
            dec = [T("dec%d" % g, [128, 8], F32) for g in range(2)]
            cd = [T("cd%d" % g, [128, 8], F32) for g in range(2)]
            tmpy = [T("tmpy%d" % g, [128, 512], F32) for g in range(2)]
            ydir = [T("ydir%d" % i, [128, 1024], F32) for i in range(2)]
            state = [T("state%d" % g, [128, 512], F32) for g in range(2)]
            stbf = [T("stbf%d" % g, [128, 512], BF16) for g in range(2)]
            xs_t = [T("xs_t%d" % i, [128, 1024], BF16) for i in range(2)]
            zs_t = [T("zs_t%d" % i, [128, 1024], BF16) for i in range(2)]
            yb_t = [T("yb_t%d" % i, [128, 1024], F32) for i in range(2)]
            gg = T("gg", [128, 1024], F32)
            junk2 = T("junk2", [128, 1024], BF16)
            ssq = T("ssq1", [128, 1], F32)
            ssdtm = [T("ssdtm0", [128, 1024], BF16)] * 2

            def ssd_pass(fwd):
                o = 0 if fwd else 16
                tri = trif if fwd else trib
                ktri = "trif" if fwd else "trib"
                lend = 127 if fwd else 0
                order = list(range(NT)) if fwd else list(range(NT - 1, -1, -1))
                for idx, i in enumerate(order):
                    p = idx % 2
                    tsl = slice(i * 128, (i + 1) * 128)
                    kxs = "xs_t%d" % p
                    cx.dma("sync", kxs, xs_t[p][:], xstm_d[tsl, :], reads=[("xstm", i)], writes=[kxs])
                    if fwd:
                        cx.dma("sync", "zs_t%d" % p, zs_t[p][:], zs_d[tsl, :], writes=["zs_t%d" % p])
                        cx.dma("sync", "yb_t%d" % p, yb_t[p][:], yb_d[tsl, :], reads=[("yb_d", i)], writes=["yb_t%d" % p])
                    cx.op("tensor", lambda e, i=i: e.matmul(psAcs, lhsT=tri[:], rhs=Adt[:, i, o:o + 16], start=True, stop=True),
                          reads=[ktri, "Adt"], writes=["psAcs"])
                    cx.op("vector", lambda e: e.tensor_copy(out=Acs[:], in_=psAcs), reads=["psAcs"], writes=["Acs"])
                    cx.op("scalar", lambda e: e.activation(out=expA[:], in_=psAcs, func=AF.Exp), reads=["psAcs"], writes=["expA"])
                    cx.op("vector", lambda e, p=p, i=i: e.tensor_tensor(
                        out=Xb[:].rearrange("p (h d) -> p h d", d=64), in0=xs_t[p][:].rearrange("p (h d) -> p h d", d=64),
                        in1=dtv[:, i, o:o + 16].unsqueeze(2).to_broadcast([128, 16, 64]), op=ALU.mult),
                        reads=[kxs, "dtv"], writes=["Xb"])
                    yd = ydir[p]; kyd = "ydir%d" % p
                    for g in range(2):
                        hs = slice(g * 8, (g + 1) * 8)
                        G = str(g)
                        cx.op("vector", lambda e, i=i, g=g: e.tensor_tensor(
                            out=rhs2[g][:], in0=tri[:].unsqueeze(1).to_broadcast([128, 8, 128]),
                            in1=Adt[:, i, o + g * 8:o + g * 8 + 8].unsqueeze(2).to_broadcast([128, 8, 128]), op=ALU.mult),
                            reads=[ktri, "Adt"], writes=["rhs2" + G])
                        for hh in range(2):
                            cx.op("tensor", lambda e, hh=hh, g=g: e.matmul(psBC[g][:, hh * 4:(hh + 1) * 4, :], lhsT=ones_f[:],
                                                                           rhs=rhs2[g][:, hh * 4:(hh + 1) * 4, :], start=True, stop=True),
                                  reads=["rhs2" + G, "ones_f"], writes=["psBC" + G])
                        cx.op("tensor", lambda e, g=g: e.matmul(psCBA[:, g, :], lhsT=BT[:, g, tsl], rhs=CT[:, g, tsl], start=True, stop=True),
                              reads=["BT", "CT"], writes=["psCB" + G])
                    for g in range(2):
                        hs = slice(g * 8, (g + 1) * 8)
                        G = str(g)
                        cx.op("vector", lambda e, g=g: e.tensor_tensor(out=cbm[g][:], in0=psCBA[:, g, :], in1=tri[:], op=ALU.mult),
                              reads=["psCB" + G, ktri], writes=["cbm" + G])
                        cx.op("vector", lambda e, hs=hs, g=g: e.tensor_tensor(
                            out=d1[g][:], in0=psBC[g][:], in1=Acs[:, hs].unsqueeze(2).to_broadcast([128, 8, 128]), op=ALU.subtract),
                            reads=["psBC" + G, "Acs"], writes=["d1" + G])
                        cx.op("scalar", lambda e, g=g: e.activation(out=LT[g][:], in_=d1[g][:], func=AF.Exp), reads=["d1" + G], writes=["LT" + G])
                        if idx < NT - 1:
                            cx.op("vector", lambda e, hs=hs, g=g: e.tensor_tensor(
                                out=dec[g][:].unsqueeze(2), in0=psBC[g][:, :, lend:lend + 1], in1=Acs[:, hs].unsqueeze(2), op=ALU.subtract),
                                reads=["psBC" + G, "Acs"], writes=["dec" + G])
                            cx.op("scalar", lambda e, g=g: e.activation(out=dec[g][:], in_=dec[g][:], func=AF.Exp), reads=["dec" + G], writes=["dec" + G])
                            cx.op("scalar", lambda e, g=g: e.activation(out=cd[g][:].unsqueeze(2), in_=psBC[g][:, :, lend:lend + 1], func=AF.Exp),
                                  reads=["psBC" + G], writes=["cd" + G])
                    for g in range(2):
                        G = str(g)
                        gsl = slice(g * 512, (g + 1) * 512)
                        cx.op("vector", lambda e, g=g: e.scalar_tensor_tensor(
                            out=Mb[g][:], in0=LT[g][:], scalar=1.0, in1=cbm[g][:].unsqueeze(1).to_broadcast([128, 8, 128]),
                            op0=ALU.min, op1=ALU.mult), reads=["LT" + G, "cbm" + G], writes=["Mb" + G])
                        for h in range(8):
                            cx.op("tensor", lambda e, h=h, g=g: e.matmul(
                                psY[g][:, h * 64:(h + 1) * 64], lhsT=Mb[g][:, h, :], rhs=Xb[:, (g * 8 + h) * 64:(g * 8 + h + 1) * 64],
                                start=True, stop=True), reads=["Mb" + G, "Xb"], writes=["psY" + G])
                        if idx < NT - 1:
                            cx.op("vector", lambda e, gsl=gsl, g=g: e.tensor_tensor(
                                out=Xd[g][:].rearrange("p (h d) -> p h d", d=64), in0=Xb[:, gsl].rearrange("p (h d) -> p h d", d=64),
                                in1=dec[g][:].unsqueeze(2).to_broadcast([128, 8, 64]), op=ALU.mult), reads=["Xb", "dec" + G], writes=["Xd" + G])
                    for g in range(2):
                        hs = slice(g * 8, (g + 1) * 8)
                        G = str(g)
                        gsl = slice(g * 512, (g + 1) * 512)
                        if idx > 0:
                            cx.op("tensor", lambda e, g=g: e.matmul(psYS[:], lhsT=CT[:, g, tsl], rhs=stbf[g][:], start=True, stop=True),
                                  reads=["CT", "stbf" + G], writes=["psYS"])
                            cx.op("vector", lambda e, hs=hs, g=g: e.tensor_tensor(
                                out=tmpy[g][:].rearrange("p (h d) -> p h d", d=64), in0=psYS[:].rearrange("p (h d) -> p h d", d=64),
                                in1=expA[:, hs].unsqueeze(2).to_broadcast([128, 8, 64]), op=ALU.mult),
                                reads=["psYS", "expA"], writes=["tmpy" + G])
                            cx.op("vector", lambda e, yd=yd, gsl=gsl, g=g: e.tensor_tensor(out=yd[:, gsl], in0=tmpy[g][:], in1=psY[g][:], op=ALU.add),
                                  reads=["tmpy" + G, "psY" + G], writes=[(kyd, g)])
                        else:
                            cx.op("vector", lambda e, yd=yd, gsl=gsl, g=g: e.tensor_copy(out=yd[:, gsl], in_=psY[g][:]),
                                  reads=["psY" + G], writes=[(kyd, g)])
                        if idx < NT - 1:
                            cx.op("tensor", lambda e, i=i, g=g: e.matmul(psYS[:], lhsT=Btm[:, i, g, :], rhs=Xd[g][:], start=True, stop=True),
                                  reads=["Btm", "Xd" + G], writes=["psYS"])
                            if idx > 0:
                                cx.op("vector", lambda e, g=g: e.tensor_tensor(
                                    out=state[g][:].rearrange("p (h d) -> p h d", d=64), in0=state[g][:].rearrange("p (h d) -> p h d", d=64),
                                    in1=cd[g][:].unsqueeze(2).to_broadcast([128, 8, 64]), op=ALU.mult),
                                    reads=["state" + G, "cd" + G], writes=["state" + G])
                                cx.op("vector", lambda e, g=g: e.tensor_tensor(out=state[g][:], in0=state[g][:], in1=psYS[:], op=ALU.add),
                                      reads=["state" + G, "psYS"], writes=["state" + G])
                            else:
                                cx.op("vector", lambda e, g=g: e.tensor_copy(out=state[g][:], in_=psYS[:]),
                                      reads=["psYS"], writes=["state" + G])
                            cx.op("scalar", lambda e, g=g: e.activation(out=stbf[g][:], in_=state[g][:], func=AF.Copy),
                                  reads=["state" + G], writes=["stbf" + G])
                    ykeys = [(kyd, 0), (kyd, 1)]
                    if not fwd:
                        cx.dma("sync", kyd, yb_d[tsl, :], yd[:], reads=ykeys, writes=[("yb_d", i)])
                    else:
                        cx.op("vector", lambda e, yd=yd, p=p: e.tensor_tensor(out=yd[:], in0=yd[:], in1=yb_t[p][:], op=ALU.add),
                              reads=ykeys + ["yb_t%d" % p], writes=ykeys)
                        cx.op("vector", lambda e, p=p: e.tensor_tensor(
                            out=gg[:].rearrange("p (h d) -> p h d", d=64), in0=xs_t[p][:].rearrange("p (h d) -> p h d", d=64),
                            in1=ssdp[:, 64:80].unsqueeze(2).to_broadcast([128, 16, 64]), op=ALU.mult),
                            reads=[kxs, "ssdp"], writes=["gg"])
                        cx.op("vector", lambda e, yd=yd: e.tensor_tensor(out=gg[:], in0=gg[:], in1=yd[:], op=ALU.add),
                              reads=["gg"] + ykeys, writes=["gg"])
                        cx.op("vector", lambda e, p=p: e.tensor_tensor(out=gg[:], in0=gg[:], in1=zs_t[p][:], op=ALU.mult),
                              reads=["gg", "zs_t%d" % p], writes=["gg"])
                        cx.op("scalar", lambda e: e.activation(out=junk2[:], in_=gg[:], func=AF.Square, accum_out=ssq[:]),
                              reads=["gg"], writes=["junk2", "ssq1"])
                        cx.op("vector", lambda e: e.tensor_scalar(out=ssq[:], in0=ssq[:], scalar1=1.0 / 1024, scalar2=EPS,
                                                                  op0=ALU.mult, op1=ALU.add), reads=["ssq1"], writes=["ssq1"])
                        cx.op("scalar", lambda e: e.activation(out=ssq[:], in_=ssq[:], func=AF.Sqrt), reads=["ssq1"], writes=["ssq1"])
                        cx.op("vector", lambda e: e.reciprocal(out=ssq[:], in_=ssq[:]), reads=["ssq1"], writes=["ssq1"])
                        cx.op("vector", lambda e, p=p: e.scalar_tensor_tensor(out=ssdtm[p][:], in0=gg[:], scalar=ssq[:, 0:1], in1=ssdnw[:],
                                                                              op0=ALU.mult, op1=ALU.mult),
                              reads=["gg", "ssq1", "ssdnw"], writes=["ssdtm0"])
                        cx.dma("sync", "ssdtm0", ssdtm_d[tsl, :], ssdtm[p][:], reads=["ssdtm0"], writes=[("ssdtm_d", i)])

            ssd_pass(False)
            ssd_pass(True)
            cx.barrier()
        with contextlib.ExitStack() as st:
            psT8 = [st.enter_context(nc.psum_tensor("psT8%d" % i, [128, 8, 128], BF16)) for i in range(2)]
            stl = [st.enter_context(nc.sbuf_tensor("y_stl%d" % i, [128, 1024], BF16)) for i in range(2)]
            for i in range(NT):
                p = i % 2
                cx.dma("sync", "stl%d" % p, stl[p][:], ssdtm_d[i * 128:(i + 1) * 128, :], reads=[("ssdtm_d", i)], writes=["stl%d" % p])
                for c in range(8):
                    cx.op("tensor", lambda e, c=c, p=p: e.transpose(out=psT8[p][:, c, :], in_=stl[p][:, c * 128:(c + 1) * 128],
                                                                    identity=identb[:]), reads=["stl%d" % p], writes=["psT8%d" % p])
                cx.op("scalar", lambda e, p=p, i=i: e.activation(out=ssdT[:, :, i * 128:(i + 1) * 128], in_=psT8[p][:], func=AF.Copy),
                      reads=["psT8%d" % p], writes=[("hT", i)])
            if "d_ssdT" in debug:
                cx.dma("sync", "dbg", dbg_out["d_ssdT"][:, :], ssdT[:].rearrange("p c s -> p (c s)"),
                       reads=[("hT", i) for i in range(NT)])
            cx.barrier()


        g2bc = sb("g2bc", [128, D], F32)
        g1scope = contextlib.ExitStack()
        g1bc = g1scope.enter_context(nc.sbuf_tensor("s_g1bc", [128, D], F32))
        with contextlib.ExitStack() as st:
            diag = [st.enter_context(nc.sbuf_tensor("diag%d" % i, [128, 128], F32)) for i in range(2)]
            psG = st.enter_context(nc.psum_tensor("psG", [128, 1024], F32))
            for (dst, kd, c0) in [(g1bc, "g1bc", 16), (g2bc, "g2bc", 40)]:
                for c in range(8):
                    dg = diag[c % 2]; kg = "diag%d" % (c % 2)
                    cx.op("vector", lambda e, dg=dg, c=c, c0=c0: e.tensor_scalar(out=dg[:], in0=ident[:], scalar1=modT[:, c0 + c:c0 + c + 1],
                                                                                 scalar2=None, op0=ALU.mult), reads=["ident", "modT"], writes=[kg])
                    cx.op("tensor", lambda e, dg=dg, c=c: e.matmul(psG[:, c * 128:(c + 1) * 128], lhsT=ones_f[:], rhs=dg[:], start=True, stop=True),
                          reads=[kg, "ones_f"], writes=["psG"])
                cx.op("vector", lambda e, dst=dst: e.tensor_copy(out=dst[:], in_=psG[:]), reads=["psG"], writes=[kd])
            cx.barrier()
        ssdT = hT
        with contextlib.ExitStack() as st:
            def T(name, shape, dt):
                return st.enter_context(nc.sbuf_tensor("z_" + name, list(shape), dt))
            wau = T("wau", [128, 4, D], BF16)
            wsu = T("wsu", [128, 8, D], BF16)
            wout = T("wout", [128, 8, D], BF16)
            attg = [T("attg%d" % i, [128, 4, 512], BF16) for i in range(2)]
            g0t = [T("g0t%d" % i, [128, 512], BF16) for i in range(2)]
            g1t = [T("g1t%d" % i, [128, 512], BF16) for i in range(2)]
            t1 = T("t1", [128, 512], F32)
            t2 = T("t2", [128, 512], F32)
            mT = T("mT", [128, 8, 512], BF16)
            xin = [T("xin%d" % i, [128, D], F32) for i in range(2)]
            x1t = [T("x1t%d" % i, [128, D], F32) for i in range(2)]
            psUa = [st.enter_context(nc.psum_tensor("psUa%d" % i, [128, 512], F32)) for i in range(2)]
            psUs = [st.enter_context(nc.psum_tensor("psUs%d" % i, [128, 512], F32)) for i in range(2)]
            psM = [st.enter_context(nc.psum_tensor("psM%d" % i, [128, 1024], F32)) for i in range(2)]
            cx.dma("gpsimd", "wau", wau[:], wau_in.rearrange("(kc p) n -> p kc n", p=128), writes=["wau"])
            cx.dma("gpsimd", "wsu", wsu[:], wsu_in.rearrange("(kc p) n -> p kc n", p=128), writes=["wsu"])
            cx.dma("gpsimd", "wout", wout[:], wout_in.rearrange("(kc p) n -> p kc n", p=128), writes=["wout"])
            n = 0
            for grp in range(8):
                gs = slice(grp * 512, (grp + 1) * 512)
                ag = attg[grp % 2]; ka = "attg%d" % (grp % 2)
                cx.dma("sync", ka, ag[:], attT_d[:, gs].rearrange("(c p) s -> p c s", p=128), reads=["attT_d"], writes=[ka])
                for dc in range(8):
                    b = n % 2; n += 1
                    dsl = slice(dc * 128, (dc + 1) * 128)
                    cx.dma("sync", "g0t%d" % b, g0t[b][:], gatesT_d[dc * 128:(dc + 1) * 128, gs], writes=["g0t%d" % b])
                    cx.dma("sync", "g1t%d" % b, g1t[b][:], gatesT_d[1024 + dc * 128:1024 + (dc + 1) * 128, gs], writes=["g1t%d" % b])
                    for kc in range(4):
                        cx.op("tensor", lambda e, b=b, kc=kc, dsl=dsl, ag=ag: e.matmul(psUa[b][:], lhsT=wau[:, kc, dsl], rhs=ag[:, kc, :],
                                                                                     start=(kc == 0), stop=(kc == 3)),
                              reads=["wau", ka], writes=["psUa%d" % b])
                    for kc in range(8):
                        cx.op("tensor", lambda e, b=b, kc=kc, dsl=dsl, gs=gs: e.matmul(psUs[b][:], lhsT=wsu[:, kc, dsl], rhs=ssdT[:, kc, gs],
                                                                                     start=(kc == 0), stop=(kc == 7)),
                              reads=["wsu"] + [("hT", grp * 4 + k) for k in range(4)], writes=["psUs%d" % b])
                    cx.op("vector", lambda e, b=b: e.tensor_tensor(out=t1[:], in0=psUa[b][:], in1=g0t[b][:], op=ALU.mult),
                          reads=["psUa%d" % b, "g0t%d" % b], writes=["t1"])
                    cx.op("vector", lambda e, b=b: e.tensor_tensor(out=t2[:], in0=psUs[b][:], in1=g1t[b][:], op=ALU.mult),
                          reads=["psUs%d" % b, "g1t%d" % b], writes=["t2"])
                    cx.op("vector", lambda e, dc=dc: e.tensor_tensor(out=mT[:, dc, :], in0=t1[:], in1=t2[:], op=ALU.add),
                          reads=["t1", "t2"], writes=[("mT", dc)])
                for sub in range(4):
                    i = grp * 4 + sub
                    p = i % 2
                    tsl = slice(i * 128, (i + 1) * 128)
                    cx.dma("sync", "xin%d" % p, xin[p][:], x[tsl, :], writes=["xin%d" % p])
                    for half in range(2):
                        for dc in range(8):
                            cx.op("tensor", lambda e, p=p, half=half, dc=dc, sub=sub: e.matmul(
                                psM[p][:, half * 512:(half + 1) * 512], lhsT=mT[:, dc, sub * 128:(sub + 1) * 128],
                                rhs=wout[:, dc, half * 512:(half + 1) * 512], start=(dc == 0), stop=(dc == 7)),
                                reads=["wout"] + [("mT", d_) for d_ in range(8)], writes=["psM%d" % p])
                    cx.op("vector", lambda e, p=p: e.tensor_tensor(out=x1t[p][:], in0=psM[p][:], in1=g1bc[:], op=ALU.mult),
                          reads=["psM%d" % p, "g1bc"], writes=["x1t%d" % p])
                    cx.op("vector", lambda e, p=p: e.tensor_tensor(out=x1t[p][:], in0=x1t[p][:], in1=xin[p][:], op=ALU.add),
                          reads=["x1t%d" % p, "xin%d" % p], writes=["x1t%d" % p])
                    cx.dma("sync", "x1t%d" % p, x1_d[tsl, :], x1t[p][:], reads=["x1t%d" % p], writes=[("x1_d", i)])
            cx.barrier()
        g1scope.close()
        with contextlib.ExitStack() as st:
            norm_mod_transpose(x1_d, gam2, modT[:, 24:32], st, "b", rkey="x1_d")
            if "d_h2T" in debug:
                cx.dma("sync", "dbg", dbg_out["d_h2T"][:, :], hT[:].rearrange("p c s -> p (c s)"),
                       reads=[("hT", i) for i in range(NT)])
            cx.barrier()


        h2T = hT
        i1T = sb("i1T", [128, S], BF16)
        i2T = sb("i2T", [128, S], BF16)
        gT = sb("gT", [128, S], BF16)
        GELU = AF.Gelu_apprx_tanh
        with contextlib.ExitStack() as st:
            def T(name, shape, dt):
                return st.enter_context(nc.sbuf_tensor("r_" + name, list(shape), dt))
            wq = T("wq", [128, 8, 2048], BF16)
            keysT = T("keysT", [128, 16, 128], BF16)
            iota16 = T("iota16", [128, 16], F32)
            qT = T("qT", [128, 16, 512], BF16)
            bufA = T("bufA", [128, 2048], F32)
            bufB = T("bufB", [128, 2048], F32)
            sc = bufA[:].rearrange("p (a b) -> p a b", b=128)
            sc2 = bufB[:].rearrange("p (a b) -> p a b", b=128)
            cand = bufA[:].rearrange("p (a b) -> p a b", b=256)
            cand2 = bufB[:].rearrange("p (a b) -> p a b", b=256)
            oh = bufA[:].rearrange("p (a b) -> p a b", b=16)
            vtop = T("vtop", [128, 16, 16], F32)
            ixu = T("ixu", [128, 16, 16], U32)
            ixf = T("ixf", [128, 16, 16], F32)
            sc16 = T("sc16", [128, 8, 16], F32)
            posu = T("posu", [128, 8, 16], U32)
            au = T("au", [128, 8, 16], U32)
            bu = T("bu", [128, 8, 16], U32)
            af = T("af", [128, 8, 16], F32)
            bf = T("bf", [128, 8, 16], F32)
            esum = T("esum", [128, 8], F32)
            gf = T("gf", [128, 8, 16], F32)
            idf = [T("idf%d" % m, [128, 128], F32) for m in range(2)]
            psQ = st.enter_context(nc.psum_tensor("psQ", [128, 512], F32))
            psSc = st.enter_context(nc.psum_tensor("psSc", [128, 16, 128], F32))
            psT3 = st.enter_context(nc.psum_tensor("psT3", [128, 3, 128], F32))
            cx.dma("gpsimd", "wq", wq[:], wq_in.rearrange("(kc p) n -> p kc n", p=128), writes=["wq"])
            cx.dma("gpsimd", "r0", keysT[:].rearrange("p a b -> p (a b)"), keysT_in[:, :], writes=["keysT"])
            cx.dma("sync", "r1", iota16[:], iota16_in[:, :], writes=["iota16"])
            for grp in range(8):
                gs = slice(grp * 512, (grp + 1) * 512)
                for j in range(16):
                    for kc in range(8):
                        cx.op("tensor", lambda e, j=j, kc=kc, gs=gs: e.matmul(psQ[:], lhsT=wq[:, kc, j * 128:(j + 1) * 128], rhs=h2T[:, kc, gs],
                                                                             start=(kc == 0), stop=(kc == 7)), reads=["wq"], writes=["psQ"])
                    cx.op("scalar", lambda e, j=j: e.activation(out=qT[:, j, :], in_=psQ[:], func=AF.Copy), reads=["psQ"], writes=[("qT", j)])
                for sub in range(4):
                    i = grp * 4 + sub
                    tsl = slice(i * 128, (i + 1) * 128)
                    for j in range(16):
                        cx.op("tensor", lambda e, j=j, sub=sub: e.matmul(psSc[:, j, :], lhsT=qT[:, j, sub * 128:(sub + 1) * 128], rhs=keysT[:, j, :],
                                                                         start=True, stop=True), reads=[("qT", j), "keysT"], writes=["psSc"])
                    cx.op("scalar", lambda e: e.activation(out=sc, in_=psSc[:], func=AF.Copy), reads=["psSc"], writes=["bufA"])
                    for j in range(16):
                        cx.op("vector", lambda e, j=j: e.max(out=vtop[:, j, 0:8], in_=sc[:, j, :]), reads=["bufA"], writes=[("vtop", j)])
                    for j in range(16):
                        cx.op("vector", lambda e, j=j: e.max_index(out=ixu[:, j, 0:8], in_max=vtop[:, j, 0:8], in_values=sc[:, j, :]),
                              reads=["bufA", ("vtop", j)], writes=[("ixu", j)])
                    for j in range(16):
                        cx.op("vector", lambda e, j=j: e.match_replace(out=sc2[:, j, :], in_to_replace=vtop[:, j, 0:8], in_values=sc[:, j, :],
                                                                       imm_value=-1e30), reads=["bufA", ("vtop", j)], writes=[("bufB", j)])
                    for j in range(16):
                        cx.op("vector", lambda e, j=j: e.max(out=vtop[:, j, 8:16], in_=sc2[:, j, :]), reads=[("bufB", j)], writes=[("vtop", j)])
                    for j in range(16):
                        cx.op("vector", lambda e, j=j: e.max_index(out=ixu[:, j, 8:16], in_max=vtop[:, j, 8:16], in_values=sc2[:, j, :]),
                              reads=[("bufB", j), ("vtop", j)], writes=[("ixu", j)])
                    vk = [("vtop", j) for j in range(16)]
                    ik = [("ixu", j) for j in range(16)]
                    cx.op("vector", lambda e: e.tensor_copy(out=ixf[:], in_=ixu[:]), reads=ik, writes=["ixf"])
                    vv = vtop[:].rearrange("p (h two) k -> p h two k", two=2)
                    cx.op("vector", lambda e, vv=vv: e.tensor_tensor(
                        out=cand.rearrange("p h (a b) -> p h a b", b=16), in0=vv[:, :, 0, :].unsqueeze(3).to_broadcast([128, 8, 16, 16]),
                        in1=vv[:, :, 1, :].unsqueeze(2).to_broadcast([128, 8, 16, 16]), op=ALU.add), reads=vk, writes=["bufA"])
                    for h in range(8):
                        cx.op("vector", lambda e, h=h: e.max(out=sc16[:, h, 0:8], in_=cand[:, h, :]), reads=["bufA"], writes=[("sc16", h)])
                    for h in range(8):
                        cx.op("vector", lambda e, h=h: e.max_index(out=posu[:, h, 0:8], in_max=sc16[:, h, 0:8], in_values=cand[:, h, :]),
                              reads=["bufA", ("sc16", h)], writes=[("posu", h)])
                    for h in range(8):
                        cx.op("vector", lambda e, h=h: e.match_replace(out=cand2[:, h, :], in_to_replace=sc16[:, h, 0:8], in_values=cand[:, h, :],
                                                                       imm_value=-1e30), reads=["bufA", ("sc16", h)],
                              writes=[("bufB", 2 * h), ("bufB", 2 * h + 1)])
                    for h in range(8):
                        cx.op("vector", lambda e, h=h: e.max(out=sc16[:, h, 8:16], in_=cand2[:, h, :]),
                              reads=[("bufB", 2 * h), ("bufB", 2 * h + 1)], writes=[("sc16", h)])
                    for h in range(8):
                        cx.op("vector", lambda e, h=h: e.max_index(out=posu[:, h, 8:16], in_max=sc16[:, h, 8:16], in_values=cand2[:, h, :]),
                              reads=[("bufB", 2 * h), ("bufB", 2 * h + 1), ("sc16", h)], writes=[("posu", h)])
                    sk = [("sc16", h) for h in range(8)]
                    pk = [("posu", h) for h in range(8)]
                    cx.op("vector", lambda e: e.tensor_tensor(out=gf[:], in0=sc16[:], in1=sc16[:, :, 0:1].to_broadcast([128, 8, 16]),
                                                              op=ALU.subtract), reads=sk, writes=["gf"])
                    cx.op("scalar", lambda e: e.activation(out=gf[:], in_=gf[:], func=AF.Exp), reads=["gf"], writes=["gf"])
                    cx.op("vector", lambda e: e.tensor_reduce(out=esum[:], in_=gf[:], axis=AX.X, op=ALU.add), reads=["gf"], writes=["esum"])
                    cx.op("vector", lambda e: e.reciprocal(out=esum[:], in_=esum[:]), reads=["esum"], writes=["esum"])
                    cx.op("vector", lambda e: e.tensor_tensor(out=gf[:], in0=gf[:], in1=esum[:].unsqueeze(2).to_broadcast([128, 8, 16]),
                                                              op=ALU.mult), reads=["gf", "esum"], writes=["gf"])
                    cx.op("vector", lambda e: e.tensor_single_scalar(out=au[:], in_=posu[:], scalar=4, op=ALU.logical_shift_right),
                          reads=pk, writes=["au"])
                    cx.op("vector", lambda e: e.tensor_single_scalar(out=bu[:], in_=posu[:], scalar=15, op=ALU.bitwise_and),
                          reads=pk, writes=["bu"])
                    cx.op("vector", lambda e: e.tensor_copy(out=af[:], in_=au[:]), reads=["au"], writes=["af"])
                    cx.op("vector", lambda e: e.tensor_copy(out=bf[:], in_=bu[:]), reads=["bu"], writes=["bf"])
                    ixv = ixf[:].rearrange("p (h two) k -> p h two k", two=2)
                    for m, (sel, ksel) in enumerate([(af, "af"), (bf, "bf")]):
                        cx.op("vector", lambda e, sel=sel: e.tensor_tensor(
                            out=oh, in0=sel[:].rearrange("p h k -> p (h k)").unsqueeze(2).to_broadcast([128, 128, 16]),
                            in1=iota16[:].unsqueeze(1).to_broadcast([128, 128, 16]), op=ALU.is_equal), reads=[ksel, "iota16"], writes=["bufA"])
                        cx.op("vector", lambda e, m=m, ixv=ixv: e.tensor_tensor(
                            out=oh.rearrange("p (h k) a -> p h k a", k=16), in0=oh.rearrange("p (h k) a -> p h k a", k=16),
                            in1=ixv[:, :, m, :].unsqueeze(2).to_broadcast([128, 8, 16, 16]), op=ALU.mult), reads=["bufA", "ixf"], writes=["bufA"])
                        cx.op("vector", lambda e, m=m: e.tensor_reduce(out=idf[m][:], in_=oh, axis=AX.X, op=ALU.add),
                              reads=["bufA"], writes=["idf%d" % m])
                    cx.op("tensor", lambda e: e.transpose(out=psT3[:, 0, :], in_=idf[0][:], identity=ident[:]), reads=["idf0"], writes=["psT3"])
                    cx.op("tensor", lambda e: e.transpose(out=psT3[:, 1, :], in_=idf[1][:], identity=ident[:]), reads=["idf1"], writes=["psT3"])
                    cx.op("tensor", lambda e: e.transpose(out=psT3[:, 2, :], in_=gf[:].rearrange("p h k -> p (h k)"), identity=ident[:]),
                          reads=["gf"], writes=["psT3"])
                    cx.op("scalar", lambda e: e.activation(out=i1T[:, tsl], in_=psT3[:, 0, :], func=AF.Copy), reads=["psT3"], writes=[("rt", i)])
                    cx.op("scalar", lambda e: e.activation(out=i2T[:, tsl], in_=psT3[:, 1, :], func=AF.Identity, scale=-1.0), reads=["psT3"], writes=[("rt", i)])
                    cx.op("scalar", lambda e: e.activation(out=gT[:, tsl], in_=psT3[:, 2, :], func=AF.Copy), reads=["psT3"], writes=[("rt", i)])
            for kc in range(8):
                cx.dma("sync", "h2Td", h2T_d[kc * 128:(kc + 1) * 128, :], h2T[:, kc, :])
            for nm, t in [("d_i1T", i1T), ("d_i2T", i2T), ("d_gT", gT)]:
                if nm in debug:
                    cx.dma("sync", "dbg", dbg_out[nm][:, :], t[:])
            cx.barrier()

        with contextlib.ExitStack() as st:
            def T(name, shape, dt):
                return st.enter_context(nc.sbuf_tensor("p_" + name, list(shape), dt))
            TG = 256
            iota128f = T("iota128f", [128, 128], F32)
            iota128 = T("iota128", [128, 128], BF16)
            niota128 = T("niota128", [128, 128], BF16)
            Btmp = [T("Btmp%d" % i, [128, 128], BF16) for i in range(4)]
            fnw = T("fnw", [128, D], F32)
            gwb = [T("gw", [128, 128, TG], BF16), hT[:].rearrange("p c (a b) -> p (c a) b", b=TG)]
            h2g = [T("h2g%d" % i, [128, 8, TG], BF16) for i in range(2)]
            Aoh = [T("Aoh%d" % i, [128, 128], BF16) for i in range(4)]
            Boh = [T("Boh%d" % i, [128, 128], BF16) for i in range(4)]
            ut = [T("ut%d" % i, [128, 2, 8, 128], BF16) for i in range(2)]
            vt = [T("vt%d" % i, [128, 2, D], BF16) for i in range(2)]
            gel = [T("gel%d" % i, [128, TG], F32) for i in range(2)]
            Pb = [T("Pb%d" % i, [128, TG], BF16) for i in range(2)]
            x1s = [T("x1s%d" % i, [128, D], F32) for i in range(1)] * 2
            xo = [T("xo%d" % i, [128, D], F32) for i in range(1)] * 2
            ssq = T("ssq3", [128, 2], F32)
            psW = [st.enter_context(nc.psum_tensor("psW%d" % i, [128, 4, 128], F32)) for i in range(2)]
            psA = [st.enter_context(nc.psum_tensor("psA_%d" % i, [128, 512], F32)) for i in range(2)]
            psO = [st.enter_context(nc.psum_tensor("psO_%d" % i, [128, 1024], F32)) for i in range(2)]
            cx.dma("sync", "p0", iota128f[:], iota128_in[:, :], writes=["iota128f"])
            cx.op("vector", lambda e: e.tensor_copy(out=iota128[:], in_=iota128f[:]), reads=["iota128f"], writes=["iota128"])
            cx.op("vector", lambda e: e.tensor_scalar(out=niota128[:], in0=iota128f[:], scalar1=-1.0, scalar2=None, op0=ALU.mult),
                  reads=["iota128f"], writes=["niota128"])
            cx.dma("sync", "p1", fnw[:], fnw_in.partition_broadcast(128), writes=["fnw"])
            NG = S // TG
            nWc = [0]

            def gw_onehots(grp, q4, toks):
                t0 = grp * TG
                for tq in toks:
                    col = t0 + q4 * 4 + tq
                    r = tq
                    cx.op("vector", lambda e, r=r, col=col: e.tensor_scalar(
                        out=Aoh[r][:], in0=iota128[:], scalar1=i1T[:, col:col + 1], scalar2=gT[:, col:col + 1],
                        op0=ALU.is_equal, op1=ALU.mult), reads=["iota128"], writes=["Aoh%d" % r])
                    if tq % 2 == 1:
                        cx.op("vector", lambda e, r=r, col=col: e.tensor_scalar(
                            out=Boh[r][:], in0=niota128[:], scalar1=i2T[:, col:col + 1], scalar2=None,
                            op0=ALU.is_equal), reads=["niota128"], writes=["Boh%d" % r])
                    else:
                        cx.op("scalar", lambda e, r=r, col=col: e.activation(out=Btmp[r][:], in_=iota128[:], func=AF.Abs,
                                                                              bias=i2T[:, col:col + 1], scale=1.0),
                              reads=["iota128"], writes=["Btmp%d" % r])
                        cx.op("scalar", lambda e, r=r: e.activation(out=Boh[r][:], in_=Btmp[r][:], func=AF.Relu, bias=1.0, scale=-1.0),
                              reads=["Btmp%d" % r], writes=["Boh%d" % r])

            def gw_mm(grp, q4):
                b = q4 % 2
                for tq in range(4):
                    cx.op("tensor", lambda e, b=b, tq=tq: e.matmul(psW[b][:, tq, :], lhsT=Aoh[tq][:], rhs=Boh[tq][:], start=True, stop=True),
                          reads=["Aoh%d" % tq, "Boh%d" % tq], writes=["psW%d" % b])

            def gw_evac(grp, q4):
                b = q4 % 2
                g_ = gwb[grp % 2]
                cx.op("scalar", lambda e: e.activation(out=g_[:, :, q4 * 4:(q4 + 1) * 4],
                                                       in_=psW[b][:].rearrange("p t j -> p j t"), func=AF.Copy),
                      reads=["psW%d" % b], writes=["gw%d" % (grp % 2)])

            def emit_gw(grp, q4):
                gw_onehots(grp, q4, (0, 1, 2, 3))
                gw_mm(grp, q4)
                gw_evac(grp, q4)

            def load_h2(grp):
                cx.dma("gpsimd", "h2g%d" % (grp % 2), h2g[grp % 2][:], h2T_d[:, grp * TG:(grp + 1) * TG].rearrange("(c p) t -> p c t", p=128),
                       writes=["h2g%d" % (grp % 2)])

            def load_w(gb):
                jb = (gb % 64) * 2
                pb = gb % 2
                rs = slice(jb * 128, (jb + 2) * 128)
                bs = slice((gb % 64) * 128, (gb % 64 + 1) * 128)
                cx.dma("sync", "ut%d" % pb, ut[pb][:].rearrange("p t a b -> p (t a b)"), u_bf[bs, :],
                       reads=[("u_bf", jb), ("u_bf", jb + 1)], writes=["ut%d" % pb])
                cx.dma("sync", "vt%d" % pb, vt[pb][:].rearrange("p t n -> p (t n)"), v_bf[bs, :],
                       reads=[("v_bf", jb), ("v_bf", jb + 1)], writes=["vt%d" % pb])

            load_h2(0)
            load_w(0)
            load_w(1)
            for q4 in range(TG // 4):
                emit_gw(0, q4)
            for grp in range(NG):
                t0 = grp * TG
                gw = gwb[grp % 2]
                kgw = "gw%d" % (grp % 2)
                hg = h2g[grp % 2]
                khg = "h2g%d" % (grp % 2)
                if grp + 1 < NG:
                    load_h2(grp + 1)

                def emit_u(j):
                    p3 = (j // 2) % 2
                    ja = j % 2
                    b = j % 2
                    for kc in range(8):
                        cx.op("tensor", lambda e, kc=kc: e.matmul(psA[b][:, 0:TG], lhsT=ut[p3][:, ja, kc, :], rhs=hg[:, kc, :],
                                                                  start=(kc == 0), stop=(kc == 7)),
                              reads=["ut%d" % p3, khg], writes=["psA_%d" % b])

                emit_u(0)
                for j in range(128):
                    p3 = (j // 2) % 2
                    ja = j % 2
                    b = j % 2
                    if j + 1 < 128:
                        emit_u(j + 1)
                    cx.op("scalar", lambda e, b=b: e.activation(out=gel[b][:], in_=psA[b][:, 0:TG], func=GELU),
                          reads=["psA_%d" % b], writes=["gel%d" % b])
                    cx.op("vector", lambda e, b=b, j=j: e.tensor_tensor(out=Pb[b][:], in0=gel[b][:], in1=gw[:, j, :], op=ALU.mult),
                          reads=["gel%d" % b, kgw], writes=["Pb%d" % b])
                    for sub in range(2):
                        for half in range(2):
                            cx.op("tensor", lambda e, b=b, sub=sub, half=half, p3=p3, j=j, ja=ja: e.matmul(
                                psO[sub][:, half * 512:(half + 1) * 512], lhsT=Pb[b][:, sub * 128:(sub + 1) * 128],
                                rhs=vt[p3][:, ja, half * 512:(half + 1) * 512], start=(j == 0), stop=(j == 127)),
                                reads=["Pb%d" % b, "vt%d" % p3], writes=["psO_%d" % sub])
                    if j % 2 == 1:
                        gb = grp * 64 + j // 2 + 2
                        if gb < NG * 64:
                            load_w(gb)
                    if grp + 1 < NG:
                        q4 = j // 2
                        if j % 2 == 0:
                            gw_onehots(grp + 1, q4, (0, 1))
                        else:
                            gw_onehots(grp + 1, q4, (2, 3))
                            gw_mm(grp + 1, q4)
                            if q4 > 0:
                                gw_evac(grp + 1, q4 - 1)
                if grp + 1 < NG:
                    gw_evac(grp + 1, 63)
                for sub in range(2):
                    i = grp * 2 + sub
                    tsl = slice(i * 128, (i + 1) * 128)
                    cx.dma("gpsimd", "x1s0", x1s[sub][:], x1_d[tsl, :], reads=[("x1_d", i)], writes=["x1s0"])
                    X = xo[sub]; kx = "xo0"
                    cx.op("vector", lambda e, X=X, sub=sub: e.tensor_tensor(out=X[:], in0=psO[sub][:], in1=g2bc[:], op=ALU.mult),
                          reads=["psO_%d" % sub], writes=[kx])
                    cx.op("vector", lambda e, X=X, sub=sub: e.tensor_tensor(out=X[:], in0=X[:], in1=x1s[sub][:], op=ALU.add),
                          reads=[kx, "x1s0"], writes=[kx])
                    cx.op("scalar", lambda e, X=X, sub=sub: e.activation(out=x1s[sub][:], in_=X[:], func=AF.Square, accum_out=ssq[:, sub:sub + 1]),
                          reads=[kx], writes=["x1s0", ("ssq3", sub)])
                    cx.op("vector", lambda e, sub=sub: e.tensor_scalar(out=ssq[:, sub:sub + 1], in0=ssq[:, sub:sub + 1], scalar1=1.0 / D, scalar2=EPS,
                                                                       op0=ALU.mult, op1=ALU.add), reads=[("ssq3", sub)], writes=[("ssq3", sub)])
                    cx.op("scalar", lambda e, sub=sub: e.activation(out=ssq[:, sub:sub + 1], in_=ssq[:, sub:sub + 1], func=AF.Sqrt),
                          reads=[("ssq3", sub)], writes=[("ssq3", sub)])
                    cx.op("vector", lambda e, sub=sub: e.reciprocal(out=ssq[:, sub:sub + 1], in_=ssq[:, sub:sub + 1]),
                          reads=[("ssq3", sub)], writes=[("ssq3", sub)])
                    cx.op("vector", lambda e, X=X, sub=sub: e.scalar_tensor_tensor(out=X[:], in0=X[:], scalar=ssq[:, sub:sub + 1], in1=fnw[:],
                                                                                  op0=ALU.mult, op1=ALU.mult),
                          reads=[kx, ("ssq3", sub), "fnw"], writes=[kx])
                    cx.dma("gpsimd", kx, out[tsl, :], X[:], reads=[kx], writes=[("out", i)])
            cx.barrier()

        cx.barrier()
    print("instructions:", cx.ninst)
    return nc


def make_in_maps(inputs, ncores=8):
    f = np.float32
    C, Sg = rope_tables()
    ident = np.eye(128, dtype=f)
    qperm = np.concatenate([np.arange(h * 64, (h + 1) * 64) for h in [0, 4, 1, 5, 2, 6, 3, 7]])
    cols = np.concatenate([qperm, np.arange(512, 768), np.arange(3328, 3360), np.arange(768, 1792),
                           np.arange(1792, 3328), np.arange(3360, 5408)])
    w_in_p = np.ascontiguousarray(inputs["w_in"][0][:, cols])
    qkgain = np.concatenate([np.tile(inputs["q_gain"][0], 8), np.tile(inputs["k_gain"][0], 2)])[None, :].astype(f)
    convwT = np.ascontiguousarray(inputs["conv_w"][0].reshape(5, 12, 128).transpose(2, 1, 0).reshape(128, 60))
    convbT = np.ascontiguousarray(inputs["conv_b"][0].reshape(12, 128).T)
    wau = np.ascontiguousarray(inputs["w_attn_up"][0][qperm, :])
    wq_h = np.ascontiguousarray(inputs["peer_w_query"][0])
    keysT_h = np.ascontiguousarray(np.stack([inputs["peer_keys1"][0], inputs["peer_keys2"][0]], axis=1)
                                   .transpose(3, 0, 1, 2).reshape(128, 2048))
    iota128 = np.tile(np.arange(128, dtype=f)[None, :], (128, 1))
    iota16 = np.tile(np.arange(16, dtype=f)[None, :], (128, 1))
    fnw_h = inputs["final_norm_w"][None, :].astype(f)
    U_h = np.ascontiguousarray(inputs["peer_u"][0].reshape(128, 128, 8, 128).transpose(1, 3, 2, 0)).reshape(128 * 128, D)
    V_h = np.ascontiguousarray(inputs["peer_v"][0].reshape(128, 128, D).transpose(1, 0, 2)).reshape(128 * 128, D)
    ii = np.arange(128)
    trif = (ii[:, None] <= ii[None, :]).astype(f)
    maskf = np.where(ii[None, :] >= ii[:, None], 0.0, -30000.0).astype(f)
    ssdp = np.concatenate([inputs["dt_bias_f"][0], inputs["dt_bias_b"][0], inputs["a_log_f"][0], inputs["a_log_b"][0],
                           inputs["d_skip"][0]])[None, :].astype(f)
    maps = []
    for b in range(ncores):
        m = {
            "x": np.ascontiguousarray(inputs["x"][b]),
            "c_col": np.ascontiguousarray(inputs["c"][b].reshape(8, 128).T),
            "ada_w": np.ascontiguousarray(inputs["ada_w"][0]),
            "ada_bT": np.ascontiguousarray(inputs["ada_b"][0].reshape(48, 128).T),
            "n1T": np.ascontiguousarray(inputs["norm1_w"][0].reshape(8, 128).T),
            "n2T": np.ascontiguousarray(inputs["norm2_w"][0].reshape(8, 128).T),
            "w_in": w_in_p,
            "trif": trif, "trib": np.ascontiguousarray(trif.T), "maskf": maskf, "maskb": np.ascontiguousarray(maskf.T),
            "ssdp": ssdp, "ssdnw": inputs["ssd_norm_w"][0][None, :].astype(f),
            "wau": wau, "wsu": np.ascontiguousarray(inputs["w_ssd_up"][0]), "wout": np.ascontiguousarray(inputs["w_out"][0]),
            "wq": wq_h, "keysT": keysT_h, "iota128": iota128, "iota16": iota16, "fnw": fnw_h, "U_h": U_h, "V_h": V_h,
            "ropeC": C, "ropeS": Sg, "qkgain": qkgain, "convwT": convwT, "convbT": convbT,
            "ident": ident,
        }
        maps.append(m)
    return maps


def kernel(**inputs):
    inputs = {k: np.asarray(v) for k, v in inputs.items()}
    nc = build()
    maps = make_in_maps(inputs)
    res = run_bass_kernel_spmd(nc, maps, core_ids=list(range(8)))
    return np.stack([r["out"] for r in res.results], axis=0).astype(np.float32)
```

```python
import contextlib
import numpy as np
import concourse.bass as bass
import concourse.mybir as mybir
from concourse.bass_utils import run_bass_kernel_spmd

F32 = mybir.dt.float32
BF16 = mybir.dt.bfloat16
U32 = mybir.dt.uint32
I32 = mybir.dt.int32
AF = mybir.ActivationFunctionType
ALU = mybir.AluOpType
AX = mybir.AxisListType

S = 4096
D = 1024
NT = S // 128
EPS = 1e-6
IN_W = 5408
STRICT = True


class Sem:
    def __init__(self, h):
        self.h = h
        self.cnt = 0


class Eng:
    def __init__(self, name, h, sem):
        self.name = name
        self.h = h
        self.sem = sem
        self.seen = {}


class Buf:
    __slots__ = ("w", "rs")

    def __init__(self):
        self.w = None
        self.rs = []


class Ctx:
    def __init__(self, nc, es):
        self.nc = nc
        self.es = es
        self.engs = {}
        for name in ["tensor", "vector", "scalar", "gpsimd", "sync"]:
            sem = Sem(es.enter_context(nc.semaphore("e_" + name)))
            self.engs[name] = Eng(name, getattr(nc, name), sem)
        self.bufs = {}
        self.dsems = {}
        self.ninst = 0

    def buf(self, key):
        b = self.bufs.get(key)
        if b is None:
            b = Buf()
            self.bufs[key] = b
        return b

    def dsem(self, key):
        s = self.dsems.get(key)
        if s is None:
            s = Sem(self.es.enter_context(self.nc.semaphore("d%d" % len(self.dsems))))
            self.dsems[key] = s
        return s

    def _waits(self, E, reads, writes, skip_self=False):
        need = {}

        def add(st):
            sem, val = st
            if need.get(sem, 0) < val:
                need[sem] = val

        for k in reads:
            b = self.bufs.get(k)
            if b is not None and b.w is not None:
                add(b.w)
        for k in writes:
            b = self.buf(k)
            if b.w is not None:
                add(b.w)
            for r in b.rs:
                add(r)
        for sem, val in need.items():
            if sem is E.sem and (skip_self or not STRICT):
                continue
            if E.seen.get(sem, 0) >= val:
                continue
            E.h.wait_ge(sem.h, val)
            E.seen[sem] = val
            self.ninst += 1

    def _stamp(self, stamp, reads, writes):
        for k in reads:
            self.buf(k).rs.append(stamp)
        for k in writes:
            b = self.buf(k)
            b.w = stamp
            b.rs = []

    def op(self, eng, fn, reads=(), writes=()):
        E = self.engs[eng]
        self._waits(E, reads, writes, skip_self=(eng == "tensor"))
        inst = fn(E.h)
        E.sem.cnt += 1
        inst.then_inc(E.sem.h, 1)
        self.ninst += 1
        self._stamp((E.sem, E.sem.cnt), reads, writes)
        return inst

    def dma(self, queue, semkey, out, in_, reads=(), writes=(), **kw):
        E = self.engs[queue]
        sem = self.dsem(semkey)
        self._waits(E, reads, writes)
        if E.seen.get(sem, 0) < sem.cnt:
            E.h.wait_ge(sem.h, sem.cnt)
            E.seen[sem] = sem.cnt
        inst = E.h.dma_start(out=out, in_=in_, **kw)
        sem.cnt += 16
        inst.then_inc(sem.h, 16)
        self.ninst += 1
        self._stamp((sem, sem.cnt), reads, writes)
        return inst

    def barrier(self):
        sems = [e.sem for e in self.engs.values()] + list(self.dsems.values())
        for E in self.engs.values():
            for s in sems:
                if s is E.sem and not STRICT:
                    continue
                if s.cnt > 0 and E.seen.get(s, 0) < s.cnt:
                    E.h.wait_ge(s.h, s.cnt)
                    E.seen[s] = s.cnt
        self.bufs = {k: v for k, v in self.bufs.items() if isinstance(k, tuple) and k[0] in KEEP}


KEEP = ("u_bf", "v_bf", "x1_d", "ssdtm_d")


def rope_tables():
    half = 32
    inv = (10000.0 ** (-np.arange(0, half, 2, dtype=np.float32) / half)).astype(np.float32)
    t = np.arange(S)
    row = (t // 64).astype(np.float32)
    col = (t % 64).astype(np.float32)
    ar = row[:, None] * inv
    ac = col[:, None] * inv
    C = np.concatenate([np.cos(ar), np.cos(ar), np.cos(ac), np.cos(ac)], axis=1).astype(np.float32)
    Sg = np.concatenate([-np.sin(ar), np.sin(ar), -np.sin(ac), np.sin(ac)], axis=1).astype(np.float32)
    return C, Sg


def build(debug=None):
    debug = debug or ()
    nc = bass.Bass("TRN2", target_bir_lowering=False)
    es = contextlib.ExitStack()

    def din(name, shape, dt=F32):
        return nc.dram_tensor(name, list(shape), dt, kind="ExternalInput").ap()

    def dscratch(name, shape, dt):
        kind = "ExternalOutput" if name in debug else "Internal"
        return nc.dram_tensor(name, list(shape), dt, kind=kind).ap()

    x = din("x", [S, D])
    c_col = din("c_col", [128, 8])
    ada_w = din("ada_w", [D, 6 * D])
    ada_bT = din("ada_bT", [128, 48])
    n1T = din("n1T", [128, 8])
    n2T = din("n2T", [128, 8])
    w_in = din("w_in", [D, IN_W])
    ident_in = din("ident", [128, 128])
    ropeC_in = din("ropeC", [S, 64])
    ropeS_in = din("ropeS", [S, 64])
    qkgain_in = din("qkgain", [1, 640])
    convwT_in = din("convwT", [128, 12 * 5])
    convbT_in = din("convbT", [128, 12])
    trif_in = din("trif", [128, 128])
    trib_in = din("trib", [128, 128])
    maskf_in = din("maskf", [128, 128])
    maskb_in = din("maskb", [128, 128])
    ssdp_in = din("ssdp", [1, 80])
    ssdnw_in = din("ssdnw", [1, 1024])
    wq_in = din("wq", [D, 2048])
    keysT_in = din("keysT", [128, 2048])
    iota128_in = din("iota128", [128, 128])
    iota16_in = din("iota16", [128, 16])
    fnw_in = din("fnw", [1, D])
    U_in = din("U_h", [128 * 128, D])
    V_in = din("V_h", [128 * 128, D])
    h2T_d = dscratch("h2T_d", [D, S], BF16)
    u_bf = dscratch("u_bf", [64 * 128, 2 * D], BF16)
    v_bf = dscratch("v_bf", [64 * 128, 2 * D], BF16)
    wau_in = din("wau", [512, D])
    wsu_in = din("wsu", [D, D])
    wout_in = din("wout", [D, D])
    x1_d = dscratch("x1_d", [S, D], F32)
    attT_d = dscratch("attT_d", [512, S], BF16)
    xstm_d = dscratch("xstm_d", [S, 1024], BF16)
    yb_d = dscratch("yb_d", [S, 1024], F32)
    ssdtm_d = dscratch("ssdtm_d", [S, 1024], BF16)
    zs_d = dscratch("zs_d", [S, 1024], BF16)
    xbcT_d = dscratch("xbcT_d", [1536, S], BF16)
    gatesT_d = dscratch("gatesT_d", [2048, S], BF16)
    out = nc.dram_tensor("out", [S, D], F32, kind="ExternalOutput").ap()
    dbg_out = {}
    for name, shape, dt in [("d_modT", [128, 48], F32), ("d_hT", [128, 8 * S], BF16),
                            ("d_QT", [128, 4 * S], BF16), ("d_KT", [128, S], BF16), ("d_V", [128, NT * 130], BF16),
                            ("d_dtraw", [128, NT * 32], F32),
                            ("d_attm", [128, NT * 512], BF16),
                            ("d_ssdT", [128, 8 * S], BF16), ("d_h2T", [128, 8 * S], BF16), ("d_i1T", [128, S], BF16),
                            ("d_i2T", [128, S], BF16), ("d_gT", [128, S], BF16)]:
        if name in debug:
            dbg_out[name] = nc.dram_tensor(name, shape, dt, kind="ExternalOutput").ap()

    with es:
        cx = Ctx(nc, es)

        def sb(name, shape, dt):
            return es.enter_context(nc.sbuf_tensor("s_" + name, list(shape), dt))

        ident = sb("ident", [128, 128], F32)
        identb = sb("identb", [128, 128], BF16)
        ones_f = sb("ones_f", [128, 128], F32)
        cact = sb("cact", [128, 8], F32)
        modT = sb("modT", [128, 48], F32)
        abT = sb("abT", [128, 48], F32)
        n1 = sb("n1", [128, 8], F32)
        n2 = sb("n2", [128, 8], F32)
        gam1 = sb("gam1", [128, 8], F32)
        gam2 = sb("gam2", [128, 8], F32)
        hT = sb("hT", [128, 8, S], BF16)
        dtraw = sb("dtraw", [128, NT, 32], F32)
        mid = contextlib.ExitStack()

        def sbm(name, shape, dt):
            return mid.enter_context(nc.sbuf_tensor("m_" + name, list(shape), dt))
        QT = sbm("QT", [128, 4, S], BF16)
        KT = sbm("KT", [128, S], BF16)
        Vsb = sbm("Vsb", [128, NT, 2, 65], BF16)
        ropeC = sbm("ropeC", [128, NT, 64], F32)
        ropeS = sbm("ropeS", [128, NT, 64], F32)
        qkgain = sbm("qkgain", [128, 640], F32)
        convwT = sbm("convwT", [128, 12, 5], F32)
        convbT = sbm("convbT", [128, 12], F32)

        cx.dma("sync", "c0", ident[:], ident_in[:, :], writes=["ident"])
        cx.dma("sync", "c1", cact[:], c_col[:, :], writes=["cact"])
        cx.dma("sync", "c2", abT[:], ada_bT[:, :], writes=["abT"])
        cx.dma("sync", "c3", n1[:], n1T[:, :], writes=["n1"])
        cx.dma("sync", "c4", n2[:], n2T[:, :], writes=["n2"])
        cx.dma("sync", "c5", ropeC[:], ropeC_in.rearrange("(i p) j -> p i j", p=128), writes=["ropeC"])
        cx.dma("sync", "c6", ropeS[:], ropeS_in.rearrange("(i p) j -> p i j", p=128), writes=["ropeS"])
        cx.dma("sync", "c7", qkgain[:], qkgain_in.partition_broadcast(128), writes=["qkgain"])
        cx.dma("sync", "c8", convwT[:].rearrange("p a b -> p (a b)"), convwT_in[:, :], writes=["convwT"])
        cx.dma("sync", "c9", convbT[:], convbT_in[:, :], writes=["convbT"])
        cx.op("vector", lambda e: e.tensor_scalar(out=qkgain[:, 0:512], in0=qkgain[:, 0:512], scalar1=0.125, scalar2=None,
                                                  op0=ALU.mult), reads=["qkgain"], writes=["qkgain"])
        cx.op("vector", lambda e: e.memset(Vsb[:].rearrange("p a b c -> p (a b c)"), 1.0), writes=["Vsb"])
        cx.op("vector", lambda e: e.tensor_copy(out=identb[:], in_=ident[:]), reads=["ident"], writes=["identb"])
        cx.op("vector", lambda e: e.memset(ones_f[:], 1.0), writes=["ones_f"])
        cx.op("scalar", lambda e: e.activation(out=cact[:], in_=cact[:], func=AF.Silu), reads=["cact"], writes=["cact"])

        with contextlib.ExitStack() as st:
            aw = [st.enter_context(nc.sbuf_tensor("aw%d" % i, [128, 8, 512], F32)) for i in range(2)]
            psm = st.enter_context(nc.psum_tensor("psm", [128, 48], F32))
            awv = ada_w.rearrange("(kc p) n -> p kc n", p=128)
            for blk in range(12):
                t = aw[blk % 2]
                key = "aw%d" % (blk % 2)
                cx.dma("sync", key, t[:], awv[:, :, blk * 512:(blk + 1) * 512], writes=[key])
                for jj in range(4):
                    j = blk * 4 + jj
                    for kc in range(8):
                        cx.op("tensor", lambda e, t=t, jj=jj, kc=kc, j=j: e.matmul(
                            psm[:, j:j + 1], lhsT=t[:, kc, jj * 128:(jj + 1) * 128], rhs=cact[:, kc:kc + 1],
                            start=(kc == 0), stop=(kc == 7)), reads=[key, "cact"], writes=["psm"])
            cx.op("vector", lambda e: e.tensor_tensor(out=modT[:], in0=psm[:], in1=abT[:], op=ALU.add),
                  reads=["psm", "abT"], writes=["modT"])
            cx.op("vector", lambda e: e.scalar_tensor_tensor(out=gam1[:], in0=modT[:, 8:16], scalar=1.0, in1=n1[:],
                                                             op0=ALU.add, op1=ALU.mult),
                  reads=["modT", "n1"], writes=["gam1"])
            cx.op("vector", lambda e: e.scalar_tensor_tensor(out=gam2[:], in0=modT[:, 32:40], scalar=1.0, in1=n2[:],
                                                             op0=ALU.add, op1=ALU.mult),
                  reads=["modT", "n2"], writes=["gam2"])
            if "d_modT" in debug:
                cx.dma("sync", "dbg", dbg_out["d_modT"][:, :], modT[:], reads=["modT"])
            s0_scope = st.pop_all()

        def norm_mod_transpose(src, gam, shT, st, pfx, rkey=None, gkeys=()):
            xt = [st.enter_context(nc.sbuf_tensor(pfx + "xt%d" % i, [128, D], F32)) for i in range(2)]
            xn = [st.enter_context(nc.sbuf_tensor(pfx + "xn%d" % i, [128, D], BF16)) for i in range(2)]
            junk = st.enter_context(nc.sbuf_tensor(pfx + "junk", [128, D], BF16))
            ssq = st.enter_context(nc.sbuf_tensor(pfx + "ssq", [128, NT], F32))
            rstd = st.enter_context(nc.sbuf_tensor(pfx + "rstd", [128, NT], F32))
            pst = [st.enter_context(nc.psum_tensor(pfx + "pst%d" % i, [128, 8, 128], BF16)) for i in range(2)]
            for i in range(NT):
                p = i % 2
                kx, kn, kp = pfx + "xt%d" % p, pfx + "xn%d" % p, pfx + "pst%d" % p
                cx.dma("sync", kx, xt[p][:], src[i * 128:(i + 1) * 128, :], reads=([(rkey, i)] if rkey else []), writes=[kx])
                cx.op("scalar", lambda e, p=p, i=i: e.activation(out=junk[:], in_=xt[p][:], func=AF.Square,
                                                                 accum_out=ssq[:, i:i + 1]),
                      reads=[kx], writes=[pfx + "junk", (pfx + "ssq", i)])
                cx.op("vector", lambda e, i=i: e.tensor_scalar(out=rstd[:, i:i + 1], in0=ssq[:, i:i + 1],
                                                               scalar1=1.0 / D, scalar2=EPS, op0=ALU.mult, op1=ALU.add),
                      reads=[(pfx + "ssq", i)], writes=[(pfx + "rstd", i)])
                cx.op("scalar", lambda e, i=i: e.activation(out=rstd[:, i:i + 1], in_=rstd[:, i:i + 1], func=AF.Sqrt),
                      reads=[(pfx + "rstd", i)], writes=[(pfx + "rstd", i)])
                cx.op("vector", lambda e, i=i: e.reciprocal(out=rstd[:, i:i + 1], in_=rstd[:, i:i + 1]),
                      reads=[(pfx + "rstd", i)], writes=[(pfx + "rstd", i)])
                cx.op("vector", lambda e, p=p, i=i: e.tensor_scalar(out=xn[p][:], in0=xt[p][:], scalar1=rstd[:, i:i + 1],
                                                                    scalar2=None, op0=ALU.mult),
                      reads=[kx, (pfx + "rstd", i)], writes=[kn])
                for c in range(8):
                    cx.op("tensor", lambda e, p=p, c=c: e.transpose(out=pst[p][:, c, :], in_=xn[p][:, c * 128:(c + 1) * 128],
                                                                    identity=identb[:]),
                          reads=[kn, "identb"], writes=[kp])
                for c in range(8):
                    cx.op("scalar", lambda e, p=p, c=c, i=i: e.activation(
                        out=hT[:, c, i * 128:(i + 1) * 128], in_=pst[p][:, c, :], func=AF.Identity,
                        scale=gam[:, c:c + 1], bias=shT[:, c:c + 1]),
                        reads=[kp] + list(gkeys), writes=[("hT", i)])

        with contextlib.ExitStack() as st:
            norm_mod_transpose(x, gam1, modT[:, 0:8], st, "a", gkeys=["gam1", "modT"])
            if "d_hT" in debug:
                cx.dma("sync", "dbg", dbg_out["d_hT"][:, :], hT[:].rearrange("p c s -> p (c s)"),
                       reads=[("hT", i) for i in range(NT)])
            cx.barrier()
        s0_scope.close()


        wv = w_in.rearrange("(kc p) n -> p kc n", p=128)
        with contextlib.ExitStack() as st:
            wA = st.enter_context(nc.sbuf_tensor("wA", [128, 8, 800], BF16))
            wZ = st.enter_context(nc.sbuf_tensor("wZ", [128, 8, 1024], BF16))
            psA = [st.enter_context(nc.psum_tensor("psA%d" % i, [128, 1024], F32)) for i in range(2)]
            psZ = st.enter_context(nc.psum_tensor("psZ", [128, 1024], F32))
            pT = st.enter_context(nc.psum_tensor("pT", [128, 5, 128], BF16))
            qkv = [st.enter_context(nc.sbuf_tensor("qkv%d" % i, [128, 800], F32)) for i in range(2)]
            sq = st.enter_context(nc.sbuf_tensor("sq", [128, 640], F32))
            qn = st.enter_context(nc.sbuf_tensor("qn", [128, 640], F32))
            qa = st.enter_context(nc.sbuf_tensor("qa", [128, 640], F32))
            qb = st.enter_context(nc.sbuf_tensor("qb", [128, 640], F32))
            qr = st.enter_context(nc.sbuf_tensor("qr", [128, 640], BF16))
            ss = st.enter_context(nc.sbuf_tensor("ss10", [128, 10], F32))
            zst = [st.enter_context(nc.sbuf_tensor("zst%d" % i, [128, 1024], BF16)) for i in range(2)]
            cx.dma("gpsimd", "wA", wA[:], wv[:, :, 0:800], writes=["wA"])
            cx.dma("gpsimd", "wZ", wZ[:], wv[:, :, 800:1824], writes=["wZ"])
            for i in range(NT):
                p = i % 2
                tsl = slice(i * 128, (i + 1) * 128)
                kA, kq = "psA%d" % p, "qkv%d" % p
                for (c0, c1) in [(0, 512), (512, 800)]:
                    for kc in range(8):
                        cx.op("tensor", lambda e, p=p, c0=c0, c1=c1, kc=kc: e.matmul(
                            psA[p][:, c0:c1], lhsT=hT[:, kc, tsl], rhs=wA[:, kc, c0:c1], start=(kc == 0), stop=(kc == 7)),
                            reads=[("hT", i), "wA"], writes=[kA])
                for (c0, c1) in [(0, 512), (512, 1024)]:
                    for kc in range(8):
                        cx.op("tensor", lambda e, c0=c0, c1=c1, kc=kc: e.matmul(
                            psZ[:, c0:c1], lhsT=hT[:, kc, tsl], rhs=wZ[:, kc, c0:c1], start=(kc == 0), stop=(kc == 7)),
                            reads=[("hT", i), "wZ"], writes=["psZ"])
                cx.op("scalar", lambda e, p=p: e.activation(out=qkv[p][:], in_=psA[p][:, 0:800], func=AF.Copy),
                      reads=[kA], writes=[kq])
                kz = "zst%d" % p
                cx.op("scalar", lambda e, p=p: e.activation(out=zst[p][:], in_=psZ[:], func=AF.Silu),
                      reads=["psZ"], writes=[kz])
                cx.dma("sync", kz, zs_d[tsl, :], zst[p][:], reads=[kz], writes=[("zs_d", i)])
                Q = qkv[p]
                cx.op("vector", lambda e, Q=Q: e.tensor_tensor(out=sq[:], in0=Q[:, 0:640], in1=Q[:, 0:640], op=ALU.mult),
                      reads=[kq], writes=["sq"])
                cx.op("vector", lambda e: e.tensor_reduce(out=ss[:], in_=sq[:].rearrange("p (h d) -> p h d", d=64),
                                                          axis=AX.X, op=ALU.add), reads=["sq"], writes=["ss10"])
                cx.op("vector", lambda e: e.tensor_scalar(out=ss[:], in0=ss[:], scalar1=1.0 / 64, scalar2=EPS,
                                                          op0=ALU.mult, op1=ALU.add), reads=["ss10"], writes=["ss10"])
                cx.op("scalar", lambda e: e.activation(out=ss[:], in_=ss[:], func=AF.Sqrt), reads=["ss10"], writes=["ss10"])
                cx.op("vector", lambda e: e.reciprocal(out=ss[:], in_=ss[:]), reads=["ss10"], writes=["ss10"])
                cx.op("vector", lambda e, Q=Q: e.tensor_tensor(
                    out=qn[:].rearrange("p (h d) -> p h d", d=64), in0=Q[:, 0:640].rearrange("p (h d) -> p h d", d=64),
                    in1=ss[:].unsqueeze(2).to_broadcast([128, 10, 64]), op=ALU.mult), reads=[kq, "ss10"], writes=["qn"])
                cx.op("vector", lambda e: e.tensor_tensor(out=qn[:], in0=qn[:], in1=qkgain[:], op=ALU.mult),
                      reads=["qn", "qkgain"], writes=["qn"])
                cx.op("vector", lambda e, i=i: e.tensor_tensor(
                    out=qa[:].rearrange("p (h d) -> p h d", d=64), in0=qn[:].rearrange("p (h d) -> p h d", d=64),
                    in1=ropeC[:, i, :].unsqueeze(1).to_broadcast([128, 10, 64]), op=ALU.mult),
                    reads=["qn", "ropeC"], writes=["qa"])
                for blk in range(2):
                    for hh in range(2):
                        o0 = blk * 32 + hh * 16
                        i0 = blk * 32 + (1 - hh) * 16
                        cx.op("vector", lambda e, i=i, o0=o0, i0=i0: e.tensor_tensor(
                            out=qb[:].rearrange("p (h d) -> p h d", d=64)[:, :, o0:o0 + 16],
                            in0=qn[:].rearrange("p (h d) -> p h d", d=64)[:, :, i0:i0 + 16],
                            in1=ropeS[:, i, o0:o0 + 16].unsqueeze(1).to_broadcast([128, 10, 16]), op=ALU.mult),
                            reads=["qn", "ropeS"], writes=["qb"])
                cx.op("vector", lambda e: e.tensor_tensor(out=qr[:], in0=qa[:], in1=qb[:], op=ALU.add),
                      reads=["qa", "qb"], writes=["qr"])
                for j in range(5):
                    cx.op("tensor", lambda e, j=j: e.transpose(out=pT[:, j, :], in_=qr[:, j * 128:(j + 1) * 128],
                                                               identity=identb[:]), reads=["qr", "identb"], writes=["pT"])
                cx.op("scalar", lambda e: e.activation(out=QT[:, :, tsl], in_=pT[:, 0:4, :], func=AF.Copy),
                      reads=["pT"], writes=[("QT", i)])
                cx.op("scalar", lambda e: e.activation(out=KT[:, tsl], in_=pT[:, 4, :], func=AF.Copy),
                      reads=["pT"], writes=[("KT", i)])
                cx.op("vector", lambda e, Q=Q, i=i: e.tensor_copy(
                    out=Vsb[:, i, :, 0:64], in_=Q[:, 640:768].rearrange("p (h d) -> p h d", d=64)),
                    reads=[kq, "Vsb"], writes=[("Vsb", i)])
                cx.op("vector", lambda e, Q=Q, i=i: e.tensor_copy(out=dtraw[:, i, :], in_=Q[:, 768:800]),
                      reads=[kq], writes=[("dtraw", i)])
            cx.barrier()
        with contextlib.ExitStack() as st:
            wF = [st.enter_context(nc.sbuf_tensor("wF%d" % i, [128, 8, 512], BF16)) for i in range(2)]
            psF = [st.enter_context(nc.psum_tensor("psF%d" % i, [128, 512], F32)) for i in range(4)]
            stage = st.enter_context(nc.sbuf_tensor("stage", [128, S + 4], F32))
            acc = st.enter_context(nc.sbuf_tensor("acc", [128, S], F32))
            ob = [st.enter_context(nc.sbuf_tensor("ob%d" % i, [128, S], BF16)) for i in range(2)]
            cx.op("vector", lambda e: e.memset(stage[:], 0.0), writes=["stage"])
            n = 0
            for j in range(28):
                isx = j < 12
                jj = j if isx else j - 12
                bb = j // 4
                W = wF[bb % 2]
                kW = "wF%d" % (bb % 2)
                if j % 4 == 0:
                    cx.dma("gpsimd", kW, W[:], wv[:, :, 1824 + bb * 512:1824 + (bb + 1) * 512], writes=[kW])
                jw = j % 4
                o = ob[j % 2]
                ko = "ob%d" % (j % 2)
                for g in range(8):
                    b = n % 4
                    n += 1
                    gs = slice(g * 512, (g + 1) * 512)
                    for kc in range(8):
                        cx.op("tensor", lambda e, b=b, kc=kc, W=W, jw=jw, gs=gs: e.matmul(
                            psF[b][:], lhsT=W[:, kc, jw * 128:(jw + 1) * 128], rhs=hT[:, kc, gs],
                            start=(kc == 0), stop=(kc == 7)),
                            reads=["hTall", kW], writes=["psF%d" % b])
                    if isx:
                        cx.op("scalar", lambda e, b=b, g=g: e.activation(out=stage[:, 2 + g * 512:2 + (g + 1) * 512],
                                                                         in_=psF[b][:], func=AF.Copy),
                              reads=["psF%d" % b, "acc0", "stage"], writes=[("stage", g)])
                    else:
                        cx.op("scalar", lambda e, b=b, gs=gs, o=o: e.activation(out=o[:, gs], in_=psF[b][:], func=AF.Sigmoid),
                              reads=["psF%d" % b], writes=[ko])
                if isx:
                    srd = [("stage", g) for g in range(8)] + ["stage"]
                    cx.op("vector", lambda e, jj=jj: e.tensor_scalar(
                        out=acc[:], in0=stage[:, 0:S], scalar1=convwT[:, jj, 0:1], scalar2=convbT[:, jj:jj + 1],
                        op0=ALU.mult, op1=ALU.add), reads=srd + ["convwT", "convbT"], writes=["acc"])
                    for k in range(1, 5):
                        cx.op("vector", lambda e, jj=jj, k=k: e.scalar_tensor_tensor(
                            out=acc[:], in0=stage[:, k:k + S], scalar=convwT[:, jj, k:k + 1], in1=acc[:],
                            op0=ALU.mult, op1=ALU.add), reads=srd + ["acc"], writes=["acc"] + (["acc0"] if k == 4 else []))
                    cx.op("scalar", lambda e, o=o: e.activation(out=o[:], in_=acc[:], func=AF.Silu),
                          reads=["acc"], writes=[ko])
                    cx.dma("sync", ko, xbcT_d[jj * 128:(jj + 1) * 128, :], o[:], reads=[ko], writes=[("xbcT_d", jj)])
                else:
                    cx.dma("sync", ko, gatesT_d[jj * 128:(jj + 1) * 128, :], o[:], reads=[ko], writes=[("gatesT_d", jj)])
            for nm, t in [("d_QT", QT), ("d_KT", KT), ("d_V", Vsb), ("d_dtraw", dtraw)]:
                if nm in debug:
                    flat = t[:] if len(t.shape) == 2 else (
                        t[:].rearrange("p a b -> p (a b)") if len(t.shape) == 3 else t[:].rearrange("p a b c -> p (a b c)"))
                    cx.dma("sync", "dbg", dbg_out[nm][:, :], flat)
            cx.barrier()


        with contextlib.ExitStack() as st:
            psS = [st.enter_context(nc.psum_tensor("psS%d" % i, [128, 512], F32)) for i in range(4)]
            psO = [st.enter_context(nc.psum_tensor("psO%d" % i, [128, 512], F32)) for i in range(2)]
            pTr = st.enter_context(nc.psum_tensor("pTr", [128, 4, 65], F32))
            PT = [st.enter_context(nc.sbuf_tensor("PT%d" % i, [128, 512], BF16)) for i in range(4)]
            oT = [st.enter_context(nc.sbuf_tensor("oT%d" % i, [128, 512], F32)) for i in range(2)]
            attm = st.enter_context(nc.sbuf_tensor("attm", [128, NT, 512], BF16))
            rden = st.enter_context(nc.sbuf_tensor("rden", [128, 4], F32))
            pTa = st.enter_context(nc.psum_tensor("pTa", [128, 4, 128], BF16))
            attT = hT
            cu = [st.enter_context(nc.sbuf_tensor("cu%d" % i, [128, 2, D], BF16)) for i in range(2)]
            cv = [st.enter_context(nc.sbuf_tensor("cv%d" % i, [128, 2, D], BF16)) for i in range(2)]
            for jj in range(64):
                p = jj % 2
                rs = slice(jj * 256, (jj + 1) * 256)
                cx.dma("gpsimd", "cu%d" % p, cu[p][:], U_in[rs, :].rearrange("(a p) n -> p a n", p=128), writes=["cu%d" % p])
                cx.dma("sync", "cuo%d" % p, u_bf[jj * 128:(jj + 1) * 128, :], cu[p][:].rearrange("p a n -> p (a n)"), reads=["cu%d" % p],
                       writes=[("u_bf", 2 * jj), ("u_bf", 2 * jj + 1)])
                cx.dma("gpsimd", "cv%d" % p, cv[p][:], V_in[rs, :].rearrange("(a p) n -> p a n", p=128), writes=["cv%d" % p])
                cx.dma("sync", "cvo%d" % p, v_bf[jj * 128:(jj + 1) * 128, :], cv[p][:].rearrange("p a n -> p (a n)"), reads=["cv%d" % p],
                       writes=[("v_bf", 2 * jj), ("v_bf", 2 * jj + 1)])
            steps = [(j, qg, kt) for j in range(4) for qg in range(8) for kt in range(NT)]

            def emit_qk(n):
                j, qg, kt = steps[n]
                for grp in range(2):
                    b = 2 * (n % 2) + grp
                    ps_ = slice(grp * 64, (grp + 1) * 64)
                    cx.op("tensor", lambda e, b=b, ps_=ps_: e.matmul(
                        psS[b][:], lhsT=KT[ps_, kt * 128:(kt + 1) * 128], rhs=QT[ps_, j, qg * 512:(qg + 1) * 512],
                        start=True, stop=True), reads=["QT", "KT"], writes=["psS%d" % b])

            emit_qk(0)
            for n, (j, qg, kt) in enumerate(steps):
                if n + 1 < len(steps):
                    emit_qk(n + 1)
                for grp in range(2):
                    b = 2 * (n % 2) + grp
                    cx.op("scalar", lambda e, b=b: e.activation(out=PT[b][:], in_=psS[b][:], func=AF.Exp),
                          reads=["psS%d" % b], writes=["PT%d" % b])
                for grp in range(2):
                    b = 2 * (n % 2) + grp
                    cx.op("tensor", lambda e, b=b, kt=kt, grp=grp: e.matmul(
                        psO[grp][0:65, :], lhsT=Vsb[:, kt, grp, :], rhs=PT[b][:],
                        start=(kt == 0), stop=(kt == NT - 1)), reads=["PT%d" % b, "Vsb"], writes=["psO%d" % grp])
                if kt == NT - 1:
                    for grp in range(2):
                        pos = 2 * j + grp
                        cx.op("scalar", lambda e, grp=grp: e.activation(out=oT[grp][0:65, :], in_=psO[grp][0:65, :], func=AF.Copy),
                              reads=["psO%d" % grp], writes=["oT%d" % grp])
                        for sub in range(4):
                            cx.op("tensor", lambda e, grp=grp, sub=sub: e.transpose(
                                out=pTr[:, sub, :], in_=oT[grp][0:65, sub * 128:(sub + 1) * 128], identity=ident[0:65, 0:65]),
                                reads=["oT%d" % grp, "ident"], writes=["pTr"])
                        cx.op("vector", lambda e: e.reciprocal(out=rden[:].unsqueeze(2), in_=pTr[:, :, 64:65]),
                              reads=["pTr"], writes=["rden"])
                        cx.op("vector", lambda e, qg=qg, pos=pos: e.tensor_tensor(
                            out=attm[:, qg * 4:(qg + 1) * 4, pos * 64:(pos + 1) * 64], in0=pTr[:, :, 0:64],
                            in1=rden[:].unsqueeze(2).to_broadcast([128, 4, 64]), op=ALU.mult),
                            reads=["pTr", "rden"], writes=[("attm", qg * 4 + k) for k in range(4)])
            if "d_attm" in debug:
                cx.dma("sync", "dbg", dbg_out["d_attm"][:, :], attm[:].rearrange("p a b -> p (a b)"),
                       reads=[("attm", ti) for ti in range(NT)])
            for i in range(NT):
                for c in range(4):
                    cx.op("tensor", lambda e, i=i, c=c: e.transpose(out=pTa[:, c, :], in_=attm[:, i, c * 128:(c + 1) * 128],
                                                                    identity=identb[:]), reads=[("attm", i)], writes=["pTa"])
                cx.op("scalar", lambda e, i=i: e.activation(out=attT[:, 0:4, i * 128:(i + 1) * 128], in_=pTa[:], func=AF.Copy),
                      reads=["pTa"], writes=[("attT", i)])
            for c in range(4):
                cx.dma("sync", "attTd", attT_d[c * 128:(c + 1) * 128, :], attT[:, c, :],
                       reads=[("attT", i) for i in range(NT)], writes=["attT_d"])
            cx.barrier()
        mid.close()


        ssdT = hT
        with contextlib.ExitStack() as st:
            def T(name, shape, dt):
                return st.enter_context(nc.sbuf_tensor("y_" + name, list(shape), dt))
            trif = T("trif", [128, 128], F32); trib = T("trib", [128, 128], F32)
            ssdp = T("ssdp", [128, 80], F32)
            ssdnw = T("ssdnw", [128, 1024], F32)
            dtv = T("dtv", [128, NT, 32], F32)
            Adt = T("Adt", [128, NT, 32], F32)
            BT = T("BT", [128, 2, S], BF16)
            CT = T("CT", [128, 2, S], BF16)
            Btm = T("Btm", [128, NT, 2, 128], BF16)
            cx.dma("sync", "k0", trif[:], trif_in[:, :], writes=["trif"])
            cx.dma("sync", "k1", trib[:], trib_in[:, :], writes=["trib"])
            cx.dma("sync", "k4", ssdp[:], ssdp_in.partition_broadcast(128), writes=["ssdp"])
            cx.dma("sync", "k5", ssdnw[:], ssdnw_in.partition_broadcast(128), writes=["ssdnw"])
            for g in range(2):
                cx.dma("sync", "k6", BT[:, g, :], xbcT_d[1024 + g * 128:1024 + (g + 1) * 128, :], writes=["BT"])
                cx.dma("sync", "k7", CT[:, g, :], xbcT_d[1280 + g * 128:1280 + (g + 1) * 128, :], writes=["CT"])
            cx.op("vector", lambda e: e.tensor_tensor(out=dtv[:], in0=dtraw[:], in1=ssdp[:, 0:32].unsqueeze(1).to_broadcast([128, NT, 32]),
                                                      op=ALU.add), reads=["ssdp"], writes=["dtv"])
            cx.op("scalar", lambda e: e.activation(out=dtv[:], in_=dtv[:], func=AF.Exp), reads=["dtv"], writes=["dtv"])
            cx.op("scalar", lambda e: e.activation(out=dtv[:], in_=dtv[:], func=AF.Ln, bias=1.0), reads=["dtv"], writes=["dtv"])
            cx.op("scalar", lambda e: e.activation(out=ssdp[:, 32:64], in_=ssdp[:, 32:64], func=AF.Exp), reads=["ssdp"], writes=["ssdp"])
            cx.op("vector", lambda e: e.scalar_tensor_tensor(out=Adt[:], in0=dtv[:], scalar=-1.0,
                                                             in1=ssdp[:, 32:64].unsqueeze(1).to_broadcast([128, NT, 32]),
                                                             op0=ALU.mult, op1=ALU.mult), reads=["dtv", "ssdp"], writes=["Adt"])
            with contextlib.ExitStack() as st2:
                pT4 = [st2.enter_context(nc.psum_tensor("pT4%d" % i, [128, 4, 128], BF16)) for i in range(2)]
                xch = [st2.enter_context(nc.sbuf_tensor("y_xch%d" % i, [128, S], BF16)) for i in range(2)]
                xst = [st2.enter_context(nc.sbuf_tensor("y_xst%d" % i, [128, 1024], BF16)) for i in range(2)]
                n = 0
                for g in range(2):
                    for i0 in range(0, NT, 4):
                        b = n % 2; n += 1
                        for k in range(4):
                            cx.op("tensor", lambda e, b=b, k=k, g=g, i0=i0: e.transpose(
                                out=pT4[b][:, k, :], in_=BT[:, g, (i0 + k) * 128:(i0 + k + 1) * 128], identity=identb[:]),
                                reads=["BT"], writes=["pT4%d" % b])
                        cx.op("scalar", lambda e, b=b, g=g, i0=i0: e.activation(out=Btm[:, i0:i0 + 4, g, :], in_=pT4[b][:], func=AF.Copy),
                              reads=["pT4%d" % b], writes=["Btm"])
                for i in range(NT):
                    cx.buf(("xstm", i))
                for c in range(8):
                    X = xch[c % 2]; kx = "xch%d" % (c % 2)
                    cx.dma("sync", kx, X[:], xbcT_d[c * 128:(c + 1) * 128, :], writes=[kx])
                    for i0 in range(0, NT, 4):
                        b = n % 2; n += 1
                        for k in range(4):
                            cx.op("tensor", lambda e, b=b, k=k, X=X, i0=i0: e.transpose(
                                out=pT4[b][:, k, :], in_=X[:, (i0 + k) * 128:(i0 + k + 1) * 128], identity=identb[:]),
                                reads=[kx], writes=["pT4%d" % b])
                        q = (i0 // 4) % 2
                        cx.op("scalar", lambda e, b=b, q=q: e.activation(out=xst[q][:, 0:512].rearrange("p (a b) -> p a b", a=4),
                                                                         in_=pT4[b][:], func=AF.Copy),
                              reads=["pT4%d" % b], writes=["xst%d" % q])
                        cx.dma("sync", "xst%d" % q,
                               xstm_d[i0 * 128:(i0 + 4) * 128, c * 128:(c + 1) * 128].rearrange("(a p) n -> p a n", p=128),
                               xst[q][:, 0:512].rearrange("p (a b) -> p a b", a=4),
                               reads=["xst%d" % q], writes=[("xstm", i0 + k) for k in range(4)])
                cx.barrier()

            psCBA = st.enter_context(nc.psum_tensor("psCBA", [128, 3, 128], F32))
            psBC = [st.enter_context(nc.psum_tensor("psBC%d" % g, [128, 8, 128], F32)) for g in range(2)]
            psY = [st.enter_context(nc.psum_tensor("psY%d" % g, [128, 512], F32)) for g in range(2)]
            psYS = st.enter_context(nc.psum_tensor("psYS", [128, 512], F32))
            psAcs = psCBA[:, 2, 0:16]
            Acs = T("Acs", [128, 16], F32)
            expA = T("expA", [128, 16], F32)
            rhs2 = [T("rhs2%d" % g, [128, 8, 128], F32) for g in range(2)]
            d1 = [T("d1%d" % g, [128, 8, 128], F32) for g in range(2)]
            LT = [T("LT%d" % g, [128, 8, 128], F32) for g in range(2)]
            Mb = [T("Mb%d" % g, [128, 8, 128], BF16) for g in range(2)]
            cbm = [T("cbm%d" % g, [128, 128], F32) for g in range(2)]
            Xb = T("Xb", [128, 1024], BF16)
            Xd = [T("Xd%d" % g, [128, 512], BF16) for g in range(2)]
            dec = [T("dec%d" % g, [128, 8], F32) for g in range(2)]
            cd = [T("cd%d" % g, [128, 8], F32) for g in range(2)]
            tmpy = [T("tmpy%d" % g, [128, 512], F32) for g in range(2)]
            ydir = [T("ydir%d" % i, [128, 1024], F32) for i in range(2)]
            state = [T("state%d" % g, [128, 512], F32) for g in range(2)]
            stbf = [T("stbf%d" % g, [128, 512], BF16) for g in range(2)]
            xs_t = [T("xs_t%d" % i, [128, 1024], BF16) for i in range(2)]
            zs_t = [T("zs_t%d" % i, [128, 1024], BF16) for i in range(2)]
            yb_t = [T("yb_t%d" % i, [128, 1024], F32) for i in range(2)]
            gg = T("gg", [128, 1024], F32)
            junk2 = T("junk2", [128, 1024], BF16)
            ssq = T("ssq1", [128, 1], F32)
            ssdtm = [T("ssdtm0", [128, 1024], BF16)] * 2

            def ssd_pass(fwd):
                o = 0 if fwd else 16
                tri = trif if fwd else trib
                ktri = "trif" if fwd else "trib"
                lend = 127 if fwd else 0
                order = list(range(NT)) if fwd else list(range(NT - 1, -1, -1))
                for idx, i in enumerate(order):
                    p = idx % 2
                    tsl = slice(i * 128, (i + 1) * 128)
                    kxs = "xs_t%d" % p
                    cx.dma("sync", kxs, xs_t[p][:], xstm_d[tsl, :], reads=[("xstm", i)], writes=[kxs])
                    if fwd:
                        cx.dma("sync", "zs_t%d" % p, zs_t[p][:], zs_d[tsl, :], writes=["zs_t%d" % p])
                        cx.dma("sync", "yb_t%d" % p, yb_t[p][:], yb_d[tsl, :], reads=[("yb_d", i)], writes=["yb_t%d" % p])
                    cx.op("tensor", lambda e, i=i: e.matmul(psAcs, lhsT=tri[:], rhs=Adt[:, i, o:o + 16], start=True, stop=True),
                          reads=[ktri, "Adt"], writes=["psAcs"])
                    cx.op("vector", lambda e: e.tensor_copy(out=Acs[:], in_=psAcs), reads=["psAcs"], writes=["Acs"])
                    cx.op("scalar", lambda e: e.activation(out=expA[:], in_=psAcs, func=AF.Exp), reads=["psAcs"], writes=["expA"])
                    cx.op("vector", lambda e, p=p, i=i: e.tensor_tensor(
                        out=Xb[:].rearrange("p (h d) -> p h d", d=64), in0=xs_t[p][:].rearrange("p (h d) -> p h d", d=64),
                        in1=dtv[:, i, o:o + 16].unsqueeze(2).to_broadcast([128, 16, 64]), op=ALU.mult),
                        reads=[kxs, "dtv"], writes=["Xb"])
                    yd = ydir[p]; kyd = "ydir%d" % p
                    for g in range(2):
                        hs = slice(g * 8, (g + 1) * 8)
                        G = str(g)
                        cx.op("vector", lambda e, i=i, g=g: e.tensor_tensor(
                            out=rhs2[g][:], in0=tri[:].unsqueeze(1).to_broadcast([128, 8, 128]),
                            in1=Adt[:, i, o + g * 8:o + g * 8 + 8].unsqueeze(2).to_broadcast([128, 8, 128]), op=ALU.mult),
                            reads=[ktri, "Adt"], writes=["rhs2" + G])
                        for hh in range(2):
                            cx.op("tensor", lambda e, hh=hh, g=g: e.matmul(psBC[g][:, hh * 4:(hh + 1) * 4, :], lhsT=ones_f[:],
                                                                           rhs=rhs2[g][:, hh * 4:(hh + 1) * 4, :], start=True, stop=True),
                                  reads=["rhs2" + G, "ones_f"], writes=["psBC" + G])
                        cx.op("tensor", lambda e, g=g: e.matmul(psCBA[:, g, :], lhsT=BT[:, g, tsl], rhs=CT[:, g, tsl], start=True, stop=True),
                              reads=["BT", "CT"], writes=["psCB" + G])
                    for g in range(2):
                        hs = slice(g * 8, (g + 1) * 8)
                        G = str(g)
                        cx.op("vector", lambda e, g=g: e.tensor_tensor(out=cbm[g][:], in0=psCBA[:, g, :], in1=tri[:], op=ALU.mult),
                              reads=["psCB" + G, ktri], writes=["cbm" + G])
                        cx.op("vector", lambda e, hs=hs, g=g: e.tensor_tensor(
                            out=d1[g][:], in0=psBC[g][:], in1=Acs[:, hs].unsqueeze(2).to_broadcast([128, 8, 128]), op=ALU.subtract),
                            reads=["psBC" + G, "Acs"], writes=["d1" + G])
                        cx.op("scalar", lambda e, g=g: e.activation(out=LT[g][:], in_=d1[g][:], func=AF.Exp), reads=["d1" + G], writes=["LT" + G])
                        if idx < NT - 1:
                            cx.op("vector", lambda e, hs=hs, g=g: e.tensor_tensor(
                                out=dec[g][:].unsqueeze(2), in0=psBC[g][:, :, lend:lend + 1], in1=Acs[:, hs].unsqueeze(2), op=ALU.subtract),
                                reads=["psBC" + G, "Acs"], writes=["dec" + G])
                            cx.op("scalar", lambda e, g=g: e.activation(out=dec[g][:], in_=dec[g][:], func=AF.Exp), reads=["dec" + G], writes=["dec" + G])
                            cx.op("scalar", lambda e, g=g: e.activation(out=cd[g][:].unsqueeze(2), in_=psBC[g][:, :, lend:lend + 1], func=AF.Exp),
                                  reads=["psBC" + G], writes=["cd" + G])
                    for g in range(2):
                        G = str(g)
                        gsl = slice(g * 512, (g + 1) * 512)
                        cx.op("vector", lambda e, g=g: e.scalar_tensor_tensor(
                            out=Mb[g][:], in0=LT[g][:], scalar=1.0, in1=cbm[g][:].unsqueeze(1).to_broadcast([128, 8, 128]),
                            op0=ALU.min, op1=ALU.mult), reads=["LT" + G, "cbm" + G], writes=["Mb" + G])
                        for h in range(8):
                            cx.op("tensor", lambda e, h=h, g=g: e.matmul(
                                psY[g][:, h * 64:(h + 1) * 64], lhsT=Mb[g][:, h, :], rhs=Xb[:, (g * 8 + h) * 64:(g * 8 + h + 1) * 64],
                                start=True, stop=True), reads=["Mb" + G, "Xb"], writes=["psY" + G])
                        if idx < NT - 1:
                            cx.op("vector", lambda e, gsl=gsl, g=g: e.tensor_tensor(
                                out=Xd[g][:].rearrange("p (h d) -> p h d", d=64), in0=Xb[:, gsl].rearrange("p (h d) -> p h d", d=64),
                                in1=dec[g][:].unsqueeze(2).to_broadcast([128, 8, 64]), op=ALU.mult), reads=["Xb", "dec" + G], writes=["Xd" + G])
                    for g in range(2):
                        hs = slice(g * 8, (g + 1) * 8)
                        G = str(g)
                        gsl = slice(g * 512, (g + 1) * 512)
                        if idx > 0:
                            cx.op("tensor", lambda e, g=g: e.matmul(psYS[:], lhsT=CT[:, g, tsl], rhs=stbf[g][:], start=True, stop=True),
                                  reads=["CT", "stbf" + G], writes=["psYS"])
                            cx.op("vector", lambda e, hs=hs, g=g: e.tensor_tensor(
                                out=tmpy[g][:].rearrange("p (h d) -> p h d", d=64), in0=psYS[:].rearrange("p (h d) -> p h d", d=64),
                                in1=expA[:, hs].unsqueeze(2).to_broadcast([128, 8, 64]), op=ALU.mult),
                                reads=["psYS", "expA"], writes=["tmpy" + G])
                            cx.op("vector", lambda e, yd=yd, gsl=gsl, g=g: e.tensor_tensor(out=yd[:, gsl], in0=tmpy[g][:], in1=psY[g][:], op=ALU.add),
                                  reads=["tmpy" + G, "psY" + G], writes=[(kyd, g)])
                        else:
                            cx.op("vector", lambda e, yd=yd, gsl=gsl, g=g: e.tensor_copy(out=yd[:, gsl], in_=psY[g][:]),
                                  reads=["psY" + G], writes=[(kyd, g)])
                        if idx < NT - 1:
                            cx.op("tensor", lambda e, i=i, g=g: e.matmul(psYS[:], lhsT=Btm[:, i, g, :], rhs=Xd[g][:], start=True, stop=True),
                                  reads=["Btm", "Xd" + G], writes=["psYS"])
                            if idx > 0:
                                cx.op("vector", lambda e, g=g: e.tensor_tensor(
                                    out=state[g][:].rearrange("p (h d) -> p h d", d=64), in0=state[g][:].rearrange("p (h d) -> p h d", d=64),
                                    in1=cd[g][:].unsqueeze(2).to_broadcast([128, 8, 64]), op=ALU.mult),
                                    reads=["state" + G, "cd" + G], writes=["state" + G])
                                cx.op("vector", lambda e, g=g: e.tensor_tensor(out=state[g][:], in0=state[g][:], in1=psYS[:], op=ALU.add),
                                      reads=["state" + G, "psYS"], writes=["state" + G])
                            else:
                                cx.op("vector", lambda e, g=g: e.tensor_copy(out=state[g][:], in_=psYS[:]),
                                      reads=["psYS"], writes=["state" + G])
                            cx.op("scalar", lambda e, g=g: e.activation(out=stbf[g][:], in_=state[g][:], func=AF.Copy),
                                  reads=["state" + G], writes=["stbf" + G])
                    ykeys = [(kyd, 0), (kyd, 1)]
                    if not fwd:
                        cx.dma("gpsimd", kyd, yb_d[tsl, :], yd[:], reads=ykeys, writes=[("yb_d", i)])
                    else:
                        cx.op("vector", lambda e, yd=yd, p=p: e.tensor_tensor(out=yd[:], in0=yd[:], in1=yb_t[p][:], op=ALU.add),
                              reads=ykeys + ["yb_t%d" % p], writes=ykeys)
                        cx.op("vector", lambda e, p=p: e.tensor_tensor(
                            out=gg[:].rearrange("p (h d) -> p h d", d=64), in0=xs_t[p][:].rearrange("p (h d) -> p h d", d=64),
                            in1=ssdp[:, 64:80].unsqueeze(2).to_broadcast([128, 16, 64]), op=ALU.mult),
                            reads=[kxs, "ssdp"], writes=["gg"])
                        cx.op("vector", lambda e, yd=yd: e.tensor_tensor(out=gg[:], in0=gg[:], in1=yd[:], op=ALU.add),
                              reads=["gg"] + ykeys, writes=["gg"])
                        cx.op("vector", lambda e, p=p: e.tensor_tensor(out=gg[:], in0=gg[:], in1=zs_t[p][:], op=ALU.mult),
                              reads=["gg", "zs_t%d" % p], writes=["gg"])
                        cx.op("scalar", lambda e: e.activation(out=junk2[:], in_=gg[:], func=AF.Square, accum_out=ssq[:]),
                              reads=["gg"], writes=["junk2", "ssq1"])
                        cx.op("vector", lambda e: e.tensor_scalar(out=ssq[:], in0=ssq[:], scalar1=1.0 / 1024, scalar2=EPS,
                                                                  op0=ALU.mult, op1=ALU.add), reads=["ssq1"], writes=["ssq1"])
                        cx.op("scalar", lambda e: e.activation(out=ssq[:], in_=ssq[:], func=AF.Sqrt), reads=["ssq1"], writes=["ssq1"])
                        cx.op("vector", lambda e: e.reciprocal(out=ssq[:], in_=ssq[:]), reads=["ssq1"], writes=["ssq1"])
                        cx.op("vector", lambda e, p=p: e.scalar_tensor_tensor(out=ssdtm[p][:], in0=gg[:], scalar=ssq[:, 0:1], in1=ssdnw[:],
                                                                              op0=ALU.mult, op1=ALU.mult),
                              reads=["gg", "ssq1", "ssdnw"], writes=["ssdtm0"])
                        cx.dma("gpsimd", "ssdtm0", ssdtm_d[tsl, :], ssdtm[p][:], reads=["ssdtm0"], writes=[("ssdtm_d", i)])

            ssd_pass(False)
            ssd_pass(True)
            cx.barrier()
        with contextlib.ExitStack() as st:
            psT8 = [st.enter_context(nc.psum_tensor("psT8%d" % i, [128, 8, 128], BF16)) for i in range(2)]
            stl = [st.enter_context(nc.sbuf_tensor("y_stl%d" % i, [128, 1024], BF16)) for i in range(2)]
            for i in range(NT):
                p = i % 2
                cx.dma("sync", "stl%d" % p, stl[p][:], ssdtm_d[i * 128:(i + 1) * 128, :], reads=[("ssdtm_d", i)], writes=["stl%d" % p])
                for c in range(8):
                    cx.op("tensor", lambda e, c=c, p=p: e.transpose(out=psT8[p][:, c, :], in_=stl[p][:, c * 128:(c + 1) * 128],
                                                                    identity=identb[:]), reads=["stl%d" % p], writes=["psT8%d" % p])
                cx.op("scalar", lambda e, p=p, i=i: e.activation(out=ssdT[:, :, i * 128:(i + 1) * 128], in_=psT8[p][:], func=AF.Copy),
                      reads=["psT8%d" % p], writes=[("hT", i)])
            if "d_ssdT" in debug:
                cx.dma("sync", "dbg", dbg_out["d_ssdT"][:, :], ssdT[:].rearrange("p c s -> p (c s)"),
                       reads=[("hT", i) for i in range(NT)])
            cx.barrier()


        g2bc = sb("g2bc", [128, D], F32)
        g1scope = contextlib.ExitStack()
        g1bc = g1scope.enter_context(nc.sbuf_tensor("s_g1bc", [128, D], F32))
        with contextlib.ExitStack() as st:
            diag = [st.enter_context(nc.sbuf_tensor("diag%d" % i, [128, 128], F32)) for i in range(2)]
            psG = st.enter_context(nc.psum_tensor("psG", [128, 1024], F32))
            for (dst, kd, c0) in [(g1bc, "g1bc", 16), (g2bc, "g2bc", 40)]:
                for c in range(8):
                    dg = diag[c % 2]; kg = "diag%d" % (c % 2)
                    cx.op("vector", lambda e, dg=dg, c=c, c0=c0: e.tensor_scalar(out=dg[:], in0=ident[:], scalar1=modT[:, c0 + c:c0 + c + 1],
                                                                                 scalar2=None, op0=ALU.mult), reads=["ident", "modT"], writes=[kg])
                    cx.op("tensor", lambda e, dg=dg, c=c: e.matmul(psG[:, c * 128:(c + 1) * 128], lhsT=ones_f[:], rhs=dg[:], start=True, stop=True),
                          reads=[kg, "ones_f"], writes=["psG"])
                cx.op("vector", lambda e, dst=dst: e.tensor_copy(out=dst[:], in_=psG[:]), reads=["psG"], writes=[kd])
            cx.barrier()
        ssdT = hT
        with contextlib.ExitStack() as st:
            def T(name, shape, dt):
                return st.enter_context(nc.sbuf_tensor("z_" + name, list(shape), dt))
            wau = T("wau", [128, 4, D], BF16)
            wsu = T("wsu", [128, 8, D], BF16)
            wout = T("wout", [128, 8, D], BF16)
            attg = [T("attg%d" % i, [128, 4, 512], BF16) for i in range(2)]
            g0t = [T("g0t%d" % i, [128, 512], BF16) for i in range(2)]
            g1t = [T("g1t%d" % i, [128, 512], BF16) for i in range(2)]
            t1 = T("t1", [128, 512], F32)
            t2 = T("t2", [128, 512], F32)
            mT = T("mT", [128, 8, 512], BF16)
            xin = [T("xin%d" % i, [128, D], F32) for i in range(2)]
            x1t = [T("x1t%d" % i, [128, D], F32) for i in range(2)]
            psUa = [st.enter_context(nc.psum_tensor("psUa%d" % i, [128, 512], F32)) for i in range(2)]
            psUs = [st.enter_context(nc.psum_tensor("psUs%d" % i, [128, 512], F32)) for i in range(2)]
            psM = [st.enter_context(nc.psum_tensor("psM%d" % i, [128, 1024], F32)) for i in range(2)]
            cx.dma("gpsimd", "wau", wau[:], wau_in.rearrange("(kc p) n -> p kc n", p=128), writes=["wau"])
            cx.dma("gpsimd", "wsu", wsu[:], wsu_in.rearrange("(kc p) n -> p kc n", p=128), writes=["wsu"])
            cx.dma("gpsimd", "wout", wout[:], wout_in.rearrange("(kc p) n -> p kc n", p=128), writes=["wout"])
            n = 0
            for grp in range(8):
                gs = slice(grp * 512, (grp + 1) * 512)
                ag = attg[grp % 2]; ka = "attg%d" % (grp % 2)
                cx.dma("sync", ka, ag[:], attT_d[:, gs].rearrange("(c p) s -> p c s", p=128), reads=["attT_d"], writes=[ka])
                for dc in range(8):
                    b = n % 2; n += 1
                    dsl = slice(dc * 128, (dc + 1) * 128)
                    cx.dma("sync", "g0t%d" % b, g0t[b][:], gatesT_d[dc * 128:(dc + 1) * 128, gs], writes=["g0t%d" % b])
                    cx.dma("sync", "g1t%d" % b, g1t[b][:], gatesT_d[1024 + dc * 128:1024 + (dc + 1) * 128, gs], writes=["g1t%d" % b])
                    for kc in range(4):
                        cx.op("tensor", lambda e, b=b, kc=kc, dsl=dsl, ag=ag: e.matmul(psUa[b][:], lhsT=wau[:, kc, dsl], rhs=ag[:, kc, :],
                                                                                     start=(kc == 0), stop=(kc == 3)),
                              reads=["wau", ka], writes=["psUa%d" % b])
                    for kc in range(8):
                        cx.op("tensor", lambda e, b=b, kc=kc, dsl=dsl, gs=gs: e.matmul(psUs[b][:], lhsT=wsu[:, kc, dsl], rhs=ssdT[:, kc, gs],
                                                                                     start=(kc == 0), stop=(kc == 7)),
                              reads=["wsu"] + [("hT", grp * 4 + k) for k in range(4)], writes=["psUs%d" % b])
                    cx.op("vector", lambda e, b=b: e.tensor_tensor(out=t1[:], in0=psUa[b][:], in1=g0t[b][:], op=ALU.mult),
                          reads=["psUa%d" % b, "g0t%d" % b], writes=["t1"])
                    cx.op("vector", lambda e, b=b: e.tensor_tensor(out=t2[:], in0=psUs[b][:], in1=g1t[b][:], op=ALU.mult),
                          reads=["psUs%d" % b, "g1t%d" % b], writes=["t2"])
                    cx.op("vector", lambda e, dc=dc: e.tensor_tensor(out=mT[:, dc, :], in0=t1[:], in1=t2[:], op=ALU.add),
                          reads=["t1", "t2"], writes=[("mT", dc)])
                for sub in range(4):
                    i = grp * 4 + sub
                    p = i % 2
                    tsl = slice(i * 128, (i + 1) * 128)
                    cx.dma("sync", "xin%d" % p, xin[p][:], x[tsl, :], writes=["xin%d" % p])
                    for half in range(2):
                        for dc in range(8):
                            cx.op("tensor", lambda e, p=p, half=half, dc=dc, sub=sub: e.matmul(
                                psM[p][:, half * 512:(half + 1) * 512], lhsT=mT[:, dc, sub * 128:(sub + 1) * 128],
                                rhs=wout[:, dc, half * 512:(half + 1) * 512], start=(dc == 0), stop=(dc == 7)),
                                reads=["wout"] + [("mT", d_) for d_ in range(8)], writes=["psM%d" % p])
                    cx.op("vector", lambda e, p=p: e.tensor_tensor(out=x1t[p][:], in0=psM[p][:], in1=g1bc[:], op=ALU.mult),
                          reads=["psM%d" % p, "g1bc"], writes=["x1t%d" % p])
                    cx.op("vector", lambda e, p=p: e.tensor_tensor(out=x1t[p][:], in0=x1t[p][:], in1=xin[p][:], op=ALU.add),
                          reads=["x1t%d" % p, "xin%d" % p], writes=["x1t%d" % p])
                    cx.dma("sync", "x1t%d" % p, x1_d[tsl, :], x1t[p][:], reads=["x1t%d" % p], writes=[("x1_d", i)])
            cx.barrier()
        g1scope.close()
        with contextlib.ExitStack() as st:
            norm_mod_transpose(x1_d, gam2, modT[:, 24:32], st, "b", rkey="x1_d")
            if "d_h2T" in debug:
                cx.dma("sync", "dbg", dbg_out["d_h2T"][:, :], hT[:].rearrange("p c s -> p (c s)"),
                       reads=[("hT", i) for i in range(NT)])
            cx.barrier()


        h2T = hT
        i1T = sb("i1T", [128, S], BF16)
        i2T = sb("i2T", [128, S], BF16)
        gT = sb("gT", [128, S], BF16)
        GELU = AF.Gelu_apprx_tanh
        with contextlib.ExitStack() as st:
            def T(name, shape, dt):
                return st.enter_context(nc.sbuf_tensor("r_" + name, list(shape), dt))
            wq = T("wq", [128, 8, 2048], BF16)
            keysT = T("keysT", [128, 16, 128], BF16)
            iota16 = T("iota16", [128, 16], F32)
            qT = T("qT", [128, 16, 512], BF16)
            bufA = T("bufA", [128, 2048], F32)
            bufB = T("bufB", [128, 2048], F32)
            sc = bufA[:].rearrange("p (a b) -> p a b", b=128)
            sc2 = bufB[:].rearrange("p (a b) -> p a b", b=128)
            cand = bufA[:].rearrange("p (a b) -> p a b", b=256)
            cand2 = bufB[:].rearrange("p (a b) -> p a b", b=256)
            oh = bufA[:].rearrange("p (a b) -> p a b", b=16)
            vtop = T("vtop", [128, 16, 16], F32)
            ixu = T("ixu", [128, 16, 16], U32)
            ixf = T("ixf", [128, 16, 16], F32)
            sc16 = T("sc16", [128, 8, 16], F32)
            posu = T("posu", [128, 8, 16], U32)
            au = T("au", [128, 8, 16], U32)
            bu = T("bu", [128, 8, 16], U32)
            af = T("af", [128, 8, 16], F32)
            bf = T("bf", [128, 8, 16], F32)
            esum = T("esum", [128, 8], F32)
            gf = T("gf", [128, 8, 16], F32)
            idf = [T("idf%d" % m, [128, 128], F32) for m in range(2)]
            psQ = st.enter_context(nc.psum_tensor("psQ", [128, 512], F32))
            psSc = st.enter_context(nc.psum_tensor("psSc", [128, 16, 128], F32))
            psT3 = st.enter_context(nc.psum_tensor("psT3", [128, 3, 128], F32))
            cx.dma("gpsimd", "wq", wq[:], wq_in.rearrange("(kc p) n -> p kc n", p=128), writes=["wq"])
            cx.dma("gpsimd", "r0", keysT[:].rearrange("p a b -> p (a b)"), keysT_in[:, :], writes=["keysT"])
            cx.dma("sync", "r1", iota16[:], iota16_in[:, :], writes=["iota16"])
            for grp in range(8):
                gs = slice(grp * 512, (grp + 1) * 512)
                for j in range(16):
                    for kc in range(8):
                        cx.op("tensor", lambda e, j=j, kc=kc, gs=gs: e.matmul(psQ[:], lhsT=wq[:, kc, j * 128:(j + 1) * 128], rhs=h2T[:, kc, gs],
                                                                             start=(kc == 0), stop=(kc == 7)), reads=["wq"], writes=["psQ"])
                    cx.op("scalar", lambda e, j=j: e.activation(out=qT[:, j, :], in_=psQ[:], func=AF.Copy), reads=["psQ"], writes=[("qT", j)])
                for sub in range(4):
                    i = grp * 4 + sub
                    tsl = slice(i * 128, (i + 1) * 128)
                    for j in range(16):
                        cx.op("tensor", lambda e, j=j, sub=sub: e.matmul(psSc[:, j, :], lhsT=qT[:, j, sub * 128:(sub + 1) * 128], rhs=keysT[:, j, :],
                                                                         start=True, stop=True), reads=[("qT", j), "keysT"], writes=["psSc"])
                    cx.op("scalar", lambda e: e.activation(out=sc, in_=psSc[:], func=AF.Copy), reads=["psSc"], writes=["bufA"])
                    for j in range(16):
                        cx.op("vector", lambda e, j=j: e.max(out=vtop[:, j, 0:8], in_=sc[:, j, :]), reads=["bufA"], writes=[("vtop", j)])
                    for j in range(16):
                        cx.op("vector", lambda e, j=j: e.max_index(out=ixu[:, j, 0:8], in_max=vtop[:, j, 0:8], in_values=sc[:, j, :]),
                              reads=["bufA", ("vtop", j)], writes=[("ixu", j)])
                    for j in range(16):
                        cx.op("vector", lambda e, j=j: e.match_replace(out=sc2[:, j, :], in_to_replace=vtop[:, j, 0:8], in_values=sc[:, j, :],
                                                                       imm_value=-1e30), reads=["bufA", ("vtop", j)], writes=[("bufB", j)])
                    for j in range(16):
                        cx.op("vector", lambda e, j=j: e.max(out=vtop[:, j, 8:16], in_=sc2[:, j, :]), reads=[("bufB", j)], writes=[("vtop", j)])
                    for j in range(16):
                        cx.op("vector", lambda e, j=j: e.max_index(out=ixu[:, j, 8:16], in_max=vtop[:, j, 8:16], in_values=sc2[:, j, :]),
                              reads=[("bufB", j), ("vtop", j)], writes=[("ixu", j)])
                    vk = [("vtop", j) for j in range(16)]
                    ik = [("ixu", j) for j in range(16)]
                    cx.op("vector", lambda e: e.tensor_copy(out=ixf[:], in_=ixu[:]), reads=ik, writes=["ixf"])
                    vv = vtop[:].rearrange("p (h two) k -> p h two k", two=2)
                    cx.op("vector", lambda e, vv=vv: e.tensor_tensor(
                        out=cand.rearrange("p h (a b) -> p h a b", b=16), in0=vv[:, :, 0, :].unsqueeze(3).to_broadcast([128, 8, 16, 16]),
                        in1=vv[:, :, 1, :].unsqueeze(2).to_broadcast([128, 8, 16, 16]), op=ALU.add), reads=vk, writes=["bufA"])
                    for h in range(8):
                        cx.op("vector", lambda e, h=h: e.max(out=sc16[:, h, 0:8], in_=cand[:, h, :]), reads=["bufA"], writes=[("sc16", h)])
                    for h in range(8):
                        cx.op("vector", lambda e, h=h: e.max_index(out=posu[:, h, 0:8], in_max=sc16[:, h, 0:8], in_values=cand[:, h, :]),
                              reads=["bufA", ("sc16", h)], writes=[("posu", h)])
                    for h in range(8):
                        cx.op("vector", lambda e, h=h: e.match_replace(out=cand2[:, h, :], in_to_replace=sc16[:, h, 0:8], in_values=cand[:, h, :],
                                                                       imm_value=-1e30), reads=["bufA", ("sc16", h)],
                              writes=[("bufB", 2 * h), ("bufB", 2 * h + 1)])
                    for h in range(8):
                        cx.op("vector", lambda e, h=h: e.max(out=sc16[:, h, 8:16], in_=cand2[:, h, :]),
                              reads=[("bufB", 2 * h), ("bufB", 2 * h + 1)], writes=[("sc16", h)])
                    for h in range(8):
                        cx.op("vector", lambda e, h=h: e.max_index(out=posu[:, h, 8:16], in_max=sc16[:, h, 8:16], in_values=cand2[:, h, :]),
                              reads=[("bufB", 2 * h), ("bufB", 2 * h + 1), ("sc16", h)], writes=[("posu", h)])
                    sk = [("sc16", h) for h in range(8)]
                    pk = [("posu", h) for h in range(8)]
                    cx.op("vector", lambda e: e.tensor_tensor(out=gf[:], in0=sc16[:], in1=sc16[:, :, 0:1].to_broadcast([128, 8, 16]),
                                                              op=ALU.subtract), reads=sk, writes=["gf"])
                    cx.op("scalar", lambda e: e.activation(out=gf[:], in_=gf[:], func=AF.Exp), reads=["gf"], writes=["gf"])
                    cx.op("vector", lambda e: e.tensor_reduce(out=esum[:], in_=gf[:], axis=AX.X, op=ALU.add), reads=["gf"], writes=["esum"])
                    cx.op("vector", lambda e: e.reciprocal(out=esum[:], in_=esum[:]), reads=["esum"], writes=["esum"])
                    cx.op("vector", lambda e: e.tensor_tensor(out=gf[:], in0=gf[:], in1=esum[:].unsqueeze(2).to_broadcast([128, 8, 16]),
                                                              op=ALU.mult), reads=["gf", "esum"], writes=["gf"])
                    cx.op("vector", lambda e: e.tensor_single_scalar(out=au[:], in_=posu[:], scalar=4, op=ALU.logical_shift_right),
                          reads=pk, writes=["au"])
                    cx.op("vector", lambda e: e.tensor_single_scalar(out=bu[:], in_=posu[:], scalar=15, op=ALU.bitwise_and),
                          reads=pk, writes=["bu"])
                    cx.op("vector", lambda e: e.tensor_copy(out=af[:], in_=au[:]), reads=["au"], writes=["af"])
                    cx.op("vector", lambda e: e.tensor_copy(out=bf[:], in_=bu[:]), reads=["bu"], writes=["bf"])
                    ixv = ixf[:].rearrange("p (h two) k -> p h two k", two=2)
                    for m, (sel, ksel) in enumerate([(af, "af"), (bf, "bf")]):
                        cx.op("vector", lambda e, sel=sel: e.tensor_tensor(
                            out=oh, in0=sel[:].rearrange("p h k -> p (h k)").unsqueeze(2).to_broadcast([128, 128, 16]),
                            in1=iota16[:].unsqueeze(1).to_broadcast([128, 128, 16]), op=ALU.is_equal), reads=[ksel, "iota16"], writes=["bufA"])
                        cx.op("vector", lambda e, m=m, ixv=ixv: e.tensor_tensor(
                            out=oh.rearrange("p (h k) a -> p h k a", k=16), in0=oh.rearrange("p (h k) a -> p h k a", k=16),
                            in1=ixv[:, :, m, :].unsqueeze(2).to_broadcast([128, 8, 16, 16]), op=ALU.mult), reads=["bufA", "ixf"], writes=["bufA"])
                        cx.op("vector", lambda e, m=m: e.tensor_reduce(out=idf[m][:], in_=oh, axis=AX.X, op=ALU.add),
                              reads=["bufA"], writes=["idf%d" % m])
                    cx.op("tensor", lambda e: e.transpose(out=psT3[:, 0, :], in_=idf[0][:], identity=ident[:]), reads=["idf0"], writes=["psT3"])
                    cx.op("tensor", lambda e: e.transpose(out=psT3[:, 1, :], in_=idf[1][:], identity=ident[:]), reads=["idf1"], writes=["psT3"])
                    cx.op("tensor", lambda e: e.transpose(out=psT3[:, 2, :], in_=gf[:].rearrange("p h k -> p (h k)"), identity=ident[:]),
                          reads=["gf"], writes=["psT3"])
                    cx.op("scalar", lambda e: e.activation(out=i1T[:, tsl], in_=psT3[:, 0, :], func=AF.Copy), reads=["psT3"], writes=[("rt", i)])
                    cx.op("scalar", lambda e: e.activation(out=i2T[:, tsl], in_=psT3[:, 1, :], func=AF.Identity, scale=-1.0), reads=["psT3"], writes=[("rt", i)])
                    cx.op("scalar", lambda e: e.activation(out=gT[:, tsl], in_=psT3[:, 2, :], func=AF.Copy), reads=["psT3"], writes=[("rt", i)])
            for kc in range(8):
                cx.dma("sync", "h2Td", h2T_d[kc * 128:(kc + 1) * 128, :], h2T[:, kc, :])
            for nm, t in [("d_i1T", i1T), ("d_i2T", i2T), ("d_gT", gT)]:
                if nm in debug:
                    cx.dma("sync", "dbg", dbg_out[nm][:, :], t[:])
            cx.barrier()

        with contextlib.ExitStack() as st:
            def T(name, shape, dt):
                return st.enter_context(nc.sbuf_tensor("p_" + name, list(shape), dt))
            TG = 256
            iota128f = T("iota128f", [128, 128], F32)
            iota128 = T("iota128", [128, 128], BF16)
            niota128 = T("niota128", [128, 128], BF16)
            Btmp = [T("Btmp%d" % i, [128, 128], BF16) for i in range(4)]
            fnw = T("fnw", [128, D], F32)
            gwb = [T("gw", [128, 128, TG], BF16), hT[:].rearrange("p c (a b) -> p (c a) b", b=TG)]
            h2g = [T("h2g%d" % i, [128, 8, TG], BF16) for i in range(2)]
            Aoh = [T("Aoh%d" % i, [128, 128], BF16) for i in range(4)]
            Boh = [T("Boh%d" % i, [128, 128], BF16) for i in range(4)]
            ut = [T("ut%d" % i, [128, 2, 8, 128], BF16) for i in range(2)]
            vt = [T("vt%d" % i, [128, 2, D], BF16) for i in range(2)]
            gel = [T("gel%d" % i, [128, TG], F32) for i in range(2)]
            Pb = [T("Pb%d" % i, [128, TG], BF16) for i in range(2)]
            x1s = [T("x1s%d" % i, [128, D], F32) for i in range(1)] * 2
            xo = [T("xo%d" % i, [128, D], F32) for i in range(1)] * 2
            ssq = T("ssq3", [128, 2], F32)
            psW = [st.enter_context(nc.psum_tensor("psW%d" % i, [128, 4, 128], F32)) for i in range(2)]
            psA = [st.enter_context(nc.psum_tensor("psA_%d" % i, [128, 512], F32)) for i in range(2)]
            psO = [st.enter_context(nc.psum_tensor("psO_%d" % i, [128, 1024], F32)) for i in range(2)]
            cx.dma("sync", "p0", iota128f[:], iota128_in[:, :], writes=["iota128f"])
            cx.op("vector", lambda e: e.tensor_copy(out=iota128[:], in_=iota128f[:]), reads=["iota128f"], writes=["iota128"])
            cx.op("vector", lambda e: e.tensor_scalar(out=niota128[:], in0=iota128f[:], scalar1=-1.0, scalar2=None, op0=ALU.mult),
                  reads=["iota128f"], writes=["niota128"])
            cx.dma("sync", "p1", fnw[:], fnw_in.partition_broadcast(128), writes=["fnw"])
            NG = S // TG
            nWc = [0]

            def gw_onehots(grp, q4, toks):
                t0 = grp * TG
                for tq in toks:
                    col = t0 + q4 * 4 + tq
                    r = tq
                    cx.op("vector", lambda e, r=r, col=col: e.tensor_scalar(
                        out=Aoh[r][:], in0=iota128[:], scalar1=i1T[:, col:col + 1], scalar2=gT[:, col:col + 1],
                        op0=ALU.is_equal, op1=ALU.mult), reads=["iota128"], writes=["Aoh%d" % r])
                    if tq % 2 == 1:
                        cx.op("vector", lambda e, r=r, col=col: e.tensor_scalar(
                            out=Boh[r][:], in0=niota128[:], scalar1=i2T[:, col:col + 1], scalar2=None,
                            op0=ALU.is_equal), reads=["niota128"], writes=["Boh%d" % r])
                    else:
                        cx.op("scalar", lambda e, r=r, col=col: e.activation(out=Btmp[r][:], in_=iota128[:], func=AF.Abs,
                                                                              bias=i2T[:, col:col + 1], scale=1.0),
                              reads=["iota128"], writes=["Btmp%d" % r])
                        cx.op("scalar", lambda e, r=r: e.activation(out=Boh[r][:], in_=Btmp[r][:], func=AF.Relu, bias=1.0, scale=-1.0),
                              reads=["Btmp%d" % r], writes=["Boh%d" % r])

            def gw_mm(grp, q4):
                b = q4 % 2
                for tq in range(4):
                    cx.op("tensor", lambda e, b=b, tq=tq: e.matmul(psW[b][:, tq, :], lhsT=Aoh[tq][:], rhs=Boh[tq][:], start=True, stop=True),
                          reads=["Aoh%d" % tq, "Boh%d" % tq], writes=["psW%d" % b])

            def gw_evac(grp, q4):
                b = q4 % 2
                g_ = gwb[grp % 2]
                cx.op("scalar", lambda e: e.activation(out=g_[:, :, q4 * 4:(q4 + 1) * 4],
                                                       in_=psW[b][:].rearrange("p t j -> p j t"), func=AF.Copy),
                      reads=["psW%d" % b], writes=["gw%d" % (grp % 2)])

            def emit_gw(grp, q4):
                gw_onehots(grp, q4, (0, 1, 2, 3))
                gw_mm(grp, q4)
                gw_evac(grp, q4)

            def load_h2(grp):
                cx.dma("gpsimd", "h2g%d" % (grp % 2), h2g[grp % 2][:], h2T_d[:, grp * TG:(grp + 1) * TG].rearrange("(c p) t -> p c t", p=128),
                       writes=["h2g%d" % (grp % 2)])

            def load_w(gb):
                jb = (gb % 64) * 2
                pb = gb % 2
                rs = slice(jb * 128, (jb + 2) * 128)
                bs = slice((gb % 64) * 128, (gb % 64 + 1) * 128)
                cx.dma("sync", "ut%d" % pb, ut[pb][:].rearrange("p t a b -> p (t a b)"), u_bf[bs, :],
                       reads=[("u_bf", jb), ("u_bf", jb + 1)], writes=["ut%d" % pb])
                cx.dma("sync", "vt%d" % pb, vt[pb][:].rearrange("p t n -> p (t n)"), v_bf[bs, :],
                       reads=[("v_bf", jb), ("v_bf", jb + 1)], writes=["vt%d" % pb])

            load_h2(0)
            load_w(0)
            load_w(1)
            for q4 in range(TG // 4):
                emit_gw(0, q4)
            for grp in range(NG):
                t0 = grp * TG
                gw = gwb[grp % 2]
                kgw = "gw%d" % (grp % 2)
                hg = h2g[grp % 2]
                khg = "h2g%d" % (grp % 2)
                if grp + 1 < NG:
                    load_h2(grp + 1)

                def emit_u(j):
                    p3 = (j // 2) % 2
                    ja = j % 2
                    b = j % 2
                    for kc in range(8):
                        cx.op("tensor", lambda e, kc=kc: e.matmul(psA[b][:, 0:TG], lhsT=ut[p3][:, ja, kc, :], rhs=hg[:, kc, :],
                                                                  start=(kc == 0), stop=(kc == 7)),
                              reads=["ut%d" % p3, khg], writes=["psA_%d" % b])

                emit_u(0)
                for j in range(128):
                    p3 = (j // 2) % 2
                    ja = j % 2
                    b = j % 2
                    if j + 1 < 128:
                        emit_u(j + 1)
                    cx.op("scalar", lambda e, b=b: e.activation(out=gel[b][:], in_=psA[b][:, 0:TG], func=GELU),
                          reads=["psA_%d" % b], writes=["gel%d" % b])
                    cx.op("vector", lambda e, b=b, j=j: e.tensor_tensor(out=Pb[b][:], in0=gel[b][:], in1=gw[:, j, :], op=ALU.mult),
                          reads=["gel%d" % b, kgw], writes=["Pb%d" % b])
                    for sub in range(2):
                        for half in range(2):
                            cx.op("tensor", lambda e, b=b, sub=sub, half=half, p3=p3, j=j, ja=ja: e.matmul(
                                psO[sub][:, half * 512:(half + 1) * 512], lhsT=Pb[b][:, sub * 128:(sub + 1) * 128],
                                rhs=vt[p3][:, ja, half * 512:(half + 1) * 512], start=(j == 0), stop=(j == 127)),
                                reads=["Pb%d" % b, "vt%d" % p3], writes=["psO_%d" % sub])
                    if j % 2 == 1:
                        gb = grp * 64 + j // 2 + 2
                        if gb < NG * 64:
                            load_w(gb)
                    if grp + 1 < NG:
                        q4 = j // 2
                        if j % 2 == 0:
                            gw_onehots(grp + 1, q4, (0, 1))
                        else:
                            gw_onehots(grp + 1, q4, (2, 3))
                            gw_mm(grp + 1, q4)
                            if q4 > 0:
                                gw_evac(grp + 1, q4 - 1)
                if grp + 1 < NG:
                    gw_evac(grp + 1, 63)
                for sub in range(2):
                    i = grp * 2 + sub
                    tsl = slice(i * 128, (i + 1) * 128)
                    cx.dma("gpsimd", "x1s0", x1s[sub][:], x1_d[tsl, :], reads=[("x1_d", i)], writes=["x1s0"])
                    X = xo[sub]; kx = "xo0"
                    cx.op("vector", lambda e, X=X, sub=sub: e.tensor_tensor(out=X[:], in0=psO[sub][:], in1=g2bc[:], op=ALU.mult),
                          reads=["psO_%d" % sub], writes=[kx])
                    cx.op("vector", lambda e, X=X, sub=sub: e.tensor_tensor(out=X[:], in0=X[:], in1=x1s[sub][:], op=ALU.add),
                          reads=[kx, "x1s0"], writes=[kx])
                    cx.op("scalar", lambda e, X=X, sub=sub: e.activation(out=x1s[sub][:], in_=X[:], func=AF.Square, accum_out=ssq[:, sub:sub + 1]),
                          reads=[kx], writes=["x1s0", ("ssq3", sub)])
                    cx.op("vector", lambda e, sub=sub: e.tensor_scalar(out=ssq[:, sub:sub + 1], in0=ssq[:, sub:sub + 1], scalar1=1.0 / D, scalar2=EPS,
                                                                       op0=ALU.mult, op1=ALU.add), reads=[("ssq3", sub)], writes=[("ssq3", sub)])
                    cx.op("scalar", lambda e, sub=sub: e.activation(out=ssq[:, sub:sub + 1], in_=ssq[:, sub:sub + 1], func=AF.Sqrt),
                          reads=[("ssq3", sub)], writes=[("ssq3", sub)])
                    cx.op("vector", lambda e, sub=sub: e.reciprocal(out=ssq[:, sub:sub + 1], in_=ssq[:, sub:sub + 1]),
                          reads=[("ssq3", sub)], writes=[("ssq3", sub)])
                    cx.op("vector", lambda e, X=X, sub=sub: e.scalar_tensor_tensor(out=X[:], in0=X[:], scalar=ssq[:, sub:sub + 1], in1=fnw[:],
                                                                                  op0=ALU.mult, op1=ALU.mult),
                          reads=[kx, ("ssq3", sub), "fnw"], writes=[kx])
                    cx.dma("gpsimd", kx, out[tsl, :], X[:], reads=[kx], writes=[("out", i)])
            cx.barrier()

        cx.barrier()
    print("instructions:", cx.ninst)
    return nc


def make_in_maps(inputs, ncores=8):
    f = np.float32
    C, Sg = rope_tables()
    ident = np.eye(128, dtype=f)
    qperm = np.concatenate([np.arange(h * 64, (h + 1) * 64) for h in [0, 4, 1, 5, 2, 6, 3, 7]])
    cols = np.concatenate([qperm, np.arange(512, 768), np.arange(3328, 3360), np.arange(768, 1792),
                           np.arange(1792, 3328), np.arange(3360, 5408)])
    w_in_p = np.ascontiguousarray(inputs["w_in"][0][:, cols])
    qkgain = np.concatenate([np.tile(inputs["q_gain"][0], 8), np.tile(inputs["k_gain"][0], 2)])[None, :].astype(f)
    convwT = np.ascontiguousarray(inputs["conv_w"][0].reshape(5, 12, 128).transpose(2, 1, 0).reshape(128, 60))
    convbT = np.ascontiguousarray(inputs["conv_b"][0].reshape(12, 128).T)
    wau = np.ascontiguousarray(inputs["w_attn_up"][0][qperm, :])
    wq_h = np.ascontiguousarray(inputs["peer_w_query"][0])
    keysT_h = np.ascontiguousarray(np.stack([inputs["peer_keys1"][0], inputs["peer_keys2"][0]], axis=1)
                                   .transpose(3, 0, 1, 2).reshape(128, 2048))
    iota128 = np.tile(np.arange(128, dtype=f)[None, :], (128, 1))
    iota16 = np.tile(np.arange(16, dtype=f)[None, :], (128, 1))
    fnw_h = inputs["final_norm_w"][None, :].astype(f)
    U_h = np.ascontiguousarray(inputs["peer_u"][0].reshape(128, 128, 8, 128).transpose(1, 3, 2, 0)).reshape(128 * 128, D)
    V_h = np.ascontiguousarray(inputs["peer_v"][0].reshape(128, 128, D).transpose(1, 0, 2)).reshape(128 * 128, D)
    ii = np.arange(128)
    trif = (ii[:, None] <= ii[None, :]).astype(f)
    maskf = np.where(ii[None, :] >= ii[:, None], 0.0, -30000.0).astype(f)
    ssdp = np.concatenate([inputs["dt_bias_f"][0], inputs["dt_bias_b"][0], inputs["a_log_f"][0], inputs["a_log_b"][0],
                           inputs["d_skip"][0]])[None, :].astype(f)
    maps = []
    for b in range(ncores):
        m = {
            "x": np.ascontiguousarray(inputs["x"][b]),
            "c_col": np.ascontiguousarray(inputs["c"][b].reshape(8, 128).T),
            "ada_w": np.ascontiguousarray(inputs["ada_w"][0]),
            "ada_bT": np.ascontiguousarray(inputs["ada_b"][0].reshape(48, 128).T),
            "n1T": np.ascontiguousarray(inputs["norm1_w"][0].reshape(8, 128).T),
            "n2T": np.ascontiguousarray(inputs["norm2_w"][0].reshape(8, 128).T),
            "w_in": w_in_p,
            "trif": trif, "trib": np.ascontiguousarray(trif.T), "maskf": maskf, "maskb": np.ascontiguousarray(maskf.T),
            "ssdp": ssdp, "ssdnw": inputs["ssd_norm_w"][0][None, :].astype(f),
            "wau": wau, "wsu": np.ascontiguousarray(inputs["w_ssd_up"][0]), "wout": np.ascontiguousarray(inputs["w_out"][0]),
            "wq": wq_h, "keysT": keysT_h, "iota128": iota128, "iota16": iota16, "fnw": fnw_h, "U_h": U_h, "V_h": V_h,
            "ropeC": C, "ropeS": Sg, "qkgain": qkgain, "convwT": convwT, "convbT": convbT,
            "ident": ident,
        }
        maps.append(m)
    return maps


def kernel(**inputs):
    inputs = {k: np.asarray(v) for k, v in inputs.items()}
    nc = build()
    maps = make_in_maps(inputs)
    res = run_bass_kernel_spmd(nc, maps, core_ids=list(range(8)))
    return np.stack([r["out"] for r in res.results], axis=0).astype(np.float32)
```
